# Optimizing a Trainium2 kernel written in Bass

```python
import jax, jax.numpy as jnp
from jax import lax
import numpy as np

D_MODEL = 1024
BATCH = 8
SEQ = 2048
DEPTH = 1

CHUNK = 64
CONV_WIDTH = D_MODEL
CONV_K = 31
LRU_WIDTH = D_MODEL
LRU_HEADS = 8
LRU_HEAD_DIM = LRU_WIDTH // LRU_HEADS
LRU_CONV_K = 4
LRU_C = 8.0
N_BRANCH = 2
N_EXPERTS = 32
TOP_K = 4
D_EXPERT = D_MODEL
SWIGLU_LIMIT = 7.0
SWIGLU_ALPHA = 1.702
MOE_BLOCK = 128
LN_EPS = 1e-5
DEEPNORM_ALPHA = (2.0 * DEPTH) ** 0.25
DEEPNORM_BETA = (8.0 * DEPTH) ** -0.25
IN_COLS = 2 * CONV_WIDTH + 2 * LRU_WIDTH + N_BRANCH * D_MODEL

kernel_name = 'hybrid_conv_rglru_moe_block'


def layer_norm(x, gain=None, bias=None):
    xf = x.astype(jnp.float32)
    mu = xf.mean(-1, keepdims=True)
    var = jnp.square(xf - mu).mean(-1, keepdims=True)
    y = (xf - mu) * lax.rsqrt(var + LN_EPS)
    if gain is not None:
        y = y * gain.astype(jnp.float32) + bias.astype(jnp.float32)
    return y.astype(x.dtype)


def causal_depthwise_conv(x, w, b):
    k = w.shape[0]
    xp = jnp.pad(x, ((0, 0), (k - 1, 0), (0, 0)))
    y = lax.conv_general_dilated(xp, w[:, None, :], window_strides=(1,), padding='VALID',
                                 dimension_numbers=('NWC', 'WIO', 'NWC'),
                                 feature_group_count=x.shape[-1])
    return y + b


def rg_lru(x, w_a, b_a, w_x, b_x, lam):
    bsz, s, _ = x.shape
    xh = x.reshape(bsz, s, LRU_HEADS, LRU_HEAD_DIM)
    gate_a = jax.nn.sigmoid(jnp.einsum('bshi,hij->bshj', xh, w_a).reshape(bsz, s, LRU_WIDTH) + b_a)
    gate_x = jax.nn.sigmoid(jnp.einsum('bshi,hij->bshj', xh, w_x).reshape(bsz, s, LRU_WIDTH) + b_x)
    log_a = (-LRU_C * gate_a.astype(jnp.float32)) * jax.nn.softplus(-lam.astype(jnp.float32))
    a = jnp.exp(log_a)
    mult = jnp.sqrt(-jnp.expm1(2.0 * log_a))
    is_first = (jnp.arange(s) == 0)[None, :, None]
    mult = jnp.where(is_first, 1.0, mult)
    u = mult * (gate_x * x).astype(jnp.float32)

    def combine(lhs, rhs):
        a1, b1 = lhs
        a2, b2 = rhs
        return a1 * a2, a2 * b1 + b2

    _, h = lax.associative_scan(combine, (a, u), axis=1)
    return h.astype(x.dtype)


def hybrid_mixer(u, w_in, b_in, w_cv_dw, b_cv_dw, ln_cv_g, ln_cv_b, w_cv_out,
                 w_lru_conv, b_lru_conv, w_lru_a, b_lru_a, w_lru_x, b_lru_x, lru_lambda,
                 w_lru_out, w_o, b_o):
    z = u @ w_in + b_in
    cv_in, lru_in, lru_gate, merge = jnp.split(
        z, [2 * CONV_WIDTH, 2 * CONV_WIDTH + LRU_WIDTH, 2 * CONV_WIDTH + 2 * LRU_WIDTH], axis=-1)
    a_val, a_gate = jnp.split(cv_in, 2, axis=-1)
    ya = a_val * jax.nn.sigmoid(a_gate)
    ya = causal_depthwise_conv(ya, w_cv_dw, b_cv_dw)
    ya = jax.nn.silu(layer_norm(ya, ln_cv_g, ln_cv_b))
    ya = ya @ w_cv_out
    yb = causal_depthwise_conv(lru_in, w_lru_conv, b_lru_conv)
    yb = rg_lru(yb, w_lru_a, b_lru_a, w_lru_x, b_lru_x, lru_lambda)
    yb = (yb * jax.nn.gelu(lru_gate)) @ w_lru_out
    g_a, g_b = jnp.split(jax.nn.sigmoid(merge), 2, axis=-1)
    return (g_a * ya + g_b * yb) @ w_o + b_o


def moe_ffn(u, w_router, b_router, w_up, b_up, w_down, b_down):
    bsz, s, d = u.shape
    t = bsz * s
    h = u.reshape(t, d)
    logits = (h @ w_router + b_router).astype(jnp.float32)
    top_logits, top_idx = lax.top_k(logits, TOP_K)
    top_w = jax.nn.softmax(top_logits, axis=-1).astype(u.dtype)
    n_assign = t * TOP_K
    flat_e = top_idx.reshape(n_assign)
    flat_tok = jnp.repeat(jnp.arange(t, dtype=jnp.int32), TOP_K)
    flat_w = top_w.reshape(n_assign)
    order = jnp.argsort(flat_e)
    sorted_e = flat_e[order]
    counts = jnp.bincount(flat_e, length=N_EXPERTS)
    padded = (counts + MOE_BLOCK - 1) // MOE_BLOCK * MOE_BLOCK
    start = jnp.cumsum(counts) - counts
    pend = jnp.cumsum(padded)
    pstart = pend - padded
    dest = pstart[sorted_e] + jnp.arange(n_assign, dtype=jnp.int32) - start[sorted_e]
    n_blocks = -(-(n_assign + N_EXPERTS * MOE_BLOCK) // MOE_BLOCK)
    n_rows = n_blocks * MOE_BLOCK
    row_tok = jnp.full((n_rows,), t, jnp.int32).at[dest].set(flat_tok[order])
    row_w = jnp.zeros((n_rows,), u.dtype).at[dest].set(flat_w[order])
    block_e = jnp.minimum(
        jnp.searchsorted(pend, jnp.arange(n_blocks) * MOE_BLOCK, side='right'), N_EXPERTS - 1)
    h_pad = jnp.concatenate([h, jnp.zeros((1, d), h.dtype)], axis=0)
    xb = h_pad[row_tok].reshape(n_blocks, MOE_BLOCK, d)

    def expert_block(args):
        xblk, e = args
        zz = xblk @ w_up[e] + b_up[e]
        z_glu = jnp.minimum(zz[:, ::2], SWIGLU_LIMIT)
        z_lin = jnp.clip(zz[:, 1::2], -SWIGLU_LIMIT, SWIGLU_LIMIT)
        act = z_glu * jax.nn.sigmoid(SWIGLU_ALPHA * z_glu) * (z_lin + 1.0)
        return act @ w_down[e] + b_down[e]

    yb = lax.map(expert_block, (xb, block_e)).reshape(n_rows, d)
    y = jnp.zeros((t + 1, d), u.dtype).at[row_tok].add(yb * row_w[:, None])
    return y[:t].reshape(bsz, s, d)


def setup_inputs(seed: int = 0) -> dict:
    key = jax.random.key(seed)
    ks = iter(jax.random.split(key, 40))
    f32 = jnp.float32
    L, D = DEPTH, D_MODEL

    def nrm(shape, scale):
        return jax.random.normal(next(ks), shape, f32) * scale

    x = nrm((BATCH, SEQ, D), 1.0)
    c = nrm((BATCH, D), 1.0)
    lam_u = jax.random.uniform(next(ks), (L, LRU_WIDTH), f32, 0.9, 0.999)
    sig = lam_u ** (1.0 / LRU_C)
    lru_lambda = jnp.log(sig) - jnp.log1p(-sig)
    return {
        'x': x,
        'c': c,
        'w_ada': nrm((L, D, 6 * D), D ** -0.5),
        'b_ada': nrm((L, 6 * D), 0.02),
        'w_in': nrm((L, D, IN_COLS), D ** -0.5),
        'b_in': nrm((L, IN_COLS), 0.02),
        'w_cv_dw': nrm((L, CONV_K, CONV_WIDTH), CONV_K ** -0.5),
        'b_cv_dw': nrm((L, CONV_WIDTH), 0.02),
        'ln_cv_g': 1.0 + nrm((L, CONV_WIDTH), 0.02),
        'ln_cv_b': nrm((L, CONV_WIDTH), 0.02),
        'w_cv_out': nrm((L, CONV_WIDTH, D), CONV_WIDTH ** -0.5 * DEEPNORM_BETA),
        'w_lru_conv': nrm((L, LRU_CONV_K, LRU_WIDTH), LRU_CONV_K ** -0.5),
        'b_lru_conv': nrm((L, LRU_WIDTH), 0.02),
        'w_lru_a': nrm((L, LRU_HEADS, LRU_HEAD_DIM, LRU_HEAD_DIM), LRU_HEAD_DIM ** -0.5),
        'b_lru_a': nrm((L, LRU_WIDTH), 0.02),
        'w_lru_x': nrm((L, LRU_HEADS, LRU_HEAD_DIM, LRU_HEAD_DIM), LRU_HEAD_DIM ** -0.5),
        'b_lru_x': nrm((L, LRU_WIDTH), 0.02),
        'lru_lambda': lru_lambda,
        'w_lru_out': nrm((L, LRU_WIDTH, D), LRU_WIDTH ** -0.5 * DEEPNORM_BETA),
        'w_o': nrm((L, D, D), D ** -0.5 * DEEPNORM_BETA),
        'b_o': nrm((L, D), 0.02),
        'ln1_g': 1.0 + nrm((L, D), 0.02),
        'ln1_b': nrm((L, D), 0.02),
        'w_router': nrm((L, D, N_EXPERTS), D ** -0.5),
        'b_router': nrm((L, N_EXPERTS), 0.01),
        'w_up': nrm((L, N_EXPERTS, D, 2 * D_EXPERT), D ** -0.5 * DEEPNORM_BETA),
        'b_up': nrm((L, N_EXPERTS, 2 * D_EXPERT), 0.02),
        'w_down': nrm((L, N_EXPERTS, D_EXPERT, D), D_EXPERT ** -0.5 * DEEPNORM_BETA),
        'b_down': nrm((L, N_EXPERTS, D), 0.02),
        'ln2_g': 1.0 + nrm((L, D), 0.02),
        'ln2_b': nrm((L, D), 0.02),
    }


def reference(x, c, w_ada, b_ada, w_in, b_in, w_cv_dw, b_cv_dw, ln_cv_g, ln_cv_b, w_cv_out,
              w_lru_conv, b_lru_conv, w_lru_a, b_lru_a, w_lru_x, b_lru_x, lru_lambda,
              w_lru_out, w_o, b_o, ln1_g, ln1_b, w_router, b_router, w_up, b_up,
              w_down, b_down, ln2_g, ln2_b):
    h = x
    for l in range(DEPTH):
        mod = jax.nn.silu(c) @ w_ada[l] + b_ada[l]
        sh1, sc1, g1, sh2, sc2, g2 = jnp.split(mod[:, None, :], 6, axis=-1)
        u = layer_norm(h) * (1.0 + sc1) + sh1
        mix = hybrid_mixer(u, w_in[l], b_in[l], w_cv_dw[l], b_cv_dw[l], ln_cv_g[l], ln_cv_b[l],
                           w_cv_out[l], w_lru_conv[l], b_lru_conv[l], w_lru_a[l], b_lru_a[l],
                           w_lru_x[l], b_lru_x[l], lru_lambda[l], w_lru_out[l], w_o[l], b_o[l])
        h = layer_norm(DEEPNORM_ALPHA * h + g1 * mix, ln1_g[l], ln1_b[l])
        u = layer_norm(h) * (1.0 + sc2) + sh2
        ffn = moe_ffn(u, w_router[l], b_router[l], w_up[l], b_up[l], w_down[l], b_down[l])
        h = layer_norm(DEEPNORM_ALPHA * h + g2 * ffn, ln2_g[l], ln2_b[l])
    return h
```

```python
import numpy as np
from contextlib import ExitStack
import concourse.bass as bass
import concourse.mybir as mybir
from concourse.bass_utils import run_bass_kernel_spmd

F32 = mybir.dt.float32
BF16 = mybir.dt.bfloat16
AF = mybir.ActivationFunctionType
ALU = mybir.AluOpType
AX = mybir.AxisListType

S = 2048
D = 1024
NE = 32
ALPHA = float(2.0 ** 0.25)
EPS = 1e-5
NT = S // 128
NQ = S // 512
SEM_ROLL = 30000
CR = [min(2048, -(-(8192 // (r + 1)) // 128) * 128) for r in range(NE)]
CUM = [0]
for _c in CR:
    CUM.append(CUM[-1] + _c)
NSLOT = CUM[-1]
I32 = mybir.dt.int32


class TR:
    def __init__(self, nc, st):
        self.nc = nc
        self.st = st
        self.E = {'pe': nc.tensor, 'act': nc.scalar, 'dve': nc.vector, 'pool': nc.gpsimd, 'sp': nc.sync}
        self.sems = []
        self.own = {}
        self.cnt = {}
        self.seen = {e: {} for e in self.E}
        self.lastw = {}
        self.readers = {}
        self.dslots = {}
        self.drr = {}
        self.ownset = {e: set() for e in self.E}

    def newsem(self, name):
        h = self.st.enter_context(self.nc.semaphore(name))
        self.sems.append(h)
        return len(self.sems) - 1

    def _own(self, e):
        if e not in self.own or self.cnt[e] >= SEM_ROLL:
            self.own[e] = self.newsem("c_%s_%d" % (e, len(self.sems)))
            self.ownset[e].add(self.own[e])
            self.cnt[e] = 0
        return self.own[e]

    def _wait(self, e, tok):
        if tok is None:
            return
        s, v = tok
        if self.seen[e].get(s, 0) >= v:
            return
        self.seen[e][s] = v
        self.E[e].wait_ge(self.sems[s], v)

    def _dep(self, e, tok):
        if tok is None:
            return
        if e == 'pe' and tok[0] in self.ownset['pe']:
            return
        self._wait(e, tok)

    def _deps(self, e, reads, writes):
        for k in reads:
            self._dep(e, self.lastw.get(k))
        for k in writes:
            self._dep(e, self.lastw.get(k))
            r = self.readers.get(k)
            if r:
                for s, v in r.items():
                    self._dep(e, (s, v))

    def _record(self, tok, reads, writes):
        s, v = tok
        for k in reads:
            r = self.readers.setdefault(k, {})
            if r.get(s, 0) < v:
                r[s] = v
        for k in writes:
            self.lastw[k] = tok
            self.readers[k] = {}

    @staticmethod
    def _excl(reads, writes):
        ps = [k for k in reads if isinstance(k, tuple) and k[0] == 'ps']
        if not ps:
            return reads, writes
        return [k for k in reads if not (isinstance(k, tuple) and k[0] == 'ps')], list(writes) + ps

    def op(self, e, fn, reads=(), writes=(), signal=True):
        reads, writes = self._excl(reads, writes)
        self._deps(e, reads, writes)
        s = self._own(e)
        inst = fn(self.E[e])
        if signal:
            self.cnt[e] += 1
            inst.then_inc(self.sems[s], 1)
            tok = (s, self.cnt[e])
        else:
            tok = (s, self.cnt[e] + 1)
        self._record(tok, reads, writes)
        return tok

    def dma(self, q, out, in_, reads=(), writes=(), nslots=6, out_offset=None, in_offset=None):
        self._deps(q, reads, writes)
        if q not in self.dslots:
            self.dslots[q] = [[self.newsem("d_%s_%d" % (q, i)), 0] for i in range(nslots)]
            self.drr[q] = 0
        slot = self.dslots[q][self.drr[q]]
        self.drr[q] = (self.drr[q] + 1) % len(self.dslots[q])
        if slot[1] > 0:
            self._wait(q, (slot[0], slot[1]))
        if out_offset is not None or in_offset is not None:
            inst = self.E[q].indirect_dma_start(out=out, out_offset=out_offset, in_=in_, in_offset=in_offset)
        else:
            inst = self.E[q].dma_start(out=out, in_=in_)
        slot[1] += 16
        inst.then_inc(self.sems[slot[0]], 16)
        tok = (slot[0], slot[1])
        self._record(tok, reads, writes)
        return tok

    def barrier(self):
        toks = []
        for e in self.own:
            if self.cnt[e] > 0:
                toks.append((self.own[e], self.cnt[e]))
        for q in self.dslots:
            for s, v in self.dslots[q]:
                if v > 0:
                    toks.append((s, v))
        for e in self.E:
            for t in toks:
                if e == 'pe' and t[0] == self.own.get(e):
                    continue
                self._wait(e, t)
        self.lastw = {}
        self.readers = {}

    def finish(self):
        for q in self.dslots:
            for s, v in self.dslots[q]:
                if v > 0:
                    self._wait('sp', (s, v))
        for e in self.own:
            if e != 'sp' and self.cnt[e] > 0:
                self._wait('sp', (self.own[e], self.cnt[e]))


class Arena:
    def __init__(self, R, nbytes):
        self.R = R
        self.free = [(0, nbytes)]
        self.pending = []

    def alloc(self, shape, dt):
        esz = 2 if dt == BF16 else 4
        n = esz
        for s in shape[1:]:
            n *= s
        n = (n + 63) // 64 * 64
        for i, (o, l) in enumerate(self.free):
            if l >= n:
                if l == n:
                    self.free.pop(i)
                else:
                    self.free[i] = (o + n, l - n)
                v = self.R[:, o // 4:(o + n) // 4]
                if dt == BF16:
                    v = v.bitcast(BF16)
                tot = 1
                for s in shape[1:]:
                    tot *= s
                v = v[:, 0:tot]
                if len(shape) == 3:
                    v = v.rearrange("p (a b) -> p a b", a=shape[1])
                elif len(shape) == 4:
                    v = v.rearrange("p (a b c) -> p a b c", a=shape[1], b=shape[2])
                return v, (o, n)
        raise RuntimeError("arena full: need %d, free=%s" % (n, self.free))

    def release(self, blk):
        self.pending.append(blk)

    def commit(self):
        fl = sorted(self.free + self.pending)
        self.pending = []
        out = []
        for o, l in fl:
            if out and out[-1][0] + out[-1][1] == o:
                out[-1] = (out[-1][0], out[-1][1] + l)
            else:
                out.append((o, l))
        self.free = out


class Scope:
    def __init__(self, A, T):
        self.A = A
        self.T = T
        self.blks = []

    def __enter__(self):
        return self

    def alloc(self, shape, dt):
        v, b = self.A.alloc(shape, dt)
        self.blks.append(b)
        return v

    def __exit__(self, *a):
        if a[0] is None:
            self.T.barrier()
            for b in self.blks:
                self.A.release(b)
            self.A.commit()
        return False


def build(debug=None):
    nc = bass.Bass("TRN2", target_bir_lowering=False)

    def din(name, shape, dt=F32):
        return nc.dram_tensor(name, list(shape), dt, kind="ExternalInput").ap()

    x_d = din("x", [S, D])
    c_col_d = din("c_col", [128, 8])
    w_ada_d = din("w_ada", [D, 6 * D])
    b_ada_col_d = din("b_ada_col", [128, 48])
    b_ada_row_d = din("b_ada_row", [1, 6 * D])
    w_in_d = din("w_in", [D, 6 * D])
    b_in_col_d = din("b_in_col", [128, 48])
    w_cvdw_d = din("w_cv_dw_col", [128, 8, 31])
    b_cvdw_d = din("b_cv_dw_col", [128, 8])
    ln_cv_g_d = din("ln_cv_g_col", [128, 8])
    ln_cv_b_d = din("ln_cv_b_col", [128, 8])
    w_cv_out_d = din("w_cv_out", [D, D])
    w_lconv_d = din("w_lru_conv_col", [128, 8, 4])
    b_lconv_d = din("b_lru_conv_col", [128, 8])
    w_lru_a_d = din("w_lru_a_t", [128, 8, 128])
    b_lru_a_d = din("b_lru_a_col", [128, 8])
    w_lru_x_d = din("w_lru_x_t", [128, 8, 128])
    b_lru_x_d = din("b_lru_x_col", [128, 8])
    lam_d = din("lru_lambda_col", [128, 8])
    w_lru_out_d = din("w_lru_out", [D, D])
    w_o_d = din("w_o", [D, D])
    b_o_d = din("b_o_row", [1, D])
    ln1_g_d = din("ln1_g_row", [1, D])
    ln1_b_d = din("ln1_b_row", [1, D])
    w_router_d = din("w_router_t", [128, 8, NE])
    b_router_d = din("b_router_row", [1, NE])
    w_up_d = din("w_up", [NE, D, 2 * D])
    b_up_d = din("b_up_rows", [NE * 128, 16])
    w_down_d = din("w_down", [NE, D, D])
    b_down_d = din("b_down", [NE, D])
    ln2_g_d = din("ln2_g_row", [1, D])
    ln2_b_d = din("ln2_b_row", [1, D])
    out_d = nc.dram_tensor("out", [S, D], F32, kind="ExternalOutput").ap()
    h1_d = nc.dram_tensor("h1_scratch", [S, D], F32, kind="Internal").ap()
    g1row_d = nc.dram_tensor("g1row_scratch", [128, D], F32, kind="Internal").ap()
    g1bo_d = nc.dram_tensor("g1bo_scratch", [128, D], F32, kind="Internal").ap()
    g2row_d = nc.dram_tensor("g2row_scratch", [128, D], F32, kind="Internal").ap()
    sh2row_d = nc.dram_tensor("sh2row_scratch", [128, D], F32, kind="Internal").ap()
    sc2prow_d = nc.dram_tensor("sc2prow_scratch", [128, D], F32, kind="Internal").ap()
    u2tok_d = nc.dram_tensor("u2tok_scratch", [S, D], BF16, kind="Internal").ap()
    xs_d = nc.dram_tensor("xs_scratch", [NSLOT, D], BF16, kind="Internal").ap()
    ys_d = nc.dram_tensor("ys_scratch", [NSLOT, D], F32, kind="Internal").ap()
    w_up2 = w_up_d.rearrange("e d (h n) -> (e d h) n", h=2)
    w_down2 = w_down_d.rearrange("e d n -> (e d) n")
    dbg_d = None
    if debug is not None:
        dbg_d = nc.dram_tensor("dbg", [128, 8 * S], F32, kind="ExternalOutput").ap()

    with ExitStack() as st:
        T = TR(nc, st)
        ARENA_BYTES = 186 * 1024
        Rt = st.enter_context(nc.sbuf_tensor("arena", [128, ARENA_BYTES // 4], F32))
        A = Arena(Rt, ARENA_BYTES)

        def sb(name, shape, dt=F32, stack=None):
            if stack is None:
                return st.enter_context(nc.sbuf_tensor(name, list(shape), dt))
            return stack.alloc(list(shape), dt)

        def alloc_long(shape, dt):
            return A.alloc(list(shape), dt)

        def free_long(blk):
            T.barrier()
            A.release(blk)
            A.commit()

        psb = [st.enter_context(nc.psum_tensor("ps%d" % i, [128, 512], F32)) for i in range(8)]
        ps_rr = [0]

        def getps():
            i = ps_rr[0]
            ps_rr[0] = (i + 1) % 8
            return i

        def mmgroup(ps_ap, pskey, pairs, reads=()):
            n = len(pairs)
            tok = None
            for i, (l, r) in enumerate(pairs):
                last = (i == n - 1)
                tok = T.op('pe', lambda e, l=l, r=r, i=i, last=last: e.matmul(
                    ps_ap, lhsT=l, rhs=r, start=(i == 0), stop=last),
                    reads=reads if i == 0 else (), writes=[pskey] if i == 0 else (), signal=last)
            if n > 1:
                T._record(tok, reads, [pskey])
            return tok

        def transpose4(pi, src, k0, rkey):
            for q in range(4):
                k = k0 + q
                T.op('pe', lambda e, q=q, k=k: e.transpose(
                    out=psb[pi][:, 128 * q:128 * (q + 1)], in_=src[:, 128 * k:128 * (k + 1)],
                    identity=ident[:]),
                    reads=[rkey] if q == 0 else (), writes=[('ps', pi)] if q == 0 else (),
                    signal=(q == 3))
            T._record((T.own['pe'], T.cnt['pe']), [rkey], [('ps', pi)])

        ident = sb("ident", [128, 128])
        ones = sb("ones", [128, 128])
        epsc = sb("epsc", [128, 1])
        T.op('pool', lambda e: e.memset(ones[:], 1.0), writes=['ones'])
        T.op('pool', lambda e: e.memset(epsc[:], EPS), writes=['epsc'])
        T.op('pool', lambda e: e.affine_select(out=ident[:], in_=ones[:], pattern=[[-1, 128]],
                                               compare_op=ALU.is_equal, fill=0.0, base=0,
                                               channel_multiplier=1), reads=['ones'], writes=['ident'])

        identb = sb("identb", [128, 128], BF16)
        T.op('pool', lambda e: e.tensor_copy(out=identb[:], in_=ident[:]), reads=['ident'], writes=['identb'])

        def ld_small(name, src, shape):
            t = sb(name, shape)
            T.dma('sp', t[:], src, writes=[name])
            return t

        c_col = ld_small("c_col_s", c_col_d[:, :], [128, 8])
        b_ada_col = ld_small("b_ada_col_s", b_ada_col_d[:, :], [128, 48])
        b_in_col = ld_small("b_in_col_s", b_in_col_d[:, :], [128, 48])
        w_cvdw = ld_small("w_cvdw_s", w_cvdw_d[:, :, :], [128, 8, 31])
        b_cvdw = ld_small("b_cvdw_s", b_cvdw_d[:, :], [128, 8])
        ln_cv_g = ld_small("ln_cv_g_s", ln_cv_g_d[:, :], [128, 8])
        ln_cv_b = ld_small("ln_cv_b_s", ln_cv_b_d[:, :], [128, 8])
        w_lconv = ld_small("w_lconv_s", w_lconv_d[:, :, :], [128, 8, 4])
        b_lconv = ld_small("b_lconv_s", b_lconv_d[:, :], [128, 8])
        b_lru_a = ld_small("b_lru_a_s", b_lru_a_d[:, :], [128, 8])
        b_lru_x = ld_small("b_lru_x_s", b_lru_x_d[:, :], [128, 8])
        lam = ld_small("lam_s", lam_d[:, :], [128, 8])
        w_router = ld_small("w_router_s", w_router_d[:, :, :], [128, 8, NE])
        T.barrier()

        mod_col = sb("mod_col", [128, 48])
        sc1p = sb("sc1p", [128, 8])
        sc2p = sb("sc2p", [128, 8])
        clam = sb("clam", [128, 8])
        clam2 = sb("clam2", [128, 8])
        G = sb("G", [128, NT, NE])
        OH = sb("OH", [128, NT, 4, NE])
        GT = sb("GT", [128, NT, 4])
        SLOT = sb("SLOT", [128, NT * 4], I32)
        WIU = sb("WIU", [128, NE, 16], I32)
        WID = sb("WID", [128, NE, 8], I32)
        BIU = sb("BIU", [128, NE], I32)

        def dbg_out(view3):
            with Scope(A, T) as dd:
                for k in range(8):
                    dt_ = sb("dbgt", [128, S], stack=dd)
                    T.op('dve', lambda e, k=k, dt_=dt_: e.tensor_copy(out=dt_[:], in_=view3[:, k, :]),
                         writes=[('dbgt', k)])
                    T.dma('sp', dbg_d[:, S * k:S * (k + 1)], dt_[:], reads=[('dbgt', k)])
            T.finish()

        with Scope(A, T) as p0:
            sc = sb("sc", [128, 8], stack=p0)
            scb = sb("scb", [128, 8, 128], stack=p0)
            wa = [sb("wa%d" % i, [128, 8, 128], stack=p0) for i in range(3)]
            wr = [sb("wr%d" % i, [128, 8, 512], stack=p0) for i in range(2)]
            brow = sb("brow", [128, 512], stack=p0)
            bo_b = sb("bo_b", [128, D], stack=p0)
            tmp1 = sb("p0tmp", [128, 8], stack=p0)
            g1row = sb("g1row", [128, D], stack=p0)
            g2row = sb("g2row", [128, D], stack=p0)
            g1bo = sb("g1bo", [128, D], stack=p0)
            sh2row = sb("sh2row", [128, D], stack=p0)
            sc2prow = sb("sc2prow", [128, D], stack=p0)
            T.op('act', lambda e: e.activation(out=sc[:], in_=c_col[:], func=AF.Silu), writes=['sc'])
            for k in range(8):
                T.op('dve', lambda e, k=k: e.tensor_copy(out=scb[:, k, :],
                                                         in_=sc[:, k:k + 1].to_broadcast([128, 128])),
                     reads=['sc'], writes=[('scb', k)])
            T.op('act', lambda e: e.activation(out=tmp1[:], in_=lam[:], func=AF.Exp, scale=-1.0),
                 writes=['p0tmp'])
            T.op('act', lambda e: e.activation(out=tmp1[:], in_=tmp1[:], func=AF.Ln, bias=1.0, scale=1.0),
                 reads=['p0tmp'], writes=['p0tmp'])
            T.op('dve', lambda e: e.tensor_scalar(out=clam[:], in0=tmp1[:], scalar1=-8.0, scalar2=None,
                                                  op0=ALU.mult), reads=['p0tmp'], writes=['clam'])
            T.op('dve', lambda e: e.tensor_scalar(out=clam2[:], in0=tmp1[:], scalar1=-16.0, scalar2=None,
                                                  op0=ALU.mult), reads=['p0tmp'], writes=['clam2'])
            pm = getps()
            for j in range(48):
                slot = j % 3
                T.dma('sp', wa[slot][:, :, :],
                      w_ada_d[:, 128 * j:128 * (j + 1)].rearrange("(k p) n -> p k n", p=128),
                      writes=[('wa', slot)])
                for k in range(8):
                    T.op('pe', lambda e, k=k, j=j, slot=slot: e.matmul(
                        psb[pm][:, j:j + 1], lhsT=wa[slot][:, k, :], rhs=sc[:, k:k + 1],
                        start=(k == 0), stop=(k == 7)),
                        reads=[('wa', slot), 'sc'] if k == 0 else (),
                        writes=[('ps', pm)] if k == 0 else (), signal=(k == 7))
                T._record((T.own['pe'], T.cnt['pe']), [('wa', slot), 'sc'], [('ps', pm)])
            T.op('dve', lambda e: e.tensor_tensor(out=mod_col[:], in0=psb[pm][:, 0:48], in1=b_ada_col[:],
                                                  op=ALU.add), reads=[('ps', pm)], writes=['mod'])
            T.op('dve', lambda e: e.tensor_scalar(out=sc1p[:], in0=mod_col[:, 8:16], scalar1=1.0, scalar2=None,
                                                  op0=ALU.add), reads=['mod'], writes=['sc1p'])
            T.op('dve', lambda e: e.tensor_scalar(out=sc2p[:], in0=mod_col[:, 32:40], scalar1=1.0, scalar2=None,
                                                  op0=ALU.add), reads=['mod'], writes=['sc2p'])
            T.dma('sp', bo_b[:], b_o_d[0:1, :].partition_broadcast(128), writes=['bo_b'])
            i_r = 0
            for sec, dst in ((2, g1row), (5, g2row), (3, sh2row), (4, sc2prow)):
                for n in range(2):
                    off = sec * D + 512 * n
                    slot = i_r % 2
                    i_r += 1
                    T.dma('sp', wr[slot][:, :, :],
                          w_ada_d[:, off:off + 512].rearrange("(k p) n -> p k n", p=128),
                          writes=[('wr', slot)])
                    T.dma('sp', brow[:], b_ada_row_d[0:1, off:off + 512].partition_broadcast(128),
                          writes=['brow'])
                    pi = getps()
                    mmgroup(psb[pi][:, :], ('ps', pi),
                            [(scb[:, k, :], wr[slot][:, k, :]) for k in range(8)],
                            reads=[('wr', slot)] + [('scb', k) for k in range(8)])
                    T.op('dve', lambda e, pi=pi, dst=dst, n=n: e.tensor_tensor(
                        out=dst[:, 512 * n:512 * (n + 1)], in0=psb[pi][:, :], in1=brow[:], op=ALU.add),
                        reads=[('ps', pi), 'brow'], writes=[('grow', sec, n)])
            T.op('dve', lambda e: e.tensor_tensor(out=g1bo[:], in0=g1row[:], in1=bo_b[:], op=ALU.mult),
                 reads=[('grow', 2, 0), ('grow', 2, 1), 'bo_b'], writes=['g1bo'])
            T.dma('sp', g1row_d[:, :], g1row[:], reads=[('grow', 2, 0), ('grow', 2, 1)])
            T.dma('sp', g2row_d[:, :], g2row[:], reads=[('grow', 5, 0), ('grow', 5, 1)])
            T.dma('sp', g1bo_d[:, :], g1bo[:], reads=['g1bo'])
            T.op('dve', lambda e: e.tensor_scalar(out=sc2prow[:], in0=sc2prow[:], scalar1=1.0, scalar2=None, op0=ALU.add),
                 reads=[('grow', 4, 0), ('grow', 4, 1)], writes=[('grow', 4, 0), ('grow', 4, 1)])
            T.dma('sp', sh2row_d[:, :], sh2row[:], reads=[('grow', 3, 0), ('grow', 3, 1)])
            T.dma('sp', sc2prow_d[:, :], sc2prow[:], reads=[('grow', 4, 0), ('grow', 4, 1)])

        if debug == 'p0':
            with Scope(A, T) as dd:
                dt_ = sb("dbgt", [128, 3 * D + 128], stack=dd)
                T.op('dve', lambda e: e.memset(dt_[:], 0.0), writes=['dbgt'])
                T.op('dve', lambda e: e.tensor_copy(out=dt_[:, 0:48], in_=mod_col[:]), reads=['dbgt'], writes=['dbgt'])
                T.op('dve', lambda e: e.tensor_copy(out=dt_[:, 48:56], in_=clam[:]), reads=['dbgt'], writes=['dbgt'])
                T.dma('sp', dt_[:, 128:128 + D], g1row_d[:, :], reads=['dbgt'], writes=['dbgt'])
                T.dma('sp', dt_[:, 128 + D:128 + 2 * D], g2row_d[:, :], reads=['dbgt'], writes=['dbgt'])
                T.dma('sp', dt_[:, 128 + 2 * D:128 + 3 * D], g1bo_d[:, :], reads=['dbgt'], writes=['dbgt'])
                T.dma('sp', dbg_d[:, 0:3 * D + 128], dt_[:, :], reads=['dbgt'])
            T.finish()
            return nc

        def layernorm_rows(xt_ap, key_in, junk, junk_key, mvt, tagi):
            T.op('dve', lambda e: e.tensor_reduce(out=mvt[:, 0:1], in_=xt_ap[:, :], axis=AX.X, op=ALU.add),
                 reads=[key_in], writes=[('mv0', tagi)])
            T.op('act', lambda e: e.activation(out=junk[:, :], in_=xt_ap[:, :], func=AF.Square,
                                               accum_out=mvt[:, 1:2]),
                 reads=[key_in], writes=[junk_key, ('mv1', tagi)])
            T.op('dve', lambda e: e.tensor_scalar(out=mvt[:, 2:3], in0=mvt[:, 0:1], scalar1=1.0 / D, scalar2=None,
                                                  op0=ALU.mult),
                 reads=[('mv0', tagi)], writes=[('mv2', tagi)])
            T.op('dve', lambda e: e.tensor_tensor(out=mvt[:, 3:4], in0=mvt[:, 2:3], in1=mvt[:, 2:3], op=ALU.mult),
                 reads=[('mv2', tagi)], writes=[('mv3', tagi)])
            T.op('dve', lambda e: e.scalar_tensor_tensor(out=mvt[:, 3:4], in0=mvt[:, 1:2], scalar=1.0 / D,
                                                         in1=mvt[:, 3:4], op0=ALU.mult, op1=ALU.subtract),
                 reads=[('mv1', tagi), ('mv3', tagi)], writes=[('mv3', tagi)])
            T.op('act', lambda e: e.activation(out=mvt[:, 3:4], in_=mvt[:, 3:4], func=AF.Sqrt,
                                               bias=epsc[:, 0:1], scale=1.0),
                 reads=[('mv3', tagi)], writes=[('mv3', tagi)])
            T.op('dve', lambda e: e.reciprocal(out=mvt[:, 4:5], in_=mvt[:, 3:4]),
                 reads=[('mv3', tagi)], writes=[('rs', tagi)])
            T.op('dve', lambda e: e.tensor_scalar(out=mvt[:, 5:6], in0=mvt[:, 2:3], scalar1=mvt[:, 4:5],
                                                  scalar2=-1.0, op0=ALU.mult, op1=ALU.mult),
                 reads=[('mv2', tagi), ('rs', tagi)], writes=[('nm', tagi)])
            return mvt[:, 4:5], mvt[:, 5:6], [('rs', tagi), ('nm', tagi)]

        uT, uT_blk = alloc_long([128, 8, S], BF16)
        with Scope(A, T) as p1:
            xt = [sb("xt%d" % i, [128, D], stack=p1) for i in range(3)]
            xn = [sb("xn%d" % i, [128, D], stack=p1) for i in range(2)]
            stt = [sb("stt%d" % i, [128, 12], stack=p1) for i in range(2)]
            mvt = [sb("mvt%d" % i, [128, 8], stack=p1) for i in range(2)]
            for tt in range(NT if debug not in ('p1a', 'p1b') else 1):
                a = tt % 3
                b = tt % 2
                T.dma('sp', xt[a][:], x_d[128 * tt:128 * (tt + 1), :], writes=[('xt', a)])
                rs, nm, keys = layernorm_rows(xt[a], ('xt', a), xn[b], ('xn', b), mvt[b], b)
                T.op('act', lambda e, a=a, b=b, rs=rs, nm=nm: e.activation(
                    out=xn[b][:], in_=xt[a][:], func=AF.Identity, bias=nm, scale=rs),
                    reads=[('xt', a)] + keys, writes=[('xn', b)])
                if debug == 'p1a':
                    T.dma('sp', dbg_d[:, 0:D], xn[b][:], reads=[('xn', b)])
                    T.dma('sp', dbg_d[:, D:D + 8], mvt[b][:], reads=[('xn', b)])
                    break
                for half in range(2):
                    pi = getps()
                    transpose4(pi, xn[b], half * 4, ('xn', b))
                    for q in range(4):
                        k = half * 4 + q
                        dst = uT[:, k, 128 * tt:128 * (tt + 1)]
                        if half == 0:
                            T.op('dve', lambda e, pi=pi, q=q, k=k, dst=dst: e.tensor_scalar(
                                out=dst, in0=psb[pi][:, 128 * q:128 * (q + 1)], scalar1=sc1p[:, k:k + 1],
                                scalar2=mod_col[:, k:k + 1], op0=ALU.mult, op1=ALU.add),
                                reads=[('ps', pi)], writes=[('uT', k, tt)])
                        else:
                            T.op('act', lambda e, pi=pi, q=q, k=k, dst=dst: e.activation(
                                out=dst, in_=psb[pi][:, 128 * q:128 * (q + 1)], func=AF.Identity,
                                bias=mod_col[:, k:k + 1], scale=sc1p[:, k:k + 1]),
                                reads=[('ps', pi)], writes=[('uT', k, tt)])
        if debug == 'p1a':
            T.finish()
            return nc
        if debug in ('uT', 'p1b'):
            dbg_out(uT)
            return nc

        p2 = Scope(A, T)
        wst = [sb("wst%d" % i, [128, 8, 128], stack=p2) for i in range(3)]
        wpb = [sb("wpb%d" % i, [128, 8, 128], BF16, stack=p2) for i in range(4)]
        wcnt = [0, 0]

        def load_panel(src_ap, cast_eng=None):
            s1 = wcnt[0] % 3
            s2 = wcnt[1] % 4
            cast_eng = 'act' if wcnt[0] % 2 == 0 else 'dve'
            wcnt[0] += 1
            wcnt[1] += 1
            T.dma('sp', wst[s1][:, :, :], src_ap.rearrange("(k p) n -> p k n", p=128),
                  writes=[('wst', s1)])
            if cast_eng == 'act':
                T.op('act', lambda e: e.copy(out=wpb[s2][:, :, :], in_=wst[s1][:, :, :]),
                     reads=[('wst', s1)], writes=[('wpb', s2)])
            else:
                T.op(cast_eng, lambda e: e.tensor_copy(out=wpb[s2][:, :, :], in_=wst[s1][:, :, :]),
                     reads=[('wst', s1)], writes=[('wpb', s2)])
            return wpb[s2], ('wpb', s2)

        def inproj(panel_tile, pkey, q):
            pi = getps()
            mmgroup(psb[pi][:, :], ('ps', pi),
                    [(panel_tile[:, k, :], uT[:, k, 512 * q:512 * (q + 1)]) for k in range(8)],
                    reads=[pkey])
            return pi

        ya_act, ya_blk = alloc_long([128, 8, S], BF16)
        with Scope(A, T) as pb:
            yac = sb("yac", [128, 8, S], stack=pb)
            with Scope(A, T) as pb1:
                NG = 2
                glu = [sb("glu%d" % i, [128, 32 + S], BF16, stack=pb1) for i in range(NG)]
                dgs = [sb("dgs%d" % i, [128, 31, 128], BF16, stack=pb1) for i in range(NG)]
                tsig = [sb("tsig%d" % i, [128, 512], stack=pb1) for i in range(2)]
                for i in range(NG):
                    T.op('pool', lambda e, i=i: e.memset(glu[i][:, 0:30], 0.0), writes=[('glu', i, 'pad')])
                it = 0
                for c in range(8):
                    r2 = c % NG
                    pan_v, pk_v = load_panel(w_in_d[:, 128 * c:128 * (c + 1)])
                    pan_g, pk_g = load_panel(w_in_d[:, 1024 + 128 * c:1024 + 128 * (c + 1)])
                    for k in range(31):
                        if k % 2 == 0:
                            T.op('dve', lambda e, r2=r2, c=c, k=k: e.tensor_scalar(
                                out=dgs[r2][:, k, :], in0=identb[:, :], scalar1=w_cvdw[:, c, k:k + 1], scalar2=None,
                                op0=ALU.mult), writes=[('dgs', r2, k)])
                        else:
                            T.op('act', lambda e, r2=r2, c=c, k=k: e.activation(
                                out=dgs[r2][:, k, :], in_=identb[:, :], func=AF.Copy, scale=w_cvdw[:, c, k:k + 1]),
                                writes=[('dgs', r2, k)])
                    for q in range(NQ):
                        i2 = it % 2
                        it += 1
                        pv = inproj(pan_v, pk_v, q)
                        pg = inproj(pan_g, pk_g, q)
                        T.op('act', lambda e, i2=i2, pg=pg, c=c: e.activation(
                            out=tsig[i2][:], in_=psb[pg][:, :], func=AF.Sigmoid,
                            bias=b_in_col[:, 8 + c:9 + c], scale=1.0),
                            reads=[('ps', pg)], writes=[('tsig', i2)])
                        T.op('dve', lambda e, i2=i2, pv=pv, c=c, r2=r2, q=q: e.scalar_tensor_tensor(
                            out=glu[r2][:, 30 + 512 * q:30 + 512 * (q + 1)], in0=psb[pv][:, :],
                            scalar=b_in_col[:, c:c + 1], in1=tsig[i2][:], op0=ALU.add, op1=ALU.mult),
                            reads=[('ps', pv), ('tsig', i2)], writes=[('glu', r2, q)])
                    rk = [('glu', r2, q) for q in range(NQ)] + [('glu', r2, 'pad')] + [('dgs', r2, k) for k in range(31)]
                    for q in range(NQ):
                        pc_ = getps()
                        mmgroup(psb[pc_][:, :], ('ps', pc_),
                                [(dgs[r2][:, k, :], glu[r2][:, k + 512 * q:k + 512 * (q + 1)]) for k in range(31)],
                                reads=rk)
                        T.op('act', lambda e, pc_=pc_, c=c, q=q: e.activation(
                            out=yac[:, c, 512 * q:512 * (q + 1)], in_=psb[pc_][:, :], func=AF.Identity,
                            bias=b_cvdw[:, c:c + 1], scale=1.0),
                            reads=[('ps', pc_)], writes=[('yac', c, q)])
            with Scope(A, T) as pb2:
                tsq = [sb("tsq%d" % i, [128, 512], stack=pb2) for i in range(3)]
                mean = sb("cmean", [128, 512], stack=pb2)
                var = sb("cvar", [128, 512], stack=pb2)
                rstd = sb("crstd", [128, 512], stack=pb2)
                nmr = sb("cnmr", [128, 512], stack=pb2)
                tn = [sb("tn%d" % i, [128, 512], stack=pb2) for i in range(3)]
                isq = 0
                itn = 0
                for q in range(NQ):
                    sl = slice(512 * q, 512 * (q + 1))
                    p_s = getps()
                    mmgroup(psb[p_s][:, :], ('ps', p_s), [(ones[:, :], yac[:, c, sl]) for c in range(8)],
                            reads=[])
                    p_q = getps()
                    for c in range(8):
                        j = isq % 3
                        isq += 1
                        T.op('act', lambda e, j=j, c=c, sl=sl: e.activation(
                            out=tsq[j][:], in_=yac[:, c, sl], func=AF.Square),
                            reads=[], writes=[('tsq', j)])
                        T.op('pe', lambda e, j=j, c=c, p_q=p_q: e.matmul(
                            psb[p_q][:, :], lhsT=ones[:, :], rhs=tsq[j][:], start=(c == 0), stop=(c == 7)),
                            reads=[('tsq', j)], writes=[('ps', p_q)], signal=True)
                    T.op('act', lambda e, p_s=p_s: e.activation(
                        out=mean[:], in_=psb[p_s][:, :], func=AF.Identity, scale=1.0 / D),
                        reads=[('ps', p_s)], writes=['cmean'])
                    T.op('dve', lambda e: e.tensor_tensor(out=var[:], in0=mean[:], in1=mean[:], op=ALU.mult),
                         reads=['cmean'], writes=['cvar'])
                    T.op('dve', lambda e, p_q=p_q: e.scalar_tensor_tensor(
                        out=var[:], in0=psb[p_q][:, :], scalar=1.0 / D, in1=var[:],
                        op0=ALU.mult, op1=ALU.subtract),
                        reads=[('ps', p_q), 'cvar'], writes=['cvar'])
                    T.op('act', lambda e: e.activation(out=var[:], in_=var[:], func=AF.Sqrt,
                                                       bias=epsc[:, 0:1], scale=1.0),
                         reads=['cvar'], writes=['cvar'])
                    T.op('dve', lambda e: e.reciprocal(out=rstd[:], in_=var[:]),
                         reads=['cvar'], writes=['crstd'])
                    T.op('dve', lambda e: e.scalar_tensor_tensor(
                        out=nmr[:], in0=mean[:], scalar=-1.0, in1=rstd[:], op0=ALU.mult, op1=ALU.mult),
                        reads=['cmean', 'crstd'], writes=['cnmr'])
                    for c in range(8):
                        j = itn % 3
                        itn += 1
                        T.op('dve', lambda e, j=j, c=c, sl=sl: e.tensor_tensor(
                            out=tn[j][:], in0=yac[:, c, sl], in1=rstd[:], op=ALU.mult),
                            reads=['crstd'], writes=[('tn', j)])
                        T.op('dve', lambda e, j=j: e.tensor_tensor(
                            out=tn[j][:], in0=tn[j][:], in1=nmr[:], op=ALU.add),
                            reads=[('tn', j), 'cnmr'], writes=[('tn', j)])
                        T.op('act', lambda e, j=j, c=c, sl=sl: e.activation(
                            out=ya_act[:, c, sl], in_=tn[j][:], func=AF.Silu,
                            bias=ln_cv_b[:, c:c + 1], scale=ln_cv_g[:, c:c + 1]),
                            reads=[('tn', j)], writes=[('ya_act', c, q)])
        if debug == 'ya_act':
            dbg_out(ya_act)
            return nc

        yb_act, yb_blk = alloc_long([128, 8, S], BF16)
        with Scope(A, T) as pa:
            wla = sb("wla", [128, 8, 128], BF16, stack=pa)
            wlx = sb("wlx", [128, 8, 128], BF16, stack=pa)
            with Scope(A, T) as pa0:
                wla_s = sb("wla_s", [128, 8, 128], stack=pa0)
                wlx_s = sb("wlx_s", [128, 8, 128], stack=pa0)
                T.dma('sp', wla_s[:, :, :], w_lru_a_d[:, :, :], writes=['wla_s'])
                T.dma('sp', wlx_s[:, :, :], w_lru_x_d[:, :, :], writes=['wlx_s'])
                T.op('dve', lambda e: e.tensor_copy(out=wla[:, :, :], in_=wla_s[:, :, :]),
                     reads=['wla_s'], writes=['wla'])
                T.op('act', lambda e: e.copy(out=wlx[:, :, :], in_=wlx_s[:, :, :]),
                     reads=['wlx_s'], writes=['wlx'])
            GSQ = float(np.sqrt(0.044715))
            bgs = sb("bgs", [128, 8], stack=pa)
            T.op('dve', lambda e: e.tensor_scalar(out=bgs[:], in0=b_in_col[:, 24:32], scalar1=GSQ, scalar2=None,
                                                  op0=ALU.mult), writes=['bgs'])
            NB = 1
            ybp = [sb("ybp%d" % i, [128, 3 + S], stack=pa) for i in range(NB)]
            ybc = [sb("ybc%d" % i, [128, S], stack=pa) for i in range(NB)]
            ybcb = [sb("ybcb%d" % i, [128, S], BF16, stack=pa) for i in range(NB)]
            hh = [sb("hh%d" % i, [128, 512], stack=pa) for i in range(2)]
            NTMP = 2
            tga = [sb("tga%d" % i, [128, 512], stack=pa) for i in range(NTMP)]
            tgx = [sb("tgx%d" % i, [128, 512], stack=pa) for i in range(NTMP)]
            ta = [sb("ta%d" % i, [128, 512], stack=pa) for i in range(NTMP)]
            tm = [sb("tm%d" % i, [128, 512], stack=pa) for i in range(NTMP)]
            tu = [sb("tu%d" % i, [128, 512], stack=pa) for i in range(NTMP)]
            tz = [sb("tz%d" % i, [128, 512], stack=pa) for i in range(NTMP)]
            tp = [sb("tp%d" % i, [128, 512], stack=pa) for i in range(NTMP)]
            tsg = [sb("tsg%d" % i, [128, 512], stack=pa) for i in range(NTMP)]
            for i in range(NB):
                T.op('pool', lambda e, i=i: e.memset(ybp[i][:, 0:3], 0.0), writes=[('ybp', i, 'pad')])
            it = 0
            hcnt = 0
            for c in range(8):
                r2 = c % NB
                pan, pkey = load_panel(w_in_d[:, 2048 + 128 * c:2048 + 128 * (c + 1)])
                for q in range(NQ):
                    pi = inproj(pan, pkey, q)
                    T.op('act', lambda e, pi=pi, q=q, r2=r2, c=c: e.activation(
                        out=ybp[r2][:, 3 + 512 * q:3 + 512 * (q + 1)], in_=psb[pi][:, :],
                        func=AF.Identity, bias=b_in_col[:, 16 + c:17 + c], scale=1.0),
                        reads=[('ps', pi)], writes=[('ybp', r2, q)])
                rk = [('ybp', r2, q) for q in range(NQ)] + [('ybp', r2, 'pad')]
                T.op('dve', lambda e, r2=r2, c=c: e.tensor_scalar(
                    out=ybc[r2][:, :], in0=ybp[r2][:, 0:S], scalar1=w_lconv[:, c, 0:1],
                    scalar2=b_lconv[:, c:c + 1], op0=ALU.mult, op1=ALU.add),
                    reads=rk, writes=[('ybc', r2)])
                for kk in range(1, 4):
                    T.op('dve', lambda e, r2=r2, c=c, kk=kk: e.scalar_tensor_tensor(
                        out=ybc[r2][:, :], in0=ybp[r2][:, kk:kk + S], scalar=w_lconv[:, c, kk:kk + 1],
                        in1=ybc[r2][:, :], op0=ALU.mult, op1=ALU.add),
                        reads=rk + [('ybc', r2)], writes=[('ybc', r2)])
                T.op('act', lambda e, r2=r2: e.copy(out=ybcb[r2][:, :], in_=ybc[r2][:, :]),
                     reads=[('ybc', r2)], writes=[('ybcb', r2)])
                pan_g, pkey_g = load_panel(w_in_d[:, 3072 + 128 * c:3072 + 128 * (c + 1)])
                for q in range(NQ):
                    i3 = it % NTMP
                    it += 1
                    sl = slice(512 * q, 512 * (q + 1))
                    pa_i = getps()
                    mmgroup(psb[pa_i][:, :], ('ps', pa_i), [(wla[:, c, :], ybcb[r2][:, sl])],
                            reads=[('ybcb', r2), 'wla'])
                    px_i = getps()
                    mmgroup(psb[px_i][:, :], ('ps', px_i), [(wlx[:, c, :], ybcb[r2][:, sl])],
                            reads=[('ybcb', r2), 'wlx'])
                    T.op('act', lambda e, i3=i3, pa_i=pa_i, c=c: e.activation(
                        out=tga[i3][:], in_=psb[pa_i][:, :], func=AF.Sigmoid,
                        bias=b_lru_a[:, c:c + 1], scale=1.0),
                        reads=[('ps', pa_i)], writes=[('tga', i3)])
                    T.op('act', lambda e, i3=i3, px_i=px_i, c=c: e.activation(
                        out=tgx[i3][:], in_=psb[px_i][:, :], func=AF.Sigmoid,
                        bias=b_lru_x[:, c:c + 1], scale=1.0),
                        reads=[('ps', px_i)], writes=[('tgx', i3)])
                    T.op('act', lambda e, i3=i3, c=c: e.activation(
                        out=ta[i3][:], in_=tga[i3][:], func=AF.Exp, scale=clam[:, c:c + 1]),
                        reads=[('tga', i3)], writes=[('ta', i3)])
                    T.op('act', lambda e, i3=i3, c=c: e.activation(
                        out=tm[i3][:], in_=tga[i3][:], func=AF.Exp, scale=clam2[:, c:c + 1]),
                        reads=[('tga', i3)], writes=[('tm', i3)])
                    T.op('act', lambda e, i3=i3: e.activation(
                        out=tm[i3][:], in_=tm[i3][:], func=AF.Sqrt, bias=1.0, scale=-1.0),
                        reads=[('tm', i3)], writes=[('tm', i3)])
                    if q == 0:
                        T.op('dve', lambda e, i3=i3: e.memset(tm[i3][:, 0:1], 1.0),
                             reads=[('tm', i3)], writes=[('tm', i3)])
                    T.op('dve', lambda e, i3=i3, r2=r2, sl=sl: e.tensor_tensor(
                        out=tu[i3][:], in0=tgx[i3][:], in1=ybc[r2][:, sl], op=ALU.mult),
                        reads=[('tgx', i3), ('ybc', r2)], writes=[('tu', i3)])
                    T.op('dve', lambda e, i3=i3: e.tensor_tensor(
                        out=tu[i3][:], in0=tu[i3][:], in1=tm[i3][:], op=ALU.mult),
                        reads=[('tu', i3), ('tm', i3)], writes=[('tu', i3)])
                    hcur = hcnt % 2
                    hprev = (hcnt - 1) % 2
                    hcnt += 1
                    if q == 0:
                        T.op('dve', lambda e, i3=i3, hcur=hcur: e.tensor_tensor_scan(
                            out=hh[hcur][:], data0=ta[i3][:], data1=tu[i3][:], initial=0.0,
                            op0=ALU.mult, op1=ALU.add),
                            reads=[('ta', i3), ('tu', i3)], writes=[('hh', hcur)])
                    else:
                        T.op('dve', lambda e, i3=i3, hcur=hcur, hprev=hprev: e.tensor_tensor_scan(
                            out=hh[hcur][:], data0=ta[i3][:], data1=tu[i3][:],
                            initial=hh[hprev][:, 511:512], op0=ALU.mult, op1=ALU.add),
                            reads=[('ta', i3), ('tu', i3), ('hh', hprev)], writes=[('hh', hcur)])
                    pg_i = inproj(pan_g, pkey_g, q)
                    T.op('act', lambda e, i3=i3, pg_i=pg_i, c=c: e.activation(
                        out=tz[i3][:], in_=psb[pg_i][:, :], func=AF.Identity,
                        bias=b_in_col[:, 24 + c:25 + c], scale=1.0),
                        reads=[('ps', pg_i)], writes=[('tz', i3)])
                    T.op('act', lambda e, i3=i3, pg_i=pg_i, c=c: e.activation(
                        out=tp[i3][:], in_=psb[pg_i][:, :], func=AF.Square,
                        bias=bgs[:, c:c + 1], scale=GSQ),
                        reads=[('ps', pg_i)], writes=[('tp', i3)])
                    T.op('dve', lambda e, i3=i3: e.scalar_tensor_tensor(
                        out=tp[i3][:], in0=tp[i3][:], scalar=1.0, in1=tz[i3][:], op0=ALU.add, op1=ALU.mult),
                        reads=[('tp', i3), ('tz', i3)], writes=[('tp', i3)])
                    T.op('act', lambda e, i3=i3: e.activation(
                        out=tsg[i3][:], in_=tp[i3][:], func=AF.Sigmoid,
                        scale=float(2.0 * np.sqrt(2.0 / np.pi))),
                        reads=[('tp', i3)], writes=[('tsg', i3)])
                    T.op('dve', lambda e, i3=i3: e.tensor_tensor(
                        out=tsg[i3][:], in0=tsg[i3][:], in1=tz[i3][:], op=ALU.mult),
                        reads=[('tsg', i3), ('tz', i3)], writes=[('tsg', i3)])
                    T.op('dve', lambda e, i3=i3, hcur=hcur, c=c, sl=sl: e.tensor_tensor(
                        out=yb_act[:, c, sl], in0=hh[hcur][:], in1=tsg[i3][:], op=ALU.mult),
                        reads=[('hh', hcur), ('tsg', i3)], writes=[('yb_act', c, q)])
        if debug == 'yb_act':
            dbg_out(yb_act)
            return nc

        merged, merged_blk = alloc_long([128, 8, S], BF16)
        with Scope(A, T) as pc:
            tga_ = [sb("mga%d" % i, [128, 512], stack=pc) for i in range(2)]
            tgb_ = [sb("mgb%d" % i, [128, 512], stack=pc) for i in range(2)]
            tm1 = [sb("mm1%d" % i, [128, 512], stack=pc) for i in range(2)]
            tm2 = [sb("mm2%d" % i, [128, 512], stack=pc) for i in range(2)]
            it = 0
            for m in range(8):
                pan_a, pk_a = load_panel(w_cv_out_d[:, 128 * m:128 * (m + 1)])
                pan_b, pk_b = load_panel(w_lru_out_d[:, 128 * m:128 * (m + 1)], cast_eng='act')
                pan_ga, pk_ga = load_panel(w_in_d[:, 4096 + 128 * m:4096 + 128 * (m + 1)])
                pan_gb, pk_gb = load_panel(w_in_d[:, 5120 + 128 * m:5120 + 128 * (m + 1)], cast_eng='act')
                for q in range(NQ):
                    i2 = it % 2
                    it += 1
                    sl = slice(512 * q, 512 * (q + 1))
                    p1i = getps()
                    mmgroup(psb[p1i][:, :], ('ps', p1i),
                            [(pan_a[:, k, :], ya_act[:, k, sl]) for k in range(8)], reads=[pk_a])
                    p2i = getps()
                    mmgroup(psb[p2i][:, :], ('ps', p2i),
                            [(pan_b[:, k, :], yb_act[:, k, sl]) for k in range(8)], reads=[pk_b])
                    p3i = inproj(pan_ga, pk_ga, q)
                    p4i = inproj(pan_gb, pk_gb, q)
                    T.op('act', lambda e, i2=i2, p3i=p3i, m=m: e.activation(
                        out=tga_[i2][:], in_=psb[p3i][:, :], func=AF.Sigmoid,
                        bias=b_in_col[:, 32 + m:33 + m], scale=1.0),
                        reads=[('ps', p3i)], writes=[('mga', i2)])
                    T.op('act', lambda e, i2=i2, p4i=p4i, m=m: e.activation(
                        out=tgb_[i2][:], in_=psb[p4i][:, :], func=AF.Sigmoid,
                        bias=b_in_col[:, 40 + m:41 + m], scale=1.0),
                        reads=[('ps', p4i)], writes=[('mgb', i2)])
                    T.op('dve', lambda e, i2=i2, p1i=p1i: e.tensor_tensor(
                        out=tm1[i2][:], in0=psb[p1i][:, :], in1=tga_[i2][:], op=ALU.mult),
                        reads=[('ps', p1i), ('mga', i2)], writes=[('mm1', i2)])
                    T.op('dve', lambda e, i2=i2, p2i=p2i: e.tensor_tensor(
                        out=tm2[i2][:], in0=psb[p2i][:, :], in1=tgb_[i2][:], op=ALU.mult),
                        reads=[('ps', p2i), ('mgb', i2)], writes=[('mm2', i2)])
                    T.op('dve', lambda e, i2=i2, m=m, sl=sl: e.tensor_tensor(
                        out=merged[:, m, sl], in0=tm1[i2][:], in1=tm2[i2][:], op=ALU.add),
                        reads=[('mm1', i2), ('mm2', i2)], writes=[('merged', m, q)])
        p2.__exit__(None, None, None)
        T.barrier()
        for blk in (uT_blk, ya_blk, yb_blk):
            A.release(blk)
        A.commit()

        if debug == 'merged':
            dbg_out(merged)
            return nc

        with Scope(A, T) as pd:
            wob = sb("wob", [128, 8, D], BF16, stack=pd)
            with Scope(A, T) as pd0:
                wos = [sb("wos%d" % i, [128, D], stack=pd0) for i in range(2)]
                for k in range(8):
                    s1 = k % 2
                    T.dma('sp', wos[s1][:], w_o_d[128 * k:128 * (k + 1), :], writes=[('wos', s1)])
                    if k % 2 == 0:
                        T.op('dve', lambda e, k=k, s1=s1: e.tensor_copy(out=wob[:, k, :], in_=wos[s1][:]),
                             reads=[('wos', s1)], writes=[('wob', k)])
                    else:
                        T.op('act', lambda e, k=k, s1=s1: e.copy(out=wob[:, k, :], in_=wos[s1][:]),
                             reads=[('wos', s1)], writes=[('wob', k)])
            l1g = sb("l1g", [128, D], stack=pd)
            l1b = sb("l1b", [128, D], stack=pd)
            brt = sb("brt", [128, NE], stack=pd)
            g1row = sb("g1row2", [128, D], stack=pd)
            g1bo = sb("g1bo2", [128, D], stack=pd)
            T.dma('sp', l1g[:], ln1_g_d[0:1, :].partition_broadcast(128), writes=['l1g'])
            T.dma('sp', l1b[:], ln1_b_d[0:1, :].partition_broadcast(128), writes=['l1b'])
            T.dma('sp', brt[:], b_router_d[0:1, :].partition_broadcast(128), writes=['brt'])
            T.dma('sp', g1row[:], g1row_d[:, :], writes=['g1row'])
            T.dma('sp', g1bo[:], g1bo_d[:, :], writes=['g1bo'])
            sh2row = sb("sh2row2", [128, D], stack=pd)
            sc2prow = sb("sc2prow2", [128, D], stack=pd)
            T.dma('sp', sh2row[:], sh2row_d[:, :], writes=['sh2row'])
            T.dma('sp', sc2prow[:], sc2prow_d[:, :], writes=['sc2prow'])
            u2m = [sb("u2m%d" % i, [128, D], stack=pd) for i in range(2)]
            u2k = [sb("u2k%d" % i, [128, D], BF16, stack=pd) for i in range(2)]
            e4 = [sb("e4%d" % i, [128, 8], stack=pd) for i in range(2)]
            xt = [sb("xr%d" % i, [128, D], stack=pd) for i in range(2)]
            hp = [sb("hp%d" % i, [128, D], stack=pd) for i in range(2)]
            tq = [sb("tq%d" % i, [128, D], stack=pd) for i in range(2)]
            h1t = [sb("h1t%d" % i, [128, D], stack=pd) for i in range(2)]
            xn2 = [sb("xn2%d" % i, [128, D], stack=pd) for i in range(2)]
            u2f = [sb("u2f%d" % i, [128, 8, 128], stack=pd) for i in range(2)]
            stt = [sb("stt2%d" % i, [128, 12], stack=pd) for i in range(4)]
            mvt = [sb("mvt2%d" % i, [128, 8], stack=pd) for i in range(4)]
            lg = [sb("lg%d" % i, [128, NE], stack=pd) for i in range(2)]
            mx8 = [sb("mx8%d" % i, [128, 8], stack=pd) for i in range(2)]
            ex = [sb("ex%d" % i, [128, NE], stack=pd) for i in range(2)]
            msk = [sb("msk%d" % i, [128, NE], stack=pd) for i in range(2)]
            ssum = [sb("ssum%d" % i, [128, 2], stack=pd) for i in range(2)]
            for tt in range(NT):
                b = tt % 2
                ts_ = slice(128 * tt, 128 * (tt + 1))
                T.dma('sp', xt[b][:], x_d[ts_, :], writes=[('xr', b)])
                pm_ = []
                for n in range(2):
                    pi = getps()
                    mmgroup(psb[pi][:, :], ('ps', pi),
                            [(merged[:, k, ts_], wob[:, k, 512 * n:512 * (n + 1)]) for k in range(8)],
                            reads=[('wob', k) for k in range(8)])
                    pm_.append(pi)
                T.op('dve', lambda e, b=b: e.scalar_tensor_tensor(
                    out=hp[b][:], in0=xt[b][:], scalar=ALPHA, in1=g1bo[:], op0=ALU.mult, op1=ALU.add),
                    reads=[('xr', b), 'g1bo'], writes=[('hp', b)])
                for n in range(2):
                    T.op('dve', lambda e, b=b, n=n, pi=pm_[n]: e.tensor_tensor(
                        out=tq[b][:, 512 * n:512 * (n + 1)], in0=psb[pi][:, :],
                        in1=g1row[:, 512 * n:512 * (n + 1)], op=ALU.mult),
                        reads=[('ps', pm_[n]), 'g1row'], writes=[('tq', b, n)])
                T.op('dve', lambda e, b=b: e.tensor_tensor(out=hp[b][:], in0=hp[b][:], in1=tq[b][:], op=ALU.add),
                     reads=[('hp', b), ('tq', b, 0), ('tq', b, 1)], writes=[('hp', b)])
                rs, nm, keys = layernorm_rows(hp[b], ('hp', b), h1t[b], ('h1t', b), mvt[b], ('a', b))
                T.op('act', lambda e, b=b, rs=rs, nm=nm: e.activation(
                    out=h1t[b][:], in_=hp[b][:], func=AF.Identity, bias=nm, scale=rs),
                    reads=[('hp', b)] + keys, writes=[('h1t', b)])
                T.op('dve', lambda e, b=b: e.tensor_tensor(out=h1t[b][:], in0=h1t[b][:], in1=l1g[:], op=ALU.mult),
                     reads=[('h1t', b), 'l1g'], writes=[('h1t', b)])
                T.op('dve', lambda e, b=b: e.tensor_tensor(out=h1t[b][:], in0=h1t[b][:], in1=l1b[:], op=ALU.add),
                     reads=[('h1t', b), 'l1b'], writes=[('h1t', b)])
                T.dma('sp', h1_d[ts_, :], h1t[b][:], reads=[('h1t', b)])
                rs2, nm2, keys2 = layernorm_rows(h1t[b], ('h1t', b), xn2[b], ('xn2', b), mvt[2 + b], ('b', b))
                T.op('act', lambda e, b=b, rs2=rs2, nm2=nm2: e.activation(
                    out=xn2[b][:], in_=h1t[b][:], func=AF.Identity, bias=nm2, scale=rs2),
                    reads=[('h1t', b)] + keys2, writes=[('xn2', b)])
                for half in range(2):
                    pi = getps()
                    transpose4(pi, xn2[b], half * 4, ('xn2', b))
                    for q in range(4):
                        k = half * 4 + q
                        if half == 0:
                            T.op('dve', lambda e, pi=pi, q=q, k=k, b=b: e.tensor_scalar(
                                out=u2f[b][:, k, :], in0=psb[pi][:, 128 * q:128 * (q + 1)],
                                scalar1=sc2p[:, k:k + 1], scalar2=mod_col[:, 24 + k:25 + k],
                                op0=ALU.mult, op1=ALU.add),
                                reads=[('ps', pi)], writes=[('u2f', b, k)])
                        else:
                            T.op('act', lambda e, pi=pi, q=q, k=k, b=b: e.activation(
                                out=u2f[b][:, k, :], in_=psb[pi][:, 128 * q:128 * (q + 1)], func=AF.Identity,
                                bias=mod_col[:, 24 + k:25 + k], scale=sc2p[:, k:k + 1]),
                                reads=[('ps', pi)], writes=[('u2f', b, k)])
                T.op('dve', lambda e, b=b: e.tensor_tensor(out=u2m[b][:], in0=xn2[b][:], in1=sc2prow[:], op=ALU.mult),
                     reads=[('xn2', b), 'sc2prow'], writes=[('u2m', b)])
                T.op('dve', lambda e, b=b: e.tensor_tensor(out=u2k[b][:], in0=u2m[b][:], in1=sh2row[:], op=ALU.add),
                     reads=[('u2m', b), 'sh2row'], writes=[('u2k', b)])
                T.dma('sp', u2tok_d[ts_, :], u2k[b][:], reads=[('u2k', b)])
                pl = getps()
                mmgroup(psb[pl][:, 0:NE], ('ps', pl),
                        [(u2f[b][:, k, :], w_router[:, k, :]) for k in range(8)],
                        reads=[('u2f', b, k) for k in range(8)])
                T.op('dve', lambda e, b=b, pl=pl: e.tensor_tensor(
                    out=lg[b][:], in0=psb[pl][:, 0:NE], in1=brt[:], op=ALU.add),
                    reads=[('ps', pl), 'brt'], writes=[('lg', b)])
                T.op('dve', lambda e, b=b: e.max(out=mx8[b][:], in_=lg[b][:]),
                     reads=[('lg', b)], writes=[('mx8', b)])
                T.op('dve', lambda e, b=b: e.tensor_scalar(
                    out=msk[b][:], in0=lg[b][:], scalar1=mx8[b][:, 3:4], scalar2=None, op0=ALU.is_ge),
                    reads=[('lg', b), ('mx8', b)], writes=[('msk', b)])
                T.op('dve', lambda e, b=b: e.tensor_scalar(
                    out=ex[b][:], in0=lg[b][:], scalar1=mx8[b][:, 0:1], scalar2=None, op0=ALU.subtract),
                    reads=[('lg', b), ('mx8', b)], writes=[('ex', b)])
                T.op('act', lambda e, b=b: e.activation(out=ex[b][:], in_=ex[b][:], func=AF.Exp),
                     reads=[('ex', b)], writes=[('ex', b)])
                T.op('dve', lambda e, b=b: e.tensor_tensor(out=ex[b][:], in0=ex[b][:], in1=msk[b][:], op=ALU.mult),
                     reads=[('ex', b), ('msk', b)], writes=[('ex', b)])
                T.op('dve', lambda e, b=b: e.tensor_reduce(out=ssum[b][:, 0:1], in_=ex[b][:], axis=AX.X, op=ALU.add),
                     reads=[('ex', b)], writes=[('ssum', b)])
                T.op('dve', lambda e, b=b: e.reciprocal(out=ssum[b][:, 1:2], in_=ssum[b][:, 0:1]),
                     reads=[('ssum', b)], writes=[('ssum', b)])
                T.op('dve', lambda e, b=b, tt=tt: e.tensor_scalar(
                    out=G[:, tt, :], in0=ex[b][:], scalar1=ssum[b][:, 1:2], scalar2=None, op0=ALU.mult),
                    reads=[('ex', b), ('ssum', b)], writes=[('G', tt)])
                for j in range(4):
                    T.op('dve', lambda e, b=b, tt=tt, j=j: e.tensor_scalar(
                        out=OH[:, tt, j, :], in0=lg[b][:], scalar1=mx8[b][:, j:j + 1], scalar2=None,
                        op0=ALU.is_equal), reads=[('lg', b), ('mx8', b)], writes=[('OH', tt, j)])
                T.op('dve', lambda e, b=b: e.tensor_scalar(
                    out=e4[b][:, 6:7], in0=mx8[b][:, 0:1], scalar1=-1.0, scalar2=None, op0=ALU.mult),
                    reads=[('mx8', b)], writes=[('e4n', b)])
                T.op('act', lambda e, b=b: e.activation(out=e4[b][:, 0:4], in_=mx8[b][:, 0:4], func=AF.Exp,
                                                        bias=e4[b][:, 6:7], scale=1.0),
                     reads=[('mx8', b), ('e4n', b)], writes=[('e4', b)])
                T.op('dve', lambda e, b=b: e.tensor_reduce(out=e4[b][:, 4:5], in_=e4[b][:, 0:4], axis=AX.X, op=ALU.add),
                     reads=[('e4', b)], writes=[('e4s', b)])
                T.op('dve', lambda e, b=b: e.reciprocal(out=e4[b][:, 5:6], in_=e4[b][:, 4:5]),
                     reads=[('e4s', b)], writes=[('e4r', b)])
                T.op('dve', lambda e, b=b, tt=tt: e.tensor_scalar(
                    out=GT[:, tt, :], in0=e4[b][:, 0:4], scalar1=e4[b][:, 5:6], scalar2=float(1.0 / 1.702),
                    op0=ALU.mult, op1=ALU.mult), reads=[('e4', b), ('e4r', b)], writes=[('GT', tt)])
        free_long(merged_blk)

        if debug == 'h1':
            with Scope(A, T) as dd:
                for tt in range(NT):
                    dt_ = sb("dbgt", [128, D], stack=dd)
                    T.dma('sp', dt_[:], h1_d[128 * tt:128 * (tt + 1), :], writes=[('dbgt', tt)])
                    T.dma('sp', dbg_d[:, D * tt:D * (tt + 1)], dt_[:], reads=[('dbgt', tt)])
            T.finish()
            return nc
        if debug == 'G':
            with Scope(A, T) as dd:
                dt_ = sb("dbgt", [128, 8 * S], stack=dd)
                T.op('dve', lambda e: e.memset(dt_[:], 0.0), writes=['dbgt'])
                T.op('dve', lambda e: e.tensor_copy(out=dt_[:, 0:NT * NE], in_=G[:, :, :].rearrange("p t e -> p (t e)")),
                     reads=['dbgt'], writes=['dbgt'])
                T.op('dve', lambda e: e.tensor_copy(out=dt_[:, 1024:1024 + NT * 4], in_=GT[:, :, :].rearrange("p t j -> p (t j)")),
                     reads=['dbgt'], writes=['dbgt'])
                T.dma('sp', dbg_d[:, :], dt_[:, :], reads=['dbgt'])
            T.finish()
            return nc

        with Scope(A, T) as pr:
            MK = sb("MK", [128, NT, NE], stack=pr)
            T.op('dve', lambda e: e.tensor_tensor(out=MK[:, :, :], in0=OH[:, :, 0, :], in1=OH[:, :, 1, :], op=ALU.add),
                 writes=['MK'])
            T.op('dve', lambda e: e.tensor_tensor(out=MK[:, :, :], in0=MK[:, :, :], in1=OH[:, :, 2, :], op=ALU.add),
                 reads=['MK'], writes=['MK'])
            T.op('dve', lambda e: e.tensor_tensor(out=MK[:, :, :], in0=MK[:, :, :], in1=OH[:, :, 3, :], op=ALU.add),
                 reads=['MK'], writes=['MK'])
            erow = sb("erow", [128, NE], stack=pr)
            ecol = sb("ecol", [128, 1], stack=pr)
            iou = sb("iou", [128, 16], stack=pr)
            iod = sb("iod", [128, 8], stack=pr)
            r31row = sb("r31row", [128, NE], stack=pr)
            r31col = sb("r31col", [128, 1], stack=pr)
            T.op('pool', lambda e: e.iota(out=erow[:], pattern=[[1, NE]], base=0, channel_multiplier=0,
                                          allow_small_or_imprecise_dtypes=True), writes=['erow'])
            T.op('pool', lambda e: e.iota(out=ecol[:], pattern=[[0, 1]], base=0, channel_multiplier=1,
                                          allow_small_or_imprecise_dtypes=True), writes=['ecol'])
            T.op('pool', lambda e: e.iota(out=iou[:], pattern=[[256, 8], [1, 2]], base=0, channel_multiplier=2,
                                          allow_small_or_imprecise_dtypes=True), writes=['iou'])
            T.op('pool', lambda e: e.iota(out=iod[:], pattern=[[128, 8]], base=0, channel_multiplier=1,
                                          allow_small_or_imprecise_dtypes=True), writes=['iod'])
            T.op('dve', lambda e: e.tensor_scalar(out=r31row[:], in0=erow[:], scalar1=-1.0, scalar2=31.0,
                                                  op0=ALU.mult, op1=ALU.add), reads=['erow'], writes=['r31row'])
            T.op('dve', lambda e: e.tensor_scalar(out=r31col[:], in0=ecol[:], scalar1=-1.0, scalar2=31.0,
                                                  op0=ALU.mult, op1=ALU.add), reads=['ecol'], writes=['r31col'])
            pc_ = getps()
            mmgroup(psb[pc_][0:NE, 0:1], ('ps', pc_), [(MK[:, tt, :], ones[:, 0:1]) for tt in range(NT)], reads=['MK'])
            pw_ = getps()
            mmgroup(psb[pw_][0:NE, 0:NE], ('ps', pw_), [(ones[:, 0:NE], MK[:, tt, :]) for tt in range(NT)], reads=['MK'])
            keyc = sb("keyc", [128, 1], stack=pr)
            keyr = sb("keyr", [128, NE], stack=pr)
            gtm = sb("gtm", [128, NE], stack=pr)
            rankc = sb("rankc", [128, 1], stack=pr)
            Pm = sb("Pm", [128, NE], stack=pr)
            cumrow = sb("cumrow", [128, NE], stack=pr)
            basec = sb("basec", [128, 1], stack=pr)
            diagB = sb("diagB", [128, NE], stack=pr)
            ecolb = sb("ecolb", [128, 128], stack=pr)
            pib = sb("pib", [128, NE], stack=pr)
            Um = sb("Um", [128, 128], stack=pr)
            T.op('dve', lambda e: e.scalar_tensor_tensor(out=keyc[0:NE, :], in0=psb[pc_][0:NE, 0:1], scalar=32.0,
                                                         in1=r31col[0:NE, :], op0=ALU.mult, op1=ALU.add),
                 reads=[('ps', pc_), 'r31col'], writes=['keyc'])
            T.op('dve', lambda e: e.scalar_tensor_tensor(out=keyr[0:NE, :], in0=psb[pw_][0:NE, 0:NE], scalar=32.0,
                                                         in1=r31row[0:NE, :], op0=ALU.mult, op1=ALU.add),
                 reads=[('ps', pw_), 'r31row'], writes=['keyr'])
            T.op('dve', lambda e: e.tensor_scalar(out=gtm[0:NE, :], in0=keyr[0:NE, :], scalar1=keyc[0:NE, 0:1],
                                                  scalar2=None, op0=ALU.is_gt),
                 reads=['keyr', 'keyc'], writes=['gtm'])
            T.op('dve', lambda e: e.tensor_reduce(out=rankc[0:NE, :], in_=gtm[0:NE, :], axis=AX.X, op=ALU.add),
                 reads=['gtm'], writes=['rankc'])
            T.op('dve', lambda e: e.tensor_scalar(out=Pm[0:NE, :], in0=erow[0:NE, :], scalar1=rankc[0:NE, 0:1],
                                                  scalar2=None, op0=ALU.is_equal),
                 reads=['erow', 'rankc'], writes=['Pm'])
            for r in range(NE):
                T.op('dve', lambda e, r=r: e.memset(cumrow[:, r:r + 1], float(CUM[r])), writes=[('cumrow', r)])
            T.op('dve', lambda e: e.tensor_tensor(out=gtm[0:NE, :], in0=Pm[0:NE, :], in1=cumrow[0:NE, :], op=ALU.mult),
                 reads=['Pm', 'gtm'] + [('cumrow', r) for r in range(NE)], writes=['gtm'])
            T.op('dve', lambda e: e.tensor_reduce(out=basec[0:NE, :], in_=gtm[0:NE, :], axis=AX.X, op=ALU.add),
                 reads=['gtm'], writes=['basec'])
            T.op('dve', lambda e: e.tensor_scalar(out=diagB[0:NE, :], in0=ident[0:NE, 0:NE], scalar1=basec[0:NE, 0:1],
                                                  scalar2=None, op0=ALU.mult), reads=['basec'], writes=['diagB'])
            T.op('dve', lambda e: e.tensor_copy(out=ecolb[0:NE, :], in_=ecol[0:NE, 0:1].to_broadcast([NE, 128])),
                 reads=['ecol'], writes=['ecolb'])
            pp_ = getps()
            mmgroup(psb[pp_][:, 0:NE], ('ps', pp_), [(ecolb[0:NE, :], Pm[0:NE, :])], reads=['ecolb', 'Pm'])
            T.op('dve', lambda e: e.tensor_copy(out=pib[:], in_=psb[pp_][:, 0:NE]), reads=[('ps', pp_)], writes=['pib'])
            T.op('pool', lambda e: e.affine_select(out=Um[:], in_=ones[:], pattern=[[1, 128]], compare_op=ALU.is_gt,
                                                   fill=0.0, base=0, channel_multiplier=-1), writes=['Um'])
            slotf = sb("slotf", [128, NT * 4], stack=pr)
            tsl = [sb("tsl%d" % i, [128, NE], stack=pr) for i in range(2)]
            isl = 0
            for tt in range(NT):
                ps_ = getps()
                pairs = [(ones[:, :], MK[:, tp, :]) for tp in range(tt)] + [(Um[:, :], MK[:, tt, :])] + \
                        [(ones[0:NE, :], diagB[0:NE, :])]
                mmgroup(psb[ps_][:, 0:NE], ('ps', ps_), pairs, reads=['MK', 'Um', 'diagB'])
                for j in range(4):
                    i2 = isl % 2
                    isl += 1
                    T.op('dve', lambda e, i2=i2, tt=tt, j=j, ps_=ps_: e.tensor_tensor(
                        out=tsl[i2][:], in0=psb[ps_][:, 0:NE], in1=OH[:, tt, j, :], op=ALU.mult),
                        reads=[('ps', ps_)], writes=[('tsl', i2)])
                    T.op('dve', lambda e, i2=i2, tt=tt, j=j: e.tensor_reduce(
                        out=slotf[:, 4 * tt + j:4 * tt + j + 1], in_=tsl[i2][:], axis=AX.X, op=ALU.add),
                        reads=[('tsl', i2)], writes=[('slotf', tt, j)])
            T.op('dve', lambda e: e.tensor_copy(out=SLOT[:, :], in_=slotf[:, :]),
                 reads=[('slotf', tt, j) for tt in range(NT) for j in range(4)], writes=['SLOT'])
            wif = sb("wif", [128, NE, 16], stack=pr)
            wdf = sb("wdf", [128, NE, 8], stack=pr)
            bif = sb("bif", [128, NE], stack=pr)
            pib2 = sb("pib2", [128, NE], stack=pr)
            T.op('dve', lambda e: e.tensor_scalar(out=pib2[:], in0=pib[:], scalar1=2048.0, scalar2=None, op0=ALU.mult),
                 reads=['pib'], writes=['pib2'])
            for r in range(NE):
                T.op('dve', lambda e, r=r: e.tensor_scalar(out=wif[:, r, :], in0=iou[:], scalar1=pib2[:, r:r + 1],
                                                           scalar2=None, op0=ALU.add),
                     reads=['iou', 'pib2'], writes=[('wif', r)])
            T.op('dve', lambda e: e.tensor_copy(out=WIU[:, :, :], in_=wif[:, :, :]),
                 reads=[('wif', r) for r in range(NE)], writes=['WIU'])
            T.op('dve', lambda e: e.tensor_scalar(out=pib2[:], in0=pib[:], scalar1=1024.0, scalar2=None, op0=ALU.mult),
                 reads=['pib', 'pib2'] + [('wif', r) for r in range(NE)], writes=['pib2'])
            for r in range(NE):
                T.op('dve', lambda e, r=r: e.tensor_scalar(out=wdf[:, r, :], in0=iod[:], scalar1=pib2[:, r:r + 1],
                                                           scalar2=None, op0=ALU.add),
                     reads=['iod', 'pib2'], writes=[('wdf', r)])
            T.op('dve', lambda e: e.tensor_copy(out=WID[:, :, :], in_=wdf[:, :, :]),
                 reads=[('wdf', r) for r in range(NE)], writes=['WID'])
            T.op('dve', lambda e: e.scalar_tensor_tensor(out=bif[:], in0=pib[:], scalar=128.0,
                                                         in1=ecol[:, 0:1].to_broadcast([128, NE]),
                                                         op0=ALU.mult, op1=ALU.add),
                 reads=['pib', 'ecol'], writes=['bif'])
            T.op('dve', lambda e: e.tensor_copy(out=BIU[:, :], in_=bif[:, :]), reads=['bif'], writes=['BIU'])
            utile = [sb("utile%d" % i, [128, D], BF16, stack=pr) for i in range(2)]
            for tt in range(NT):
                b = tt % 2
                T.dma('sp', utile[b][:], u2tok_d[128 * tt:128 * (tt + 1), :], writes=[('utile', b)])
                for j in range(4):
                    T.dma('pool', xs_d[:, :], utile[b][:, :], reads=[('utile', b), 'SLOT'], writes=[('xs', tt, j)],
                          out_offset=bass.IndirectOffsetOnAxis(ap=SLOT[:, 4 * tt + j:4 * tt + j + 1], axis=0))
        if debug == 'route':
            with Scope(A, T) as dd:
                dt_ = sb("dbgt", [128, 2048], stack=dd)
                T.op('dve', lambda e: e.memset(dt_[:], 0.0), writes=['dbgt'])
                T.op('dve', lambda e: e.tensor_copy(out=dt_[:, 0:64], in_=SLOT[:, :]), reads=['dbgt'], writes=['dbgt'])
                T.op('dve', lambda e: e.tensor_copy(out=dt_[:, 64:64 + 512], in_=WIU[:, :, :].rearrange("p r j -> p (r j)")),
                     reads=['dbgt'], writes=['dbgt'])
                T.op('dve', lambda e: e.tensor_copy(out=dt_[:, 576:576 + 256], in_=WID[:, :, :].rearrange("p r j -> p (r j)")),
                     reads=['dbgt'], writes=['dbgt'])
                T.op('dve', lambda e: e.tensor_copy(out=dt_[:, 832:832 + 32], in_=BIU[:, :]), reads=['dbgt'], writes=['dbgt'])
                T.op('dve', lambda e: e.tensor_copy(out=dt_[:, 896:896 + 64], in_=GT[:, :, :].rearrange("p t j -> p (t j)")),
                     reads=['dbgt'], writes=['dbgt'])
                T.dma('sp', dbg_d[:, 0:2048], dt_[:, :], reads=['dbgt'])
            T.finish()
            return nc

        with Scope(A, T) as pe_:
            ugT = [sb("ugT%d" % i, [128, 8, 2048], BF16, stack=pe_) for i in range(2)]
            actT = sb("actT", [128, 8, 2048], BF16, stack=pe_)
            wugh = [sb("wugh%d" % i, [128, 8, 4, 128], BF16, stack=pe_) for i in range(2)]
            wulh = [sb("wulh%d" % i, [128, 8, 4, 128], BF16, stack=pe_) for i in range(2)]
            wdb = sb("wdb", [128, 8, D], BF16, stack=pe_)
            NSTG = 3
            stg = [sb("stg%d" % i, [128, D], stack=pe_) for i in range(NSTG)]
            xst = [sb("xst%d" % i, [128, D], BF16, stack=pe_) for i in range(2)]
            osb = [sb("osb%d" % i, [128, D], stack=pe_) for i in range(2)]
            bu = [sb("bu%d" % i, [128, 16], stack=pe_) for i in range(2)]
            b7 = [sb("b7%d" % i, [128, 8], stack=pe_) for i in range(2)]
            b1 = [sb("b1%d" % i, [128, 8], stack=pe_) for i in range(2)]
            NZ = 2
            zg = [sb("zg%d" % i, [128, 512], stack=pe_) for i in range(NZ)]
            sg = [sb("sg%d" % i, [128, 512], stack=pe_) for i in range(NZ)]
            zl = [sb("zl%d" % i, [128, 512], stack=pe_) for i in range(NZ)]
            cnts = {'stg': 0, 'xst': 0, 'osb': 0, 'z': 0, 'cast': 0}

            def next_stg():
                s = cnts['stg'] % NSTG
                cnts['stg'] += 1
                return s

            def cast_eng():
                cnts['cast'] += 1
                return 'act' if cnts['cast'] % 2 == 0 else 'dve'

            def load_half(r, h, hb):
                for k in range(8):
                    s = next_stg()
                    T.dma('pool', stg[s][:, :], w_up2[:, :], reads=['WIU'], writes=[('stg', s)],
                          in_offset=bass.IndirectOffsetOnAxis(ap=WIU[:, r, 2 * k + h:2 * k + h + 1], axis=0))
                    v = stg[s][:, :].rearrange("p (a b c) -> p a b c", a=4, b=128, c=2)
                    ce = cast_eng()
                    if ce == 'act':
                        T.op('act', lambda e, v=v, k=k: e.copy(out=wugh[hb][:, k, :, :], in_=v[:, :, :, 0]),
                             reads=[('stg', s)], writes=[('wugh', hb, k)])
                        T.op('dve', lambda e, v=v, k=k: e.tensor_copy(out=wulh[hb][:, k, :, :], in_=v[:, :, :, 1]),
                             reads=[('stg', s)], writes=[('wulh', hb, k)])
                    else:
                        T.op('dve', lambda e, v=v, k=k: e.tensor_copy(out=wugh[hb][:, k, :, :], in_=v[:, :, :, 0]),
                             reads=[('stg', s)], writes=[('wugh', hb, k)])
                        T.op('act', lambda e, v=v, k=k: e.copy(out=wulh[hb][:, k, :, :], in_=v[:, :, :, 1]),
                             reads=[('stg', s)], writes=[('wulh', hb, k)])

            def load_bias(r):
                rb = posof[r] % 2
                T.dma('pool', bu[rb][:, :], b_up_d[:, :], reads=['BIU'], writes=[('bu', rb)],
                      in_offset=bass.IndirectOffsetOnAxis(ap=BIU[:, r:r + 1], axis=0))
                T.op('dve', lambda e: e.tensor_scalar(out=b7[rb][:], in0=bu[rb][:, 0:16:2], scalar1=-1.0, scalar2=7.0,
                                                      op0=ALU.mult, op1=ALU.add), reads=[('bu', rb)], writes=[('b7', rb)])
                T.op('dve', lambda e: e.tensor_scalar(out=b1[rb][:], in0=bu[rb][:, 1:16:2], scalar1=1.0, scalar2=None,
                                                      op0=ALU.add), reads=[('bu', rb)], writes=[('b1', rb)])

            def load_down_chunk(r, k):
                s = next_stg()
                T.dma('pool', stg[s][:, :], w_down2[:, :], reads=['WID'], writes=[('stg', s)],
                      in_offset=bass.IndirectOffsetOnAxis(ap=WID[:, r, k:k + 1], axis=0))
                ce = cast_eng()
                if ce == 'act':
                    T.op('act', lambda e: e.copy(out=wdb[:, k, :], in_=stg[s][:]), reads=[('stg', s)], writes=[('wdb', k)])
                else:
                    T.op('dve', lambda e: e.tensor_copy(out=wdb[:, k, :], in_=stg[s][:]),
                         reads=[('stg', s)], writes=[('wdb', k)])

            def build_ug(r):
                ub = posof[r] % 2
                for blk in range(CR[r] // 128):
                    xb_ = cnts['xst'] % 2
                    cnts['xst'] += 1
                    row0 = CUM[r] + 128 * blk
                    T.dma('sp', xst[xb_][:], xs_d[row0:row0 + 128, :], writes=[('xst', xb_)])
                    pi = getps()
                    psv = psb[pi][:, :].bitcast(BF16)
                    for k in range(8):
                        T.op('pe', lambda e, k=k, psv=psv, xb_=xb_: e.transpose(
                            out=psv[:, 128 * k:128 * (k + 1)], in_=xst[xb_][:, 128 * k:128 * (k + 1)],
                            identity=identb[:]),
                            reads=[('xst', xb_)] if k == 0 else (), writes=[('ps', pi)] if k == 0 else (),
                            signal=(k == 7))
                    T._record((T.own['pe'], T.cnt['pe']), [('xst', xb_)], [('ps', pi)])
                    dstv = ugT[ub][:, :, 128 * blk:128 * (blk + 1)]
                    srcv = psv.rearrange("p (k s) -> p k s", k=8)
                    if blk % 2 == 0:
                        T.op('act', lambda e, dstv=dstv, srcv=srcv: e.copy(out=dstv, in_=srcv),
                             reads=[('ps', pi)], writes=[('ug', ub, blk)])
                    else:
                        T.op('dve', lambda e, dstv=dstv, srcv=srcv: e.tensor_copy(out=dstv, in_=srcv),
                             reads=[('ps', pi)], writes=[('ug', ub, blk)])

            order = []
            for i_ in range(NE // 2):
                order += [i_, NE - 1 - i_]
            posof = {r_: p_ for p_, r_ in enumerate(order)}
            halves = [(r, h) for r in order for h in range(2)]
            load_bias(order[0])
            load_half(order[0], 0, 0)
            build_ug(order[0])
            for ih, (r, h) in enumerate(halves):
                hb = ih % 2
                ub = posof[r] % 2
                rb = posof[r] % 2
                C = CR[r]
                if ih + 1 < len(halves):
                    r2_, h2_ = halves[ih + 1]
                    if h2_ == 0:
                        load_bias(r2_)
                    load_half(r2_, h2_, (ih + 1) % 2)
                tiles = [(o, min(512, C - o)) for o in range(0, C, 512)]
                for hp4 in range(4):
                    hp_ = 4 * h + hp4
                    for (o, n) in tiles:
                        i3 = cnts['z'] % NZ
                        cnts['z'] += 1
                        ugk = [('ug', ub, b_) for b_ in range(o // 128, (o + n) // 128)]
                        pg = getps()
                        mmgroup(psb[pg][:, 0:n], ('ps', pg),
                                [(wugh[hb][:, k, hp4, :], ugT[ub][:, k, o:o + n]) for k in range(8)],
                                reads=[('wugh', hb, k) for k in range(8)] + ugk)
                        pl = getps()
                        mmgroup(psb[pl][:, 0:n], ('ps', pl),
                                [(wulh[hb][:, k, hp4, :], ugT[ub][:, k, o:o + n]) for k in range(8)],
                                reads=[('wulh', hb, k) for k in range(8)] + ugk)
                        T.op('act', lambda e, i3=i3, pg=pg, rb=rb, hp_=hp_, n=n: e.activation(
                            out=zg[i3][:, 0:n], in_=psb[pg][:, 0:n], func=AF.Relu,
                            bias=b7[rb][:, hp_:hp_ + 1], scale=-1.0),
                            reads=[('ps', pg), ('b7', rb)], writes=[('zg', i3)])
                        T.op('act', lambda e, i3=i3, n=n: e.activation(
                            out=sg[i3][:, 0:n], in_=zg[i3][:, 0:n], func=AF.Silu, bias=float(7.0 * 1.702), scale=-1.702),
                            reads=[('zg', i3)], writes=[('sg', i3)])
                        T.op('dve', lambda e, i3=i3, pl=pl, rb=rb, hp_=hp_, n=n: e.tensor_scalar(
                            out=zl[i3][:, 0:n], in0=psb[pl][:, 0:n], scalar1=b1[rb][:, hp_:hp_ + 1], scalar2=8.0,
                            op0=ALU.add, op1=ALU.min), reads=[('ps', pl), ('b1', rb)], writes=[('zl', i3)])
                        T.op('dve', lambda e, i3=i3, hp_=hp_, o=o, n=n: e.scalar_tensor_tensor(
                            out=actT[:, hp_, o:o + n], in0=zl[i3][:, 0:n], scalar=-6.0, in1=sg[i3][:, 0:n],
                            op0=ALU.max, op1=ALU.mult),
                            reads=[('sg', i3), ('zl', i3)], writes=[('act', hp_, o // 512)])
                    load_down_chunk(r, hp_)
                if h == 0:
                    continue
                if posof[r] + 1 < NE:
                    build_ug(order[posof[r] + 1])
                for blk in range(C // 128):
                    ob = cnts['osb'] % 2
                    cnts['osb'] += 1
                    for n_ in range(2):
                        pi = getps()
                        mmgroup(psb[pi][:, :], ('ps', pi),
                                [(actT[:, k, 128 * blk:128 * (blk + 1)], wdb[:, k, 512 * n_:512 * (n_ + 1)])
                                 for k in range(8)],
                                reads=[('wdb', k) for k in range(8)] + [('act', k, blk // 4) for k in range(8)])
                        if n_ == 0:
                            T.op('act', lambda e, pi=pi, ob=ob: e.copy(out=osb[ob][:, 0:512], in_=psb[pi][:, :]),
                                 reads=[('ps', pi)], writes=[('osb', ob, 0)])
                        else:
                            T.op('dve', lambda e, pi=pi, ob=ob: e.tensor_copy(out=osb[ob][:, 512:1024], in_=psb[pi][:, :]),
                                 reads=[('ps', pi)], writes=[('osb', ob, 1)])
                    row0 = CUM[r] + 128 * blk
                    T.dma('sp', ys_d[row0:row0 + 128, :], osb[ob][:], reads=[('osb', ob, 0), ('osb', ob, 1)])

        yacc, yacc_blk = alloc_long([128, NT, D], F32)
        with Scope(A, T) as pi0:
            b_down = sb("b_down_s", [128, D], stack=pi0)
            gts = [sb("gts%d" % i, [128, 128], stack=pi0) for i in range(2)]
            ygt = [sb("ygt%d" % i, [128, D], stack=pi0) for i in range(3)]
            T.dma('sp', b_down[0:NE, :], b_down_d[:, :], writes=['b_down'])
            ig = 0
            for tt in range(NT):
                b = tt % 2
                pg_ = getps()
                T.op('pe', lambda e, pg_=pg_, tt=tt: e.transpose(
                    out=psb[pg_][0:NE, 0:128], in_=G[:, tt, :], identity=ident[:]),
                    reads=[], writes=[('ps', pg_)])
                T.op('act', lambda e, pg_=pg_, b=b: e.copy(out=gts[b][0:NE, :], in_=psb[pg_][0:NE, 0:128]),
                     reads=[('ps', pg_)], writes=[('gts', b)])
                for n in range(2):
                    pi = getps()
                    mmgroup(psb[pi][:, :], ('ps', pi), [(gts[b][0:NE, :], b_down[0:NE, 512 * n:512 * (n + 1)])],
                            reads=[('gts', b), 'b_down'])
                    T.op('act', lambda e, pi=pi, tt=tt, n=n: e.copy(
                        out=yacc[:, tt, 512 * n:512 * (n + 1)], in_=psb[pi][:, :]),
                        reads=[('ps', pi)], writes=[('y', tt, n)])
                for j in range(4):
                    g3 = ig % 3
                    ig += 1
                    T.dma('pool', ygt[g3][:, :], ys_d[:, :], writes=[('ygt', g3)],
                          in_offset=bass.IndirectOffsetOnAxis(ap=SLOT[:, 4 * tt + j:4 * tt + j + 1], axis=0))
                    T.op('dve', lambda e, g3=g3, tt=tt, j=j: e.scalar_tensor_tensor(
                        out=yacc[:, tt, :], in0=ygt[g3][:], scalar=GT[:, tt, j:j + 1], in1=yacc[:, tt, :],
                        op0=ALU.mult, op1=ALU.add),
                        reads=[('ygt', g3), ('y', tt, 0), ('y', tt, 1)], writes=[('y', tt, 0), ('y', tt, 1)])

        with Scope(A, T) as pf:
            l2g = sb("l2g", [128, D], stack=pf)
            l2b = sb("l2b", [128, D], stack=pf)
            g2row = sb("g2row2", [128, D], stack=pf)
            T.dma('sp', l2g[:], ln2_g_d[0:1, :].partition_broadcast(128), writes=['l2g'])
            T.dma('sp', l2b[:], ln2_b_d[0:1, :].partition_broadcast(128), writes=['l2b'])
            T.dma('sp', g2row[:], g2row_d[:, :], writes=['g2row'])
            h1r = [sb("h1r%d" % i, [128, D], stack=pf) for i in range(2)]
            fo = [sb("fo%d" % i, [128, D], stack=pf) for i in range(2)]
            stt = [sb("stt3%d" % i, [128, 12], stack=pf) for i in range(2)]
            mvt = [sb("mvt3%d" % i, [128, 8], stack=pf) for i in range(2)]
            for tt in range(NT):
                b = tt % 2
                ts_ = slice(128 * tt, 128 * (tt + 1))
                T.dma('sp', h1r[b][:], h1_d[ts_, :], writes=[('h1r', b)])
                T.op('dve', lambda e, tt=tt: e.tensor_tensor(out=yacc[:, tt, :], in0=yacc[:, tt, :], in1=g2row[:],
                                                             op=ALU.mult),
                     reads=[('y', tt, 0), ('y', tt, 1), 'g2row'], writes=[('y', tt, 0), ('y', tt, 1)])
                T.op('dve', lambda e, b=b, tt=tt: e.scalar_tensor_tensor(
                    out=fo[b][:], in0=h1r[b][:], scalar=ALPHA, in1=yacc[:, tt, :], op0=ALU.mult, op1=ALU.add),
                    reads=[('h1r', b), ('y', tt, 0), ('y', tt, 1)], writes=[('fo', b)])
                rs, nm, keys = layernorm_rows(fo[b], ('fo', b), h1r[b], ('h1r', b), mvt[b], ('c', b))
                T.op('act', lambda e, b=b, rs=rs, nm=nm: e.activation(
                    out=fo[b][:], in_=fo[b][:], func=AF.Identity, bias=nm, scale=rs),
                    reads=[('fo', b)] + keys, writes=[('fo', b)])
                T.op('dve', lambda e, b=b: e.tensor_tensor(out=fo[b][:], in0=fo[b][:], in1=l2g[:], op=ALU.mult),
                     reads=[('fo', b), 'l2g'], writes=[('fo', b)])
                T.op('dve', lambda e, b=b: e.tensor_tensor(out=fo[b][:], in0=fo[b][:], in1=l2b[:], op=ALU.add),
                     reads=[('fo', b), 'l2b'], writes=[('fo', b)])
                T.dma('sp', out_d[ts_, :], fo[b][:], reads=[('fo', b)])
        T.finish()
    return nc


def _col(v):
    return np.ascontiguousarray(np.asarray(v, np.float32).reshape(8, 128).T)


def make_in_maps(inputs, cores):
    f = lambda a: np.ascontiguousarray(np.asarray(a, np.float32))
    x = f(inputs['x'])
    c = f(inputs['c'])
    shared = {
        "w_ada": f(inputs['w_ada'][0]),
        "b_ada_col": np.ascontiguousarray(f(inputs['b_ada'][0]).reshape(48, 128).T),
        "b_ada_row": f(inputs['b_ada'][0]).reshape(1, 6 * D),
        "w_in": f(inputs['w_in'][0]),
        "b_in_col": np.ascontiguousarray(f(inputs['b_in'][0]).reshape(48, 128).T),
        "w_cv_dw_col": np.ascontiguousarray(f(inputs['w_cv_dw'][0]).T.reshape(8, 128, 31).transpose(1, 0, 2)),
        "b_cv_dw_col": _col(inputs['b_cv_dw'][0]),
        "ln_cv_g_col": _col(inputs['ln_cv_g'][0]),
        "ln_cv_b_col": _col(inputs['ln_cv_b'][0]),
        "w_cv_out": f(inputs['w_cv_out'][0]),
        "w_lru_conv_col": np.ascontiguousarray(f(inputs['w_lru_conv'][0]).T.reshape(8, 128, 4).transpose(1, 0, 2)),
        "b_lru_conv_col": _col(inputs['b_lru_conv'][0]),
        "w_lru_a_t": np.ascontiguousarray(f(inputs['w_lru_a'][0]).transpose(1, 0, 2)),
        "b_lru_a_col": _col(inputs['b_lru_a'][0]),
        "w_lru_x_t": np.ascontiguousarray(f(inputs['w_lru_x'][0]).transpose(1, 0, 2)),
        "b_lru_x_col": _col(inputs['b_lru_x'][0]),
        "lru_lambda_col": _col(inputs['lru_lambda'][0]),
        "w_lru_out": f(inputs['w_lru_out'][0]),
        "w_o": f(inputs['w_o'][0]),
        "b_o_row": f(inputs['b_o'][0]).reshape(1, D),
        "ln1_g_row": f(inputs['ln1_g'][0]).reshape(1, D),
        "ln1_b_row": f(inputs['ln1_b'][0]).reshape(1, D),
        "w_router_t": np.ascontiguousarray(f(inputs['w_router'][0]).reshape(8, 128, NE).transpose(1, 0, 2)),
        "b_router_row": f(inputs['b_router'][0]).reshape(1, NE),
        "w_up": f(inputs['w_up'][0]),
        "b_up_rows": np.ascontiguousarray(f(inputs['b_up'][0]).reshape(NE, 8, 128, 2).transpose(0, 2, 1, 3)).reshape(NE * 128, 16),
        "w_down": f(inputs['w_down'][0]),
        "b_down": f(inputs['b_down'][0]),
        "ln2_g_row": f(inputs['ln2_g'][0]).reshape(1, D),
        "ln2_b_row": f(inputs['ln2_b'][0]).reshape(1, D),
    }
    maps = []
    for b in cores:
        m = dict(shared)
        m["x"] = np.ascontiguousarray(x[b])
        m["c_col"] = _col(c[b])
        maps.append(m)
    return maps


def kernel(**inputs):
    nc = build()
    maps = make_in_maps(inputs, list(range(8)))
    res = run_bass_kernel_spmd(nc, maps, core_ids=list(range(8)))
    out = np.stack([np.asarray(r["out"], np.float32) for r in res.results], axis=0)
    return out
```

```python
import numpy as np
from contextlib import ExitStack
import concourse.bass as bass
import concourse.mybir as mybir
from concourse.bass_utils import run_bass_kernel_spmd

F32 = mybir.dt.float32
BF16 = mybir.dt.bfloat16
AF = mybir.ActivationFunctionType
ALU = mybir.AluOpType
AX = mybir.AxisListType

S = 2048
D = 1024
NE = 32
ALPHA = float(2.0 ** 0.25)
EPS = 1e-5
NT = S // 128
NQ = S // 512
SEM_ROLL = 30000
CR = [min(2048, -(-(8192 // (r + 1)) // 128) * 128) for r in range(NE)]
CUM = [0]
for _c in CR:
    CUM.append(CUM[-1] + _c)
NSLOT = CUM[-1]
I32 = mybir.dt.int32


class TR:
    def __init__(self, nc, st):
        self.nc = nc
        self.st = st
        self.E = {'pe': nc.tensor, 'act': nc.scalar, 'dve': nc.vector, 'pool': nc.gpsimd, 'sp': nc.sync}
        self.sems = []
        self.own = {}
        self.cnt = {}
        self.seen = {e: {} for e in self.E}
        self.lastw = {}
        self.readers = {}
        self.dslots = {}
        self.drr = {}
        self.ownset = {e: set() for e in self.E}

    def newsem(self, name):
        h = self.st.enter_context(self.nc.semaphore(name))
        self.sems.append(h)
        return len(self.sems) - 1

    def _own(self, e):
        if e not in self.own or self.cnt[e] >= SEM_ROLL:
            self.own[e] = self.newsem("c_%s_%d" % (e, len(self.sems)))
            self.ownset[e].add(self.own[e])
            self.cnt[e] = 0
        return self.own[e]

    def _wait(self, e, tok):
        if tok is None:
            return
        s, v = tok
        if self.seen[e].get(s, 0) >= v:
            return
        self.seen[e][s] = v
        self.E[e].wait_ge(self.sems[s], v)

    def _dep(self, e, tok):
        if tok is None:
            return
        if e == 'pe' and tok[0] in self.ownset['pe']:
            return
        self._wait(e, tok)

    def _deps(self, e, reads, writes):
        for k in reads:
            self._dep(e, self.lastw.get(k))
        for k in writes:
            self._dep(e, self.lastw.get(k))
            r = self.readers.get(k)
            if r:
                for s, v in r.items():
                    self._dep(e, (s, v))

    def _record(self, tok, reads, writes):
        s, v = tok
        for k in reads:
            r = self.readers.setdefault(k, {})
            if r.get(s, 0) < v:
                r[s] = v
        for k in writes:
            self.lastw[k] = tok
            self.readers[k] = {}

    @staticmethod
    def _excl(reads, writes):
        ps = [k for k in reads if isinstance(k, tuple) and k[0] == 'ps']
        if not ps:
            return reads, writes
        return [k for k in reads if not (isinstance(k, tuple) and k[0] == 'ps')], list(writes) + ps

    def op(self, e, fn, reads=(), writes=(), signal=True):
        reads, writes = self._excl(reads, writes)
        self._deps(e, reads, writes)
        s = self._own(e)
        inst = fn(self.E[e])
        if signal:
            self.cnt[e] += 1
            inst.then_inc(self.sems[s], 1)
            tok = (s, self.cnt[e])
        else:
            tok = (s, self.cnt[e] + 1)
        self._record(tok, reads, writes)
        return tok

    def dma(self, q, out, in_, reads=(), writes=(), nslots=12, out_offset=None, in_offset=None):
        self._deps(q, reads, writes)
        if q not in self.dslots:
            self.dslots[q] = [[self.newsem("d_%s_%d" % (q, i)), 0] for i in range(nslots)]
            self.drr[q] = 0
        slot = self.dslots[q][self.drr[q]]
        self.drr[q] = (self.drr[q] + 1) % len(self.dslots[q])
        if slot[1] > 0:
            self._wait(q, (slot[0], slot[1]))
        if out_offset is not None or in_offset is not None:
            inst = self.E[q].indirect_dma_start(out=out, out_offset=out_offset, in_=in_, in_offset=in_offset)
        else:
            inst = self.E[q].dma_start(out=out, in_=in_)
        slot[1] += 16
        inst.then_inc(self.sems[slot[0]], 16)
        tok = (slot[0], slot[1])
        self._record(tok, reads, writes)
        return tok

    def barrier(self):
        toks = []
        for e in self.own:
            if self.cnt[e] > 0:
                toks.append((self.own[e], self.cnt[e]))
        for q in self.dslots:
            for s, v in self.dslots[q]:
                if v > 0:
                    toks.append((s, v))
        for e in self.E:
            for t in toks:
                if e == 'pe' and t[0] == self.own.get(e):
                    continue
                self._wait(e, t)
        self.lastw = {}
        self.readers = {}

    def finish(self):
        for q in self.dslots:
            for s, v in self.dslots[q]:
                if v > 0:
                    self._wait('sp', (s, v))
        for e in self.own:
            if e != 'sp' and self.cnt[e] > 0:
                self._wait('sp', (self.own[e], self.cnt[e]))


class Arena:
    def __init__(self, R, nbytes):
        self.R = R
        self.free = [(0, nbytes)]
        self.pending = []

    def alloc(self, shape, dt):
        esz = 2 if dt == BF16 else 4
        n = esz
        for s in shape[1:]:
            n *= s
        n = (n + 63) // 64 * 64
        for i, (o, l) in enumerate(self.free):
            if l >= n:
                if l == n:
                    self.free.pop(i)
                else:
                    self.free[i] = (o + n, l - n)
                v = self.R[:, o // 4:(o + n) // 4]
                if dt == BF16:
                    v = v.bitcast(BF16)
                tot = 1
                for s in shape[1:]:
                    tot *= s
                v = v[:, 0:tot]
                if len(shape) == 3:
                    v = v.rearrange("p (a b) -> p a b", a=shape[1])
                elif len(shape) == 4:
                    v = v.rearrange("p (a b c) -> p a b c", a=shape[1], b=shape[2])
                return v, (o, n)
        raise RuntimeError("arena full: need %d, free=%s" % (n, self.free))

    def release(self, blk):
        self.pending.append(blk)

    def commit(self):
        fl = sorted(self.free + self.pending)
        self.pending = []
        out = []
        for o, l in fl:
            if out and out[-1][0] + out[-1][1] == o:
                out[-1] = (out[-1][0], out[-1][1] + l)
            else:
                out.append((o, l))
        self.free = out


class Scope:
    def __init__(self, A, T):
        self.A = A
        self.T = T
        self.blks = []

    def __enter__(self):
        return self

    def alloc(self, shape, dt):
        v, b = self.A.alloc(shape, dt)
        self.blks.append(b)
        return v

    def __exit__(self, *a):
        if a[0] is None:
            self.T.barrier()
            for b in self.blks:
                self.A.release(b)
            self.A.commit()
        return False


def build(debug=None):
    nc = bass.Bass("TRN2", target_bir_lowering=False)

    def din(name, shape, dt=F32):
        return nc.dram_tensor(name, list(shape), dt, kind="ExternalInput").ap()

    x_d = din("x", [S, D])
    c_col_d = din("c_col", [128, 8])
    w_ada_d = din("w_ada", [D, 6 * D])
    b_ada_col_d = din("b_ada_col", [128, 48])
    b_ada_row_d = din("b_ada_row", [1, 6 * D])
    w_in_d = din("w_in", [D, 6 * D])
    b_in_col_d = din("b_in_col", [128, 48])
    w_cvdw_d = din("w_cv_dw_col", [128, 8, 31])
    b_cvdw_d = din("b_cv_dw_col", [128, 8])
    ln_cv_g_d = din("ln_cv_g_col", [128, 8])
    ln_cv_b_d = din("ln_cv_b_col", [128, 8])
    w_cv_out_d = din("w_cv_out", [D, D])
    w_lconv_d = din("w_lru_conv_col", [128, 8, 4])
    b_lconv_d = din("b_lru_conv_col", [128, 8])
    w_lru_a_d = din("w_lru_a_t", [128, 8, 128])
    b_lru_a_d = din("b_lru_a_col", [128, 8])
    w_lru_x_d = din("w_lru_x_t", [128, 8, 128])
    b_lru_x_d = din("b_lru_x_col", [128, 8])
    lam_d = din("lru_lambda_col", [128, 8])
    w_lru_out_d = din("w_lru_out", [D, D])
    w_o_d = din("w_o", [D, D])
    b_o_d = din("b_o_row", [1, D])
    ln1_g_d = din("ln1_g_row", [1, D])
    ln1_b_d = din("ln1_b_row", [1, D])
    w_router_d = din("w_router_t", [128, 8, NE])
    b_router_d = din("b_router_row", [1, NE])
    w_up_d = din("w_up", [NE, D, 2 * D])
    b_up_d = din("b_up_rows", [NE * 128, 16])
    w_down_d = din("w_down", [NE, D, D])
    b_down_d = din("b_down", [NE, D])
    ln2_g_d = din("ln2_g_row", [1, D])
    ln2_b_d = din("ln2_b_row", [1, D])
    out_d = nc.dram_tensor("out", [S, D], F32, kind="ExternalOutput").ap()
    h1_d = nc.dram_tensor("h1_scratch", [S, D], F32, kind="Internal").ap()
    g1row_d = nc.dram_tensor("g1row_scratch", [128, D], F32, kind="Internal").ap()
    g1bo_d = nc.dram_tensor("g1bo_scratch", [128, D], F32, kind="Internal").ap()
    g2row_d = nc.dram_tensor("g2row_scratch", [128, D], F32, kind="Internal").ap()
    sh2row_d = nc.dram_tensor("sh2row_scratch", [128, D], F32, kind="Internal").ap()
    sc2prow_d = nc.dram_tensor("sc2prow_scratch", [128, D], F32, kind="Internal").ap()
    u2tok_d = nc.dram_tensor("u2tok_scratch", [S, D], BF16, kind="Internal").ap()
    xs_d = nc.dram_tensor("xs_scratch", [NSLOT, D], BF16, kind="Internal").ap()
    ys_d = nc.dram_tensor("ys_scratch", [NSLOT, D], F32, kind="Internal").ap()
    w_up2 = w_up_d.rearrange("e d (h n) -> (e d h) n", h=2)
    w_down2 = w_down_d.rearrange("e d n -> (e d) n")
    dbg_d = None
    if debug is not None:
        dbg_d = nc.dram_tensor("dbg", [128, 8 * S], F32, kind="ExternalOutput").ap()

    with ExitStack() as st:
        T = TR(nc, st)
        ARENA_BYTES = 194 * 1024
        Rt = st.enter_context(nc.sbuf_tensor("arena", [128, ARENA_BYTES // 4], F32))
        A = Arena(Rt, ARENA_BYTES)

        def sb(name, shape, dt=F32, stack=None):
            if stack is None:
                return st.enter_context(nc.sbuf_tensor(name, list(shape), dt))
            return stack.alloc(list(shape), dt)

        def alloc_long(shape, dt):
            return A.alloc(list(shape), dt)

        def free_long(blk):
            T.barrier()
            A.release(blk)
            A.commit()

        psb = [st.enter_context(nc.psum_tensor("ps%d" % i, [128, 512], F32)) for i in range(8)]
        ps_rr = [0]

        def getps():
            i = ps_rr[0]
            ps_rr[0] = (i + 1) % 8
            return i

        def mmgroup(ps_ap, pskey, pairs, reads=()):
            n = len(pairs)
            tok = None
            for i, (l, r) in enumerate(pairs):
                last = (i == n - 1)
                tok = T.op('pe', lambda e, l=l, r=r, i=i, last=last: e.matmul(
                    ps_ap, lhsT=l, rhs=r, start=(i == 0), stop=last),
                    reads=reads if i == 0 else (), writes=[pskey] if i == 0 else (), signal=last)
            if n > 1:
                T._record(tok, reads, [pskey])
            return tok

        def transpose4(pi, src, k0, rkey):
            for q in range(4):
                k = k0 + q
                T.op('pe', lambda e, q=q, k=k: e.transpose(
                    out=psb[pi][:, 128 * q:128 * (q + 1)], in_=src[:, 128 * k:128 * (k + 1)],
                    identity=ident[:]),
                    reads=[rkey] if q == 0 else (), writes=[('ps', pi)] if q == 0 else (),
                    signal=(q == 3))
            T._record((T.own['pe'], T.cnt['pe']), [rkey], [('ps', pi)])

        ident = sb("ident", [128, 128])
        ones = sb("ones", [128, 128])
        epsc = sb("epsc", [128, 1])
        T.op('pool', lambda e: e.memset(ones[:], 1.0), writes=['ones'])
        T.op('pool', lambda e: e.memset(epsc[:], EPS), writes=['epsc'])
        T.op('pool', lambda e: e.affine_select(out=ident[:], in_=ones[:], pattern=[[-1, 128]],
                                               compare_op=ALU.is_equal, fill=0.0, base=0,
                                               channel_multiplier=1), reads=['ones'], writes=['ident'])

        identb = sb("identb", [128, 128], BF16)
        T.op('pool', lambda e: e.tensor_copy(out=identb[:], in_=ident[:]), reads=['ident'], writes=['identb'])

        def ld_small(name, src, shape):
            t = sb(name, shape)
            T.dma('sp', t[:], src, writes=[name])
            return t

        c_col = ld_small("c_col_s", c_col_d[:, :], [128, 8])
        b_ada_col = ld_small("b_ada_col_s", b_ada_col_d[:, :], [128, 48])
        b_in_col = ld_small("b_in_col_s", b_in_col_d[:, :], [128, 48])
        w_cvdw = ld_small("w_cvdw_s", w_cvdw_d[:, :, :], [128, 8, 31])
        b_cvdw = ld_small("b_cvdw_s", b_cvdw_d[:, :], [128, 8])
        ln_cv_g = ld_small("ln_cv_g_s", ln_cv_g_d[:, :], [128, 8])
        ln_cv_b = ld_small("ln_cv_b_s", ln_cv_b_d[:, :], [128, 8])
        w_lconv = ld_small("w_lconv_s", w_lconv_d[:, :, :], [128, 8, 4])
        b_lconv = ld_small("b_lconv_s", b_lconv_d[:, :], [128, 8])
        b_lru_a = ld_small("b_lru_a_s", b_lru_a_d[:, :], [128, 8])
        b_lru_x = ld_small("b_lru_x_s", b_lru_x_d[:, :], [128, 8])
        lam = ld_small("lam_s", lam_d[:, :], [128, 8])
        w_router = ld_small("w_router_s", w_router_d[:, :, :], [128, 8, NE])
        T.barrier()

        mod_col = sb("mod_col", [128, 48])
        sc1p = sb("sc1p", [128, 8])
        sc2p = sb("sc2p", [128, 8])
        clam = sb("clam", [128, 8])
        clam2 = sb("clam2", [128, 8])
        G = sb("G", [128, NT, NE])
        GT = sb("GT", [128, NT, 4])
        SLOT = sb("SLOT", [128, NT * 4], I32)
        WIU = sb("WIU", [128, NE, 16], I32)
        WID = sb("WID", [128, NE, 8], I32)
        BIU = sb("BIU", [128, NE], I32)

        def dbg_out(view3):
            with Scope(A, T) as dd:
                for k in range(8):
                    dt_ = sb("dbgt", [128, S], stack=dd)
                    T.op('dve', lambda e, k=k, dt_=dt_: e.tensor_copy(out=dt_[:], in_=view3[:, k, :]),
                         writes=[('dbgt', k)])
                    T.dma('sp', dbg_d[:, S * k:S * (k + 1)], dt_[:], reads=[('dbgt', k)])
            T.finish()

        with Scope(A, T) as p0:
            sc = sb("sc", [128, 8], stack=p0)
            scb = sb("scb", [128, 8, 128], stack=p0)
            wa = [sb("wa%d" % i, [128, 8, 128], stack=p0) for i in range(3)]
            wr = [sb("wr%d" % i, [128, 8, 512], stack=p0) for i in range(2)]
            brow = sb("brow", [128, 512], stack=p0)
            bo_b = sb("bo_b", [128, D], stack=p0)
            tmp1 = sb("p0tmp", [128, 8], stack=p0)
            g1row = sb("g1row", [128, D], stack=p0)
            g2row = sb("g2row", [128, D], stack=p0)
            g1bo = sb("g1bo", [128, D], stack=p0)
            sh2row = sb("sh2row", [128, D], stack=p0)
            sc2prow = sb("sc2prow", [128, D], stack=p0)
            T.op('act', lambda e: e.activation(out=sc[:], in_=c_col[:], func=AF.Silu), writes=['sc'])
            for k in range(8):
                T.op('dve', lambda e, k=k: e.tensor_copy(out=scb[:, k, :],
                                                         in_=sc[:, k:k + 1].to_broadcast([128, 128])),
                     reads=['sc'], writes=[('scb', k)])
            T.op('act', lambda e: e.activation(out=tmp1[:], in_=lam[:], func=AF.Exp, scale=-1.0),
                 writes=['p0tmp'])
            T.op('act', lambda e: e.activation(out=tmp1[:], in_=tmp1[:], func=AF.Ln, bias=1.0, scale=1.0),
                 reads=['p0tmp'], writes=['p0tmp'])
            T.op('dve', lambda e: e.tensor_scalar(out=clam[:], in0=tmp1[:], scalar1=-8.0, scalar2=None,
                                                  op0=ALU.mult), reads=['p0tmp'], writes=['clam'])
            T.op('dve', lambda e: e.tensor_scalar(out=clam2[:], in0=tmp1[:], scalar1=-16.0, scalar2=None,
                                                  op0=ALU.mult), reads=['p0tmp'], writes=['clam2'])
            pm = getps()
            for j in range(48):
                slot = j % 3
                T.dma('sp', wa[slot][:, :, :],
                      w_ada_d[:, 128 * j:128 * (j + 1)].rearrange("(k p) n -> p k n", p=128),
                      writes=[('wa', slot)])
                for k in range(8):
                    T.op('pe', lambda e, k=k, j=j, slot=slot: e.matmul(
                        psb[pm][:, j:j + 1], lhsT=wa[slot][:, k, :], rhs=sc[:, k:k + 1],
                        start=(k == 0), stop=(k == 7)),
                        reads=[('wa', slot), 'sc'] if k == 0 else (),
                        writes=[('ps', pm)] if k == 0 else (), signal=(k == 7))
                T._record((T.own['pe'], T.cnt['pe']), [('wa', slot), 'sc'], [('ps', pm)])
            T.op('dve', lambda e: e.tensor_tensor(out=mod_col[:], in0=psb[pm][:, 0:48], in1=b_ada_col[:],
                                                  op=ALU.add), reads=[('ps', pm)], writes=['mod'])
            T.op('dve', lambda e: e.tensor_scalar(out=sc1p[:], in0=mod_col[:, 8:16], scalar1=1.0, scalar2=None,
                                                  op0=ALU.add), reads=['mod'], writes=['sc1p'])
            T.op('dve', lambda e: e.tensor_scalar(out=sc2p[:], in0=mod_col[:, 32:40], scalar1=1.0, scalar2=None,
                                                  op0=ALU.add), reads=['mod'], writes=['sc2p'])
            T.dma('sp', bo_b[:], b_o_d[0:1, :].partition_broadcast(128), writes=['bo_b'])
            i_r = 0
            for sec, dst in ((2, g1row), (5, g2row), (3, sh2row), (4, sc2prow)):
                for n in range(2):
                    off = sec * D + 512 * n
                    slot = i_r % 2
                    i_r += 1
                    T.dma('sp', wr[slot][:, :, :],
                          w_ada_d[:, off:off + 512].rearrange("(k p) n -> p k n", p=128),
                          writes=[('wr', slot)])
                    T.dma('sp', brow[:], b_ada_row_d[0:1, off:off + 512].partition_broadcast(128),
                          writes=['brow'])
                    pi = getps()
                    mmgroup(psb[pi][:, :], ('ps', pi),
                            [(scb[:, k, :], wr[slot][:, k, :]) for k in range(8)],
                            reads=[('wr', slot)] + [('scb', k) for k in range(8)])
                    T.op('dve', lambda e, pi=pi, dst=dst, n=n: e.tensor_tensor(
                        out=dst[:, 512 * n:512 * (n + 1)], in0=psb[pi][:, :], in1=brow[:], op=ALU.add),
                        reads=[('ps', pi), 'brow'], writes=[('grow', sec, n)])
            T.op('dve', lambda e: e.tensor_tensor(out=g1bo[:], in0=g1row[:], in1=bo_b[:], op=ALU.mult),
                 reads=[('grow', 2, 0), ('grow', 2, 1), 'bo_b'], writes=['g1bo'])
            T.dma('sp', g1row_d[:, :], g1row[:], reads=[('grow', 2, 0), ('grow', 2, 1)])
            T.dma('sp', g2row_d[:, :], g2row[:], reads=[('grow', 5, 0), ('grow', 5, 1)])
            T.dma('sp', g1bo_d[:, :], g1bo[:], reads=['g1bo'])
            T.op('dve', lambda e: e.tensor_scalar(out=sc2prow[:], in0=sc2prow[:], scalar1=1.0, scalar2=None, op0=ALU.add),
                 reads=[('grow', 4, 0), ('grow', 4, 1)], writes=[('grow', 4, 0), ('grow', 4, 1)])
            T.dma('sp', sh2row_d[:, :], sh2row[:], reads=[('grow', 3, 0), ('grow', 3, 1)])
            T.dma('sp', sc2prow_d[:, :], sc2prow[:], reads=[('grow', 4, 0), ('grow', 4, 1)])

        if debug == 'p0':
            with Scope(A, T) as dd:
                dt_ = sb("dbgt", [128, 3 * D + 128], stack=dd)
                T.op('dve', lambda e: e.memset(dt_[:], 0.0), writes=['dbgt'])
                T.op('dve', lambda e: e.tensor_copy(out=dt_[:, 0:48], in_=mod_col[:]), reads=['dbgt'], writes=['dbgt'])
                T.op('dve', lambda e: e.tensor_copy(out=dt_[:, 48:56], in_=clam[:]), reads=['dbgt'], writes=['dbgt'])
                T.dma('sp', dt_[:, 128:128 + D], g1row_d[:, :], reads=['dbgt'], writes=['dbgt'])
                T.dma('sp', dt_[:, 128 + D:128 + 2 * D], g2row_d[:, :], reads=['dbgt'], writes=['dbgt'])
                T.dma('sp', dt_[:, 128 + 2 * D:128 + 3 * D], g1bo_d[:, :], reads=['dbgt'], writes=['dbgt'])
                T.dma('sp', dbg_d[:, 0:3 * D + 128], dt_[:, :], reads=['dbgt'])
            T.finish()
            return nc

        def layernorm_rows(xt_ap, key_in, junk, junk_key, mvt, tagi):
            T.op('dve', lambda e: e.tensor_reduce(out=mvt[:, 0:1], in_=xt_ap[:, :], axis=AX.X, op=ALU.add),
                 reads=[key_in], writes=[('mv0', tagi)])
            T.op('act', lambda e: e.activation(out=junk[:, :], in_=xt_ap[:, :], func=AF.Square,
                                               accum_out=mvt[:, 1:2]),
                 reads=[key_in], writes=[junk_key, ('mv1', tagi)])
            T.op('dve', lambda e: e.tensor_scalar(out=mvt[:, 2:3], in0=mvt[:, 0:1], scalar1=1.0 / D, scalar2=None,
                                                  op0=ALU.mult),
                 reads=[('mv0', tagi)], writes=[('mv2', tagi)])
            T.op('dve', lambda e: e.tensor_tensor(out=mvt[:, 3:4], in0=mvt[:, 2:3], in1=mvt[:, 2:3], op=ALU.mult),
                 reads=[('mv2', tagi)], writes=[('mv3', tagi)])
            T.op('dve', lambda e: e.scalar_tensor_tensor(out=mvt[:, 3:4], in0=mvt[:, 1:2], scalar=1.0 / D,
                                                         in1=mvt[:, 3:4], op0=ALU.mult, op1=ALU.subtract),
                 reads=[('mv1', tagi), ('mv3', tagi)], writes=[('mv3', tagi)])
            T.op('act', lambda e: e.activation(out=mvt[:, 3:4], in_=mvt[:, 3:4], func=AF.Sqrt,
                                               bias=epsc[:, 0:1], scale=1.0),
                 reads=[('mv3', tagi)], writes=[('mv3', tagi)])
            T.op('dve', lambda e: e.reciprocal(out=mvt[:, 4:5], in_=mvt[:, 3:4]),
                 reads=[('mv3', tagi)], writes=[('rs', tagi)])
            T.op('dve', lambda e: e.tensor_scalar(out=mvt[:, 5:6], in0=mvt[:, 2:3], scalar1=mvt[:, 4:5],
                                                  scalar2=-1.0, op0=ALU.mult, op1=ALU.mult),
                 reads=[('mv2', tagi), ('rs', tagi)], writes=[('nm', tagi)])
            return mvt[:, 4:5], mvt[:, 5:6], [('rs', tagi), ('nm', tagi)]

        uT, uT_blk = alloc_long([128, 8, S], BF16)
        with Scope(A, T) as p1:
            xt = [sb("xt%d" % i, [128, D], stack=p1) for i in range(3)]
            xn = [sb("xn%d" % i, [128, D], stack=p1) for i in range(2)]
            stt = [sb("stt%d" % i, [128, 12], stack=p1) for i in range(2)]
            mvt = [sb("mvt%d" % i, [128, 8], stack=p1) for i in range(2)]
            for tt in range(NT if debug not in ('p1a', 'p1b') else 1):
                a = tt % 3
                b = tt % 2
                T.dma('sp', xt[a][:], x_d[128 * tt:128 * (tt + 1), :], writes=[('xt', a)])
                rs, nm, keys = layernorm_rows(xt[a], ('xt', a), xn[b], ('xn', b), mvt[b], b)
                T.op('act', lambda e, a=a, b=b, rs=rs, nm=nm: e.activation(
                    out=xn[b][:], in_=xt[a][:], func=AF.Identity, bias=nm, scale=rs),
                    reads=[('xt', a)] + keys, writes=[('xn', b)])
                if debug == 'p1a':
                    T.dma('sp', dbg_d[:, 0:D], xn[b][:], reads=[('xn', b)])
                    T.dma('sp', dbg_d[:, D:D + 8], mvt[b][:], reads=[('xn', b)])
                    break
                for half in range(2):
                    pi = getps()
                    transpose4(pi, xn[b], half * 4, ('xn', b))
                    for q in range(4):
                        k = half * 4 + q
                        dst = uT[:, k, 128 * tt:128 * (tt + 1)]
                        if half == 0:
                            T.op('dve', lambda e, pi=pi, q=q, k=k, dst=dst: e.tensor_scalar(
                                out=dst, in0=psb[pi][:, 128 * q:128 * (q + 1)], scalar1=sc1p[:, k:k + 1],
                                scalar2=mod_col[:, k:k + 1], op0=ALU.mult, op1=ALU.add),
                                reads=[('ps', pi)], writes=[('uT', k, tt)])
                        else:
                            T.op('act', lambda e, pi=pi, q=q, k=k, dst=dst: e.activation(
                                out=dst, in_=psb[pi][:, 128 * q:128 * (q + 1)], func=AF.Identity,
                                bias=mod_col[:, k:k + 1], scale=sc1p[:, k:k + 1]),
                                reads=[('ps', pi)], writes=[('uT', k, tt)])
        if debug == 'p1a':
            T.finish()
            return nc
        if debug in ('uT', 'p1b'):
            dbg_out(uT)
            return nc

        p2 = Scope(A, T)
        wst = [sb("wst%d" % i, [128, 8, 128], stack=p2) for i in range(3)]
        wpb = [sb("wpb%d" % i, [128, 8, 128], BF16, stack=p2) for i in range(4)]
        wcnt = [0, 0]

        def load_panel(src_ap, cast_eng=None):
            s1 = wcnt[0] % 3
            s2 = wcnt[1] % 4
            cast_eng = 'act' if wcnt[0] % 2 == 0 else 'dve'
            wcnt[0] += 1
            wcnt[1] += 1
            T.dma('sp', wst[s1][:, :, :], src_ap.rearrange("(k p) n -> p k n", p=128),
                  writes=[('wst', s1)])
            if cast_eng == 'act':
                T.op('act', lambda e: e.copy(out=wpb[s2][:, :, :], in_=wst[s1][:, :, :]),
                     reads=[('wst', s1)], writes=[('wpb', s2)])
            else:
                T.op(cast_eng, lambda e: e.tensor_copy(out=wpb[s2][:, :, :], in_=wst[s1][:, :, :]),
                     reads=[('wst', s1)], writes=[('wpb', s2)])
            return wpb[s2], ('wpb', s2)

        def inproj(panel_tile, pkey, q):
            pi = getps()
            mmgroup(psb[pi][:, :], ('ps', pi),
                    [(panel_tile[:, k, :], uT[:, k, 512 * q:512 * (q + 1)]) for k in range(8)],
                    reads=[pkey])
            return pi

        ya_act, ya_blk = alloc_long([128, 8, S], BF16)
        with Scope(A, T) as pb:
            yac = sb("yac", [128, 8, S], stack=pb)
            with Scope(A, T) as pb1:
                NG = 2
                glu = [sb("glu%d" % i, [128, 32 + S], BF16, stack=pb1) for i in range(NG)]
                dgs = [sb("dgs%d" % i, [128, 31, 128], BF16, stack=pb1) for i in range(NG)]
                tsig = [sb("tsig%d" % i, [128, 512], stack=pb1) for i in range(2)]
                for i in range(NG):
                    T.op('pool', lambda e, i=i: e.memset(glu[i][:, 0:30], 0.0), writes=[('glu', i, 'pad')])
                it = 0
                for c in range(8):
                    r2 = c % NG
                    pan_v, pk_v = load_panel(w_in_d[:, 128 * c:128 * (c + 1)])
                    pan_g, pk_g = load_panel(w_in_d[:, 1024 + 128 * c:1024 + 128 * (c + 1)])
                    for k in range(31):
                        if k % 2 == 0:
                            T.op('dve', lambda e, r2=r2, c=c, k=k: e.tensor_scalar(
                                out=dgs[r2][:, k, :], in0=identb[:, :], scalar1=w_cvdw[:, c, k:k + 1], scalar2=None,
                                op0=ALU.mult), writes=[('dgs', r2, k)])
                        else:
                            T.op('act', lambda e, r2=r2, c=c, k=k: e.activation(
                                out=dgs[r2][:, k, :], in_=identb[:, :], func=AF.Copy, scale=w_cvdw[:, c, k:k + 1]),
                                writes=[('dgs', r2, k)])
                    for q in range(NQ):
                        i2 = it % 2
                        it += 1
                        pv = inproj(pan_v, pk_v, q)
                        pg = inproj(pan_g, pk_g, q)
                        T.op('act', lambda e, i2=i2, pg=pg, c=c: e.activation(
                            out=tsig[i2][:], in_=psb[pg][:, :], func=AF.Sigmoid,
                            bias=b_in_col[:, 8 + c:9 + c], scale=1.0),
                            reads=[('ps', pg)], writes=[('tsig', i2)])
                        T.op('dve', lambda e, i2=i2, pv=pv, c=c, r2=r2, q=q: e.scalar_tensor_tensor(
                            out=glu[r2][:, 30 + 512 * q:30 + 512 * (q + 1)], in0=psb[pv][:, :],
                            scalar=b_in_col[:, c:c + 1], in1=tsig[i2][:], op0=ALU.add, op1=ALU.mult),
                            reads=[('ps', pv), ('tsig', i2)], writes=[('glu', r2, q)])
                    rk = [('glu', r2, q) for q in range(NQ)] + [('glu', r2, 'pad')] + [('dgs', r2, k) for k in range(31)]
                    for q in range(NQ):
                        pc_ = getps()
                        mmgroup(psb[pc_][:, :], ('ps', pc_),
                                [(dgs[r2][:, k, :], glu[r2][:, k + 512 * q:k + 512 * (q + 1)]) for k in range(31)],
                                reads=rk)
                        T.op('act', lambda e, pc_=pc_, c=c, q=q: e.activation(
                            out=yac[:, c, 512 * q:512 * (q + 1)], in_=psb[pc_][:, :], func=AF.Identity,
                            bias=b_cvdw[:, c:c + 1], scale=1.0),
                            reads=[('ps', pc_)], writes=[('yac', c, q)])
            with Scope(A, T) as pb2:
                tsq = [sb("tsq%d" % i, [128, 512], stack=pb2) for i in range(3)]
                mean = sb("cmean", [128, 512], stack=pb2)
                var = sb("cvar", [128, 512], stack=pb2)
                rstd = sb("crstd", [128, 512], stack=pb2)
                nmr = sb("cnmr", [128, 512], stack=pb2)
                tn = [sb("tn%d" % i, [128, 512], stack=pb2) for i in range(3)]
                isq = 0
                itn = 0
                for q in range(NQ):
                    sl = slice(512 * q, 512 * (q + 1))
                    p_s = getps()
                    mmgroup(psb[p_s][:, :], ('ps', p_s), [(ones[:, :], yac[:, c, sl]) for c in range(8)],
                            reads=[])
                    p_q = getps()
                    for c in range(8):
                        j = isq % 3
                        isq += 1
                        T.op('act', lambda e, j=j, c=c, sl=sl: e.activation(
                            out=tsq[j][:], in_=yac[:, c, sl], func=AF.Square),
                            reads=[], writes=[('tsq', j)])
                        T.op('pe', lambda e, j=j, c=c, p_q=p_q: e.matmul(
                            psb[p_q][:, :], lhsT=ones[:, :], rhs=tsq[j][:], start=(c == 0), stop=(c == 7)),
                            reads=[('tsq', j)], writes=[('ps', p_q)], signal=True)
                    T.op('act', lambda e, p_s=p_s: e.activation(
                        out=mean[:], in_=psb[p_s][:, :], func=AF.Identity, scale=1.0 / D),
                        reads=[('ps', p_s)], writes=['cmean'])
                    T.op('dve', lambda e: e.tensor_tensor(out=var[:], in0=mean[:], in1=mean[:], op=ALU.mult),
                         reads=['cmean'], writes=['cvar'])
                    T.op('dve', lambda e, p_q=p_q: e.scalar_tensor_tensor(
                        out=var[:], in0=psb[p_q][:, :], scalar=1.0 / D, in1=var[:],
                        op0=ALU.mult, op1=ALU.subtract),
                        reads=[('ps', p_q), 'cvar'], writes=['cvar'])
                    T.op('act', lambda e: e.activation(out=var[:], in_=var[:], func=AF.Sqrt,
                                                       bias=epsc[:, 0:1], scale=1.0),
                         reads=['cvar'], writes=['cvar'])
                    T.op('dve', lambda e: e.reciprocal(out=rstd[:], in_=var[:]),
                         reads=['cvar'], writes=['crstd'])
                    T.op('dve', lambda e: e.scalar_tensor_tensor(
                        out=nmr[:], in0=mean[:], scalar=-1.0, in1=rstd[:], op0=ALU.mult, op1=ALU.mult),
                        reads=['cmean', 'crstd'], writes=['cnmr'])
                    for c in range(8):
                        j = itn % 3
                        itn += 1
                        T.op('dve', lambda e, j=j, c=c, sl=sl: e.tensor_tensor(
                            out=tn[j][:], in0=yac[:, c, sl], in1=rstd[:], op=ALU.mult),
                            reads=['crstd'], writes=[('tn', j)])
                        T.op('dve', lambda e, j=j: e.tensor_tensor(
                            out=tn[j][:], in0=tn[j][:], in1=nmr[:], op=ALU.add),
                            reads=[('tn', j), 'cnmr'], writes=[('tn', j)])
                        T.op('act', lambda e, j=j, c=c, sl=sl: e.activation(
                            out=ya_act[:, c, sl], in_=tn[j][:], func=AF.Silu,
                            bias=ln_cv_b[:, c:c + 1], scale=ln_cv_g[:, c:c + 1]),
                            reads=[('tn', j)], writes=[('ya_act', c, q)])
        if debug == 'ya_act':
            dbg_out(ya_act)
            return nc

        yb_act, yb_blk = alloc_long([128, 8, S], BF16)
        with Scope(A, T) as pa:
            wla = sb("wla", [128, 8, 128], BF16, stack=pa)
            wlx = sb("wlx", [128, 8, 128], BF16, stack=pa)
            with Scope(A, T) as pa0:
                wla_s = sb("wla_s", [128, 8, 128], stack=pa0)
                wlx_s = sb("wlx_s", [128, 8, 128], stack=pa0)
                T.dma('sp', wla_s[:, :, :], w_lru_a_d[:, :, :], writes=['wla_s'])
                T.dma('sp', wlx_s[:, :, :], w_lru_x_d[:, :, :], writes=['wlx_s'])
                T.op('dve', lambda e: e.tensor_copy(out=wla[:, :, :], in_=wla_s[:, :, :]),
                     reads=['wla_s'], writes=['wla'])
                T.op('act', lambda e: e.copy(out=wlx[:, :, :], in_=wlx_s[:, :, :]),
                     reads=['wlx_s'], writes=['wlx'])
            GSQ = float(np.sqrt(0.044715))
            bgs = sb("bgs", [128, 8], stack=pa)
            T.op('dve', lambda e: e.tensor_scalar(out=bgs[:], in0=b_in_col[:, 24:32], scalar1=GSQ, scalar2=None,
                                                  op0=ALU.mult), writes=['bgs'])
            NB = 1
            ybp = [sb("ybp%d" % i, [128, 3 + S], stack=pa) for i in range(NB)]
            ybc = [sb("ybc%d" % i, [128, S], stack=pa) for i in range(NB)]
            ybcb = [sb("ybcb%d" % i, [128, S], BF16, stack=pa) for i in range(NB)]
            hh = [sb("hh%d" % i, [128, 512], stack=pa) for i in range(2)]
            NTMP = 2
            tga = [sb("tga%d" % i, [128, 512], stack=pa) for i in range(NTMP)]
            tgx = [sb("tgx%d" % i, [128, 512], stack=pa) for i in range(NTMP)]
            ta = [sb("ta%d" % i, [128, 512], stack=pa) for i in range(NTMP)]
            tm = [sb("tm%d" % i, [128, 512], stack=pa) for i in range(NTMP)]
            tu = [sb("tu%d" % i, [128, 512], stack=pa) for i in range(NTMP)]
            tz = [sb("tz%d" % i, [128, 512], stack=pa) for i in range(NTMP)]
            tp = [sb("tp%d" % i, [128, 512], stack=pa) for i in range(NTMP)]
            tsg = [sb("tsg%d" % i, [128, 512], stack=pa) for i in range(NTMP)]
            for i in range(NB):
                T.op('pool', lambda e, i=i: e.memset(ybp[i][:, 0:3], 0.0), writes=[('ybp', i, 'pad')])
            it = 0
            hcnt = 0
            for c in range(8):
                r2 = c % NB
                pan, pkey = load_panel(w_in_d[:, 2048 + 128 * c:2048 + 128 * (c + 1)])
                for q in range(NQ):
                    pi = inproj(pan, pkey, q)
                    T.op('act', lambda e, pi=pi, q=q, r2=r2, c=c: e.activation(
                        out=ybp[r2][:, 3 + 512 * q:3 + 512 * (q + 1)], in_=psb[pi][:, :],
                        func=AF.Identity, bias=b_in_col[:, 16 + c:17 + c], scale=1.0),
                        reads=[('ps', pi)], writes=[('ybp', r2, q)])
                rk = [('ybp', r2, q) for q in range(NQ)] + [('ybp', r2, 'pad')]
                T.op('dve', lambda e, r2=r2, c=c: e.tensor_scalar(
                    out=ybc[r2][:, :], in0=ybp[r2][:, 0:S], scalar1=w_lconv[:, c, 0:1],
                    scalar2=b_lconv[:, c:c + 1], op0=ALU.mult, op1=ALU.add),
                    reads=rk, writes=[('ybc', r2)])
                for kk in range(1, 4):
                    T.op('dve', lambda e, r2=r2, c=c, kk=kk: e.scalar_tensor_tensor(
                        out=ybc[r2][:, :], in0=ybp[r2][:, kk:kk + S], scalar=w_lconv[:, c, kk:kk + 1],
                        in1=ybc[r2][:, :], op0=ALU.mult, op1=ALU.add),
                        reads=rk + [('ybc', r2)], writes=[('ybc', r2)])
                T.op('act', lambda e, r2=r2: e.copy(out=ybcb[r2][:, :], in_=ybc[r2][:, :]),
                     reads=[('ybc', r2)], writes=[('ybcb', r2)])
                pan_g, pkey_g = load_panel(w_in_d[:, 3072 + 128 * c:3072 + 128 * (c + 1)])
                for q in range(NQ):
                    i3 = it % NTMP
                    it += 1
                    sl = slice(512 * q, 512 * (q + 1))
                    pa_i = getps()
                    mmgroup(psb[pa_i][:, :], ('ps', pa_i), [(wla[:, c, :], ybcb[r2][:, sl])],
                            reads=[('ybcb', r2), 'wla'])
                    px_i = getps()
                    mmgroup(psb[px_i][:, :], ('ps', px_i), [(wlx[:, c, :], ybcb[r2][:, sl])],
                            reads=[('ybcb', r2), 'wlx'])
                    T.op('act', lambda e, i3=i3, pa_i=pa_i, c=c: e.activation(
                        out=tga[i3][:], in_=psb[pa_i][:, :], func=AF.Sigmoid,
                        bias=b_lru_a[:, c:c + 1], scale=1.0),
                        reads=[('ps', pa_i)], writes=[('tga', i3)])
                    T.op('act', lambda e, i3=i3, px_i=px_i, c=c: e.activation(
                        out=tgx[i3][:], in_=psb[px_i][:, :], func=AF.Sigmoid,
                        bias=b_lru_x[:, c:c + 1], scale=1.0),
                        reads=[('ps', px_i)], writes=[('tgx', i3)])
                    T.op('act', lambda e, i3=i3, c=c: e.activation(
                        out=ta[i3][:], in_=tga[i3][:], func=AF.Exp, scale=clam[:, c:c + 1]),
                        reads=[('tga', i3)], writes=[('ta', i3)])
                    T.op('act', lambda e, i3=i3, c=c: e.activation(
                        out=tm[i3][:], in_=tga[i3][:], func=AF.Exp, scale=clam2[:, c:c + 1]),
                        reads=[('tga', i3)], writes=[('tm', i3)])
                    T.op('act', lambda e, i3=i3: e.activation(
                        out=tm[i3][:], in_=tm[i3][:], func=AF.Sqrt, bias=1.0, scale=-1.0),
                        reads=[('tm', i3)], writes=[('tm', i3)])
                    if q == 0:
                        T.op('dve', lambda e, i3=i3: e.memset(tm[i3][:, 0:1], 1.0),
                             reads=[('tm', i3)], writes=[('tm', i3)])
                    T.op('dve', lambda e, i3=i3, r2=r2, sl=sl: e.tensor_tensor(
                        out=tu[i3][:], in0=tgx[i3][:], in1=ybc[r2][:, sl], op=ALU.mult),
                        reads=[('tgx', i3), ('ybc', r2)], writes=[('tu', i3)])
                    T.op('dve', lambda e, i3=i3: e.tensor_tensor(
                        out=tu[i3][:], in0=tu[i3][:], in1=tm[i3][:], op=ALU.mult),
                        reads=[('tu', i3), ('tm', i3)], writes=[('tu', i3)])
                    hcur = hcnt % 2
                    hprev = (hcnt - 1) % 2
                    hcnt += 1
                    if q == 0:
                        T.op('dve', lambda e, i3=i3, hcur=hcur: e.tensor_tensor_scan(
                            out=hh[hcur][:], data0=ta[i3][:], data1=tu[i3][:], initial=0.0,
                            op0=ALU.mult, op1=ALU.add),
                            reads=[('ta', i3), ('tu', i3)], writes=[('hh', hcur)])
                    else:
                        T.op('dve', lambda e, i3=i3, hcur=hcur, hprev=hprev: e.tensor_tensor_scan(
                            out=hh[hcur][:], data0=ta[i3][:], data1=tu[i3][:],
                            initial=hh[hprev][:, 511:512], op0=ALU.mult, op1=ALU.add),
                            reads=[('ta', i3), ('tu', i3), ('hh', hprev)], writes=[('hh', hcur)])
                    pg_i = inproj(pan_g, pkey_g, q)
                    T.op('act', lambda e, i3=i3, pg_i=pg_i, c=c: e.activation(
                        out=tz[i3][:], in_=psb[pg_i][:, :], func=AF.Identity,
                        bias=b_in_col[:, 24 + c:25 + c], scale=1.0),
                        reads=[('ps', pg_i)], writes=[('tz', i3)])
                    T.op('act', lambda e, i3=i3, pg_i=pg_i, c=c: e.activation(
                        out=tp[i3][:], in_=psb[pg_i][:, :], func=AF.Square,
                        bias=bgs[:, c:c + 1], scale=GSQ),
                        reads=[('ps', pg_i)], writes=[('tp', i3)])
                    T.op('dve', lambda e, i3=i3: e.scalar_tensor_tensor(
                        out=tp[i3][:], in0=tp[i3][:], scalar=1.0, in1=tz[i3][:], op0=ALU.add, op1=ALU.mult),
                        reads=[('tp', i3), ('tz', i3)], writes=[('tp', i3)])
                    T.op('act', lambda e, i3=i3: e.activation(
                        out=tsg[i3][:], in_=tp[i3][:], func=AF.Sigmoid,
                        scale=float(2.0 * np.sqrt(2.0 / np.pi))),
                        reads=[('tp', i3)], writes=[('tsg', i3)])
                    T.op('dve', lambda e, i3=i3: e.tensor_tensor(
                        out=tsg[i3][:], in0=tsg[i3][:], in1=tz[i3][:], op=ALU.mult),
                        reads=[('tsg', i3), ('tz', i3)], writes=[('tsg', i3)])
                    T.op('dve', lambda e, i3=i3, hcur=hcur, c=c, sl=sl: e.tensor_tensor(
                        out=yb_act[:, c, sl], in0=hh[hcur][:], in1=tsg[i3][:], op=ALU.mult),
                        reads=[('hh', hcur), ('tsg', i3)], writes=[('yb_act', c, q)])
        if debug == 'yb_act':
            dbg_out(yb_act)
            return nc

        merged, merged_blk = alloc_long([128, 8, S], BF16)
        with Scope(A, T) as pc:
            tga_ = [sb("mga%d" % i, [128, 512], stack=pc) for i in range(2)]
            tgb_ = [sb("mgb%d" % i, [128, 512], stack=pc) for i in range(2)]
            tm1 = [sb("mm1%d" % i, [128, 512], stack=pc) for i in range(2)]
            tm2 = [sb("mm2%d" % i, [128, 512], stack=pc) for i in range(2)]
            it = 0
            for m in range(8):
                pan_a, pk_a = load_panel(w_cv_out_d[:, 128 * m:128 * (m + 1)])
                pan_b, pk_b = load_panel(w_lru_out_d[:, 128 * m:128 * (m + 1)], cast_eng='act')
                pan_ga, pk_ga = load_panel(w_in_d[:, 4096 + 128 * m:4096 + 128 * (m + 1)])
                pan_gb, pk_gb = load_panel(w_in_d[:, 5120 + 128 * m:5120 + 128 * (m + 1)], cast_eng='act')
                for q in range(NQ):
                    i2 = it % 2
                    it += 1
                    sl = slice(512 * q, 512 * (q + 1))
                    p1i = getps()
                    mmgroup(psb[p1i][:, :], ('ps', p1i),
                            [(pan_a[:, k, :], ya_act[:, k, sl]) for k in range(8)], reads=[pk_a])
                    p2i = getps()
                    mmgroup(psb[p2i][:, :], ('ps', p2i),
                            [(pan_b[:, k, :], yb_act[:, k, sl]) for k in range(8)], reads=[pk_b])
                    p3i = inproj(pan_ga, pk_ga, q)
                    p4i = inproj(pan_gb, pk_gb, q)
                    T.op('act', lambda e, i2=i2, p3i=p3i, m=m: e.activation(
                        out=tga_[i2][:], in_=psb[p3i][:, :], func=AF.Sigmoid,
                        bias=b_in_col[:, 32 + m:33 + m], scale=1.0),
                        reads=[('ps', p3i)], writes=[('mga', i2)])
                    T.op('act', lambda e, i2=i2, p4i=p4i, m=m: e.activation(
                        out=tgb_[i2][:], in_=psb[p4i][:, :], func=AF.Sigmoid,
                        bias=b_in_col[:, 40 + m:41 + m], scale=1.0),
                        reads=[('ps', p4i)], writes=[('mgb', i2)])
                    T.op('dve', lambda e, i2=i2, p1i=p1i: e.tensor_tensor(
                        out=tm1[i2][:], in0=psb[p1i][:, :], in1=tga_[i2][:], op=ALU.mult),
                        reads=[('ps', p1i), ('mga', i2)], writes=[('mm1', i2)])
                    T.op('dve', lambda e, i2=i2, p2i=p2i: e.tensor_tensor(
                        out=tm2[i2][:], in0=psb[p2i][:, :], in1=tgb_[i2][:], op=ALU.mult),
                        reads=[('ps', p2i), ('mgb', i2)], writes=[('mm2', i2)])
                    T.op('dve', lambda e, i2=i2, m=m, sl=sl: e.tensor_tensor(
                        out=merged[:, m, sl], in0=tm1[i2][:], in1=tm2[i2][:], op=ALU.add),
                        reads=[('mm1', i2), ('mm2', i2)], writes=[('merged', m, q)])
        p2.__exit__(None, None, None)
        T.barrier()
        for blk in (uT_blk, ya_blk, yb_blk):
            A.release(blk)
        A.commit()

        if debug == 'merged':
            dbg_out(merged)
            return nc

        OH, OH_blk = alloc_long([128, NT, 4, NE], F32)
        with Scope(A, T) as pd:
            wob = sb("wob", [128, 8, D], BF16, stack=pd)
            with Scope(A, T) as pd0:
                wos = [sb("wos%d" % i, [128, D], stack=pd0) for i in range(2)]
                for k in range(8):
                    s1 = k % 2
                    T.dma('sp', wos[s1][:], w_o_d[128 * k:128 * (k + 1), :], writes=[('wos', s1)])
                    if k % 2 == 0:
                        T.op('dve', lambda e, k=k, s1=s1: e.tensor_copy(out=wob[:, k, :], in_=wos[s1][:]),
                             reads=[('wos', s1)], writes=[('wob', k)])
                    else:
                        T.op('act', lambda e, k=k, s1=s1: e.copy(out=wob[:, k, :], in_=wos[s1][:]),
                             reads=[('wos', s1)], writes=[('wob', k)])
            l1g = sb("l1g", [128, D], stack=pd)
            l1b = sb("l1b", [128, D], stack=pd)
            brt = sb("brt", [128, NE], stack=pd)
            g1row = sb("g1row2", [128, D], stack=pd)
            g1bo = sb("g1bo2", [128, D], stack=pd)
            T.dma('sp', l1g[:], ln1_g_d[0:1, :].partition_broadcast(128), writes=['l1g'])
            T.dma('sp', l1b[:], ln1_b_d[0:1, :].partition_broadcast(128), writes=['l1b'])
            T.dma('sp', brt[:], b_router_d[0:1, :].partition_broadcast(128), writes=['brt'])
            T.dma('sp', g1row[:], g1row_d[:, :], writes=['g1row'])
            T.dma('sp', g1bo[:], g1bo_d[:, :], writes=['g1bo'])
            sh2row = sb("sh2row2", [128, D], stack=pd)
            sc2prow = sb("sc2prow2", [128, D], stack=pd)
            T.dma('sp', sh2row[:], sh2row_d[:, :], writes=['sh2row'])
            T.dma('sp', sc2prow[:], sc2prow_d[:, :], writes=['sc2prow'])
            u2m = [sb("u2m%d" % i, [128, D], stack=pd) for i in range(2)]
            u2k = [sb("u2k%d" % i, [128, D], BF16, stack=pd) for i in range(2)]
            e4 = [sb("e4%d" % i, [128, 8], stack=pd) for i in range(2)]
            xt = [sb("xr%d" % i, [128, D], stack=pd) for i in range(2)]
            hp = [sb("hp%d" % i, [128, D], stack=pd) for i in range(2)]
            tq = [sb("tq%d" % i, [128, D], stack=pd) for i in range(2)]
            h1t = [sb("h1t%d" % i, [128, D], stack=pd) for i in range(2)]
            xn2 = [sb("xn2%d" % i, [128, D], stack=pd) for i in range(2)]
            u2f = [sb("u2f%d" % i, [128, 8, 128], stack=pd) for i in range(2)]
            stt = [sb("stt2%d" % i, [128, 12], stack=pd) for i in range(4)]
            mvt = [sb("mvt2%d" % i, [128, 8], stack=pd) for i in range(4)]
            lg = [sb("lg%d" % i, [128, NE], stack=pd) for i in range(2)]
            mx8 = [sb("mx8%d" % i, [128, 8], stack=pd) for i in range(2)]
            ex = [sb("ex%d" % i, [128, NE], stack=pd) for i in range(2)]
            msk = [sb("msk%d" % i, [128, NE], stack=pd) for i in range(2)]
            ssum = [sb("ssum%d" % i, [128, 2], stack=pd) for i in range(2)]
            for tt in range(NT):
                b = tt % 2
                ts_ = slice(128 * tt, 128 * (tt + 1))
                T.dma('sp', xt[b][:], x_d[ts_, :], writes=[('xr', b)])
                pm_ = []
                for n in range(2):
                    pi = getps()
                    mmgroup(psb[pi][:, :], ('ps', pi),
                            [(merged[:, k, ts_], wob[:, k, 512 * n:512 * (n + 1)]) for k in range(8)],
                            reads=[('wob', k) for k in range(8)])
                    pm_.append(pi)
                T.op('dve', lambda e, b=b: e.scalar_tensor_tensor(
                    out=hp[b][:], in0=xt[b][:], scalar=ALPHA, in1=g1bo[:], op0=ALU.mult, op1=ALU.add),
                    reads=[('xr', b), 'g1bo'], writes=[('hp', b)])
                for n in range(2):
                    T.op('dve', lambda e, b=b, n=n, pi=pm_[n]: e.tensor_tensor(
                        out=tq[b][:, 512 * n:512 * (n + 1)], in0=psb[pi][:, :],
                        in1=g1row[:, 512 * n:512 * (n + 1)], op=ALU.mult),
                        reads=[('ps', pm_[n]), 'g1row'], writes=[('tq', b, n)])
                T.op('dve', lambda e, b=b: e.tensor_tensor(out=hp[b][:], in0=hp[b][:], in1=tq[b][:], op=ALU.add),
                     reads=[('hp', b), ('tq', b, 0), ('tq', b, 1)], writes=[('hp', b)])
                rs, nm, keys = layernorm_rows(hp[b], ('hp', b), h1t[b], ('h1t', b), mvt[b], ('a', b))
                T.op('act', lambda e, b=b, rs=rs, nm=nm: e.activation(
                    out=h1t[b][:], in_=hp[b][:], func=AF.Identity, bias=nm, scale=rs),
                    reads=[('hp', b)] + keys, writes=[('h1t', b)])
                T.op('dve', lambda e, b=b: e.tensor_tensor(out=h1t[b][:], in0=h1t[b][:], in1=l1g[:], op=ALU.mult),
                     reads=[('h1t', b), 'l1g'], writes=[('h1t', b)])
                T.op('dve', lambda e, b=b: e.tensor_tensor(out=h1t[b][:], in0=h1t[b][:], in1=l1b[:], op=ALU.add),
                     reads=[('h1t', b), 'l1b'], writes=[('h1t', b)])
                T.dma('sp', h1_d[ts_, :], h1t[b][:], reads=[('h1t', b)])
                rs2, nm2, keys2 = layernorm_rows(h1t[b], ('h1t', b), xn2[b], ('xn2', b), mvt[2 + b], ('b', b))
                T.op('act', lambda e, b=b, rs2=rs2, nm2=nm2: e.activation(
                    out=xn2[b][:], in_=h1t[b][:], func=AF.Identity, bias=nm2, scale=rs2),
                    reads=[('h1t', b)] + keys2, writes=[('xn2', b)])
                for half in range(2):
                    pi = getps()
                    transpose4(pi, xn2[b], half * 4, ('xn2', b))
                    for q in range(4):
                        k = half * 4 + q
                        if half == 0:
                            T.op('dve', lambda e, pi=pi, q=q, k=k, b=b: e.tensor_scalar(
                                out=u2f[b][:, k, :], in0=psb[pi][:, 128 * q:128 * (q + 1)],
                                scalar1=sc2p[:, k:k + 1], scalar2=mod_col[:, 24 + k:25 + k],
                                op0=ALU.mult, op1=ALU.add),
                                reads=[('ps', pi)], writes=[('u2f', b, k)])
                        else:
                            T.op('act', lambda e, pi=pi, q=q, k=k, b=b: e.activation(
                                out=u2f[b][:, k, :], in_=psb[pi][:, 128 * q:128 * (q + 1)], func=AF.Identity,
                                bias=mod_col[:, 24 + k:25 + k], scale=sc2p[:, k:k + 1]),
                                reads=[('ps', pi)], writes=[('u2f', b, k)])
                T.op('dve', lambda e, b=b: e.tensor_tensor(out=u2m[b][:], in0=xn2[b][:], in1=sc2prow[:], op=ALU.mult),
                     reads=[('xn2', b), 'sc2prow'], writes=[('u2m', b)])
                T.op('dve', lambda e, b=b: e.tensor_tensor(out=u2k[b][:], in0=u2m[b][:], in1=sh2row[:], op=ALU.add),
                     reads=[('u2m', b), 'sh2row'], writes=[('u2k', b)])
                T.dma('sp', u2tok_d[ts_, :], u2k[b][:], reads=[('u2k', b)])
                pl = getps()
                mmgroup(psb[pl][:, 0:NE], ('ps', pl),
                        [(u2f[b][:, k, :], w_router[:, k, :]) for k in range(8)],
                        reads=[('u2f', b, k) for k in range(8)])
                T.op('dve', lambda e, b=b, pl=pl: e.tensor_tensor(
                    out=lg[b][:], in0=psb[pl][:, 0:NE], in1=brt[:], op=ALU.add),
                    reads=[('ps', pl), 'brt'], writes=[('lg', b)])
                T.op('dve', lambda e, b=b: e.max(out=mx8[b][:], in_=lg[b][:]),
                     reads=[('lg', b)], writes=[('mx8', b)])
                T.op('dve', lambda e, b=b: e.tensor_scalar(
                    out=msk[b][:], in0=lg[b][:], scalar1=mx8[b][:, 3:4], scalar2=None, op0=ALU.is_ge),
                    reads=[('lg', b), ('mx8', b)], writes=[('msk', b)])
                T.op('dve', lambda e, b=b: e.tensor_scalar(
                    out=ex[b][:], in0=lg[b][:], scalar1=mx8[b][:, 0:1], scalar2=None, op0=ALU.subtract),
                    reads=[('lg', b), ('mx8', b)], writes=[('ex', b)])
                T.op('act', lambda e, b=b: e.activation(out=ex[b][:], in_=ex[b][:], func=AF.Exp),
                     reads=[('ex', b)], writes=[('ex', b)])
                T.op('dve', lambda e, b=b: e.tensor_tensor(out=ex[b][:], in0=ex[b][:], in1=msk[b][:], op=ALU.mult),
                     reads=[('ex', b), ('msk', b)], writes=[('ex', b)])
                T.op('dve', lambda e, b=b: e.tensor_reduce(out=ssum[b][:, 0:1], in_=ex[b][:], axis=AX.X, op=ALU.add),
                     reads=[('ex', b)], writes=[('ssum', b)])
                T.op('dve', lambda e, b=b: e.reciprocal(out=ssum[b][:, 1:2], in_=ssum[b][:, 0:1]),
                     reads=[('ssum', b)], writes=[('ssum', b)])
                T.op('dve', lambda e, b=b, tt=tt: e.tensor_scalar(
                    out=G[:, tt, :], in0=ex[b][:], scalar1=ssum[b][:, 1:2], scalar2=None, op0=ALU.mult),
                    reads=[('ex', b), ('ssum', b)], writes=[('G', tt)])
                for j in range(4):
                    T.op('dve', lambda e, b=b, tt=tt, j=j: e.tensor_scalar(
                        out=OH[:, tt, j, :], in0=lg[b][:], scalar1=mx8[b][:, j:j + 1], scalar2=None,
                        op0=ALU.is_equal), reads=[('lg', b), ('mx8', b)], writes=[('OH', tt, j)])
                T.op('dve', lambda e, b=b: e.tensor_scalar(
                    out=e4[b][:, 6:7], in0=mx8[b][:, 0:1], scalar1=-1.0, scalar2=None, op0=ALU.mult),
                    reads=[('mx8', b)], writes=[('e4n', b)])
                T.op('act', lambda e, b=b: e.activation(out=e4[b][:, 0:4], in_=mx8[b][:, 0:4], func=AF.Exp,
                                                        bias=e4[b][:, 6:7], scale=1.0),
                     reads=[('mx8', b), ('e4n', b)], writes=[('e4', b)])
                T.op('dve', lambda e, b=b: e.tensor_reduce(out=e4[b][:, 4:5], in_=e4[b][:, 0:4], axis=AX.X, op=ALU.add),
                     reads=[('e4', b)], writes=[('e4s', b)])
                T.op('dve', lambda e, b=b: e.reciprocal(out=e4[b][:, 5:6], in_=e4[b][:, 4:5]),
                     reads=[('e4s', b)], writes=[('e4r', b)])
                T.op('dve', lambda e, b=b, tt=tt: e.tensor_scalar(
                    out=GT[:, tt, :], in0=e4[b][:, 0:4], scalar1=e4[b][:, 5:6], scalar2=float(1.0 / 1.702),
                    op0=ALU.mult, op1=ALU.mult), reads=[('e4', b), ('e4r', b)], writes=[('GT', tt)])
        free_long(merged_blk)

        if debug == 'h1':
            with Scope(A, T) as dd:
                for tt in range(NT):
                    dt_ = sb("dbgt", [128, D], stack=dd)
                    T.dma('sp', dt_[:], h1_d[128 * tt:128 * (tt + 1), :], writes=[('dbgt', tt)])
                    T.dma('sp', dbg_d[:, D * tt:D * (tt + 1)], dt_[:], reads=[('dbgt', tt)])
            T.finish()
            return nc
        if debug == 'G':
            with Scope(A, T) as dd:
                dt_ = sb("dbgt", [128, 8 * S], stack=dd)
                T.op('dve', lambda e: e.memset(dt_[:], 0.0), writes=['dbgt'])
                T.op('dve', lambda e: e.tensor_copy(out=dt_[:, 0:NT * NE], in_=G[:, :, :].rearrange("p t e -> p (t e)")),
                     reads=['dbgt'], writes=['dbgt'])
                T.op('dve', lambda e: e.tensor_copy(out=dt_[:, 1024:1024 + NT * 4], in_=GT[:, :, :].rearrange("p t j -> p (t j)")),
                     reads=['dbgt'], writes=['dbgt'])
                T.dma('sp', dbg_d[:, :], dt_[:, :], reads=['dbgt'])
            T.finish()
            return nc

        with Scope(A, T) as pr:
            MK = sb("MK", [128, NT, NE], stack=pr)
            T.op('dve', lambda e: e.tensor_tensor(out=MK[:, :, :], in0=OH[:, :, 0, :], in1=OH[:, :, 1, :], op=ALU.add),
                 writes=['MK'])
            T.op('dve', lambda e: e.tensor_tensor(out=MK[:, :, :], in0=MK[:, :, :], in1=OH[:, :, 2, :], op=ALU.add),
                 reads=['MK'], writes=['MK'])
            T.op('dve', lambda e: e.tensor_tensor(out=MK[:, :, :], in0=MK[:, :, :], in1=OH[:, :, 3, :], op=ALU.add),
                 reads=['MK'], writes=['MK'])
            erow = sb("erow", [128, NE], stack=pr)
            ecol = sb("ecol", [128, 1], stack=pr)
            iou = sb("iou", [128, 16], stack=pr)
            iod = sb("iod", [128, 8], stack=pr)
            r31row = sb("r31row", [128, NE], stack=pr)
            r31col = sb("r31col", [128, 1], stack=pr)
            T.op('pool', lambda e: e.iota(out=erow[:], pattern=[[1, NE]], base=0, channel_multiplier=0,
                                          allow_small_or_imprecise_dtypes=True), writes=['erow'])
            T.op('pool', lambda e: e.iota(out=ecol[:], pattern=[[0, 1]], base=0, channel_multiplier=1,
                                          allow_small_or_imprecise_dtypes=True), writes=['ecol'])
            T.op('pool', lambda e: e.iota(out=iou[:], pattern=[[256, 8], [1, 2]], base=0, channel_multiplier=2,
                                          allow_small_or_imprecise_dtypes=True), writes=['iou'])
            T.op('pool', lambda e: e.iota(out=iod[:], pattern=[[128, 8]], base=0, channel_multiplier=1,
                                          allow_small_or_imprecise_dtypes=True), writes=['iod'])
            T.op('dve', lambda e: e.tensor_scalar(out=r31row[:], in0=erow[:], scalar1=-1.0, scalar2=31.0,
                                                  op0=ALU.mult, op1=ALU.add), reads=['erow'], writes=['r31row'])
            T.op('dve', lambda e: e.tensor_scalar(out=r31col[:], in0=ecol[:], scalar1=-1.0, scalar2=31.0,
                                                  op0=ALU.mult, op1=ALU.add), reads=['ecol'], writes=['r31col'])
            pc_ = getps()
            mmgroup(psb[pc_][0:NE, 0:1], ('ps', pc_), [(MK[:, tt, :], ones[:, 0:1]) for tt in range(NT)], reads=['MK'])
            pw_ = getps()
            mmgroup(psb[pw_][0:NE, 0:NE], ('ps', pw_), [(ones[:, 0:NE], MK[:, tt, :]) for tt in range(NT)], reads=['MK'])
            keyc = sb("keyc", [128, 1], stack=pr)
            keyr = sb("keyr", [128, NE], stack=pr)
            gtm = sb("gtm", [128, NE], stack=pr)
            rankc = sb("rankc", [128, 1], stack=pr)
            Pm = sb("Pm", [128, NE], stack=pr)
            cumrow = sb("cumrow", [128, NE], stack=pr)
            basec = sb("basec", [128, 1], stack=pr)
            diagB = sb("diagB", [128, NE], stack=pr)
            ecolb = sb("ecolb", [128, 128], stack=pr)
            pib = sb("pib", [128, NE], stack=pr)
            Um = sb("Um", [128, 128], stack=pr)
            T.op('dve', lambda e: e.scalar_tensor_tensor(out=keyc[0:NE, :], in0=psb[pc_][0:NE, 0:1], scalar=32.0,
                                                         in1=r31col[0:NE, :], op0=ALU.mult, op1=ALU.add),
                 reads=[('ps', pc_), 'r31col'], writes=['keyc'])
            T.op('dve', lambda e: e.scalar_tensor_tensor(out=keyr[0:NE, :], in0=psb[pw_][0:NE, 0:NE], scalar=32.0,
                                                         in1=r31row[0:NE, :], op0=ALU.mult, op1=ALU.add),
                 reads=[('ps', pw_), 'r31row'], writes=['keyr'])
            T.op('dve', lambda e: e.tensor_scalar(out=gtm[0:NE, :], in0=keyr[0:NE, :], scalar1=keyc[0:NE, 0:1],
                                                  scalar2=None, op0=ALU.is_gt),
                 reads=['keyr', 'keyc'], writes=['gtm'])
            T.op('dve', lambda e: e.tensor_reduce(out=rankc[0:NE, :], in_=gtm[0:NE, :], axis=AX.X, op=ALU.add),
                 reads=['gtm'], writes=['rankc'])
            T.op('dve', lambda e: e.tensor_scalar(out=Pm[0:NE, :], in0=erow[0:NE, :], scalar1=rankc[0:NE, 0:1],
                                                  scalar2=None, op0=ALU.is_equal),
                 reads=['erow', 'rankc'], writes=['Pm'])
            for r in range(NE):
                T.op('dve', lambda e, r=r: e.memset(cumrow[:, r:r + 1], float(CUM[r])), writes=[('cumrow', r)])
            T.op('dve', lambda e: e.tensor_tensor(out=gtm[0:NE, :], in0=Pm[0:NE, :], in1=cumrow[0:NE, :], op=ALU.mult),
                 reads=['Pm', 'gtm'] + [('cumrow', r) for r in range(NE)], writes=['gtm'])
            T.op('dve', lambda e: e.tensor_reduce(out=basec[0:NE, :], in_=gtm[0:NE, :], axis=AX.X, op=ALU.add),
                 reads=['gtm'], writes=['basec'])
            T.op('dve', lambda e: e.tensor_scalar(out=diagB[0:NE, :], in0=ident[0:NE, 0:NE], scalar1=basec[0:NE, 0:1],
                                                  scalar2=None, op0=ALU.mult), reads=['basec'], writes=['diagB'])
            T.op('dve', lambda e: e.tensor_copy(out=ecolb[0:NE, :], in_=ecol[0:NE, 0:1].to_broadcast([NE, 128])),
                 reads=['ecol'], writes=['ecolb'])
            pp_ = getps()
            mmgroup(psb[pp_][:, 0:NE], ('ps', pp_), [(ecolb[0:NE, :], Pm[0:NE, :])], reads=['ecolb', 'Pm'])
            T.op('dve', lambda e: e.tensor_copy(out=pib[:], in_=psb[pp_][:, 0:NE]), reads=[('ps', pp_)], writes=['pib'])
            T.op('pool', lambda e: e.affine_select(out=Um[:], in_=ones[:], pattern=[[1, 128]], compare_op=ALU.is_gt,
                                                   fill=0.0, base=0, channel_multiplier=-1), writes=['Um'])
            slotf = sb("slotf", [128, NT * 4], stack=pr)
            tsl = [sb("tsl%d" % i, [128, NE], stack=pr) for i in range(2)]
            isl = 0
            for tt in range(NT):
                ps_ = getps()
                pairs = [(ones[:, :], MK[:, tp, :]) for tp in range(tt)] + [(Um[:, :], MK[:, tt, :])] + \
                        [(ones[0:NE, :], diagB[0:NE, :])]
                mmgroup(psb[ps_][:, 0:NE], ('ps', ps_), pairs, reads=['MK', 'Um', 'diagB'])
                for j in range(4):
                    i2 = isl % 2
                    isl += 1
                    T.op('dve', lambda e, i2=i2, tt=tt, j=j, ps_=ps_: e.tensor_tensor(
                        out=tsl[i2][:], in0=psb[ps_][:, 0:NE], in1=OH[:, tt, j, :], op=ALU.mult),
                        reads=[('ps', ps_)], writes=[('tsl', i2)])
                    T.op('dve', lambda e, i2=i2, tt=tt, j=j: e.tensor_reduce(
                        out=slotf[:, 4 * tt + j:4 * tt + j + 1], in_=tsl[i2][:], axis=AX.X, op=ALU.add),
                        reads=[('tsl', i2)], writes=[('slotf', tt, j)])
            T.op('dve', lambda e: e.tensor_copy(out=SLOT[:, :], in_=slotf[:, :]),
                 reads=[('slotf', tt, j) for tt in range(NT) for j in range(4)], writes=['SLOT'])
            wif = sb("wif", [128, NE, 16], stack=pr)
            wdf = sb("wdf", [128, NE, 8], stack=pr)
            bif = sb("bif", [128, NE], stack=pr)
            pib2 = sb("pib2", [128, NE], stack=pr)
            T.op('dve', lambda e: e.tensor_scalar(out=pib2[:], in0=pib[:], scalar1=2048.0, scalar2=None, op0=ALU.mult),
                 reads=['pib'], writes=['pib2'])
            for r in range(NE):
                T.op('dve', lambda e, r=r: e.tensor_scalar(out=wif[:, r, :], in0=iou[:], scalar1=pib2[:, r:r + 1],
                                                           scalar2=None, op0=ALU.add),
                     reads=['iou', 'pib2'], writes=[('wif', r)])
            T.op('dve', lambda e: e.tensor_copy(out=WIU[:, :, :], in_=wif[:, :, :]),
                 reads=[('wif', r) for r in range(NE)], writes=['WIU'])
            T.op('dve', lambda e: e.tensor_scalar(out=pib2[:], in0=pib[:], scalar1=1024.0, scalar2=None, op0=ALU.mult),
                 reads=['pib', 'pib2'] + [('wif', r) for r in range(NE)], writes=['pib2'])
            for r in range(NE):
                T.op('dve', lambda e, r=r: e.tensor_scalar(out=wdf[:, r, :], in0=iod[:], scalar1=pib2[:, r:r + 1],
                                                           scalar2=None, op0=ALU.add),
                     reads=['iod', 'pib2'], writes=[('wdf', r)])
            T.op('dve', lambda e: e.tensor_copy(out=WID[:, :, :], in_=wdf[:, :, :]),
                 reads=[('wdf', r) for r in range(NE)], writes=['WID'])
            T.op('dve', lambda e: e.scalar_tensor_tensor(out=bif[:], in0=pib[:], scalar=128.0,
                                                         in1=ecol[:, 0:1].to_broadcast([128, NE]),
                                                         op0=ALU.mult, op1=ALU.add),
                 reads=['pib', 'ecol'], writes=['bif'])
            T.op('dve', lambda e: e.tensor_copy(out=BIU[:, :], in_=bif[:, :]), reads=['bif'], writes=['BIU'])
            utile = [sb("utile%d" % i, [128, D], BF16, stack=pr) for i in range(2)]
            for tt in range(NT):
                b = tt % 2
                T.dma('sp', utile[b][:], u2tok_d[128 * tt:128 * (tt + 1), :], writes=[('utile', b)])
                for j in range(4):
                    T.dma('pool', xs_d[:, :], utile[b][:, :], reads=[('utile', b), 'SLOT'], writes=[('xs', tt, j)],
                          out_offset=bass.IndirectOffsetOnAxis(ap=SLOT[:, 4 * tt + j:4 * tt + j + 1], axis=0))
        free_long(OH_blk)
        if debug == 'route':
            with Scope(A, T) as dd:
                dt_ = sb("dbgt", [128, 2048], stack=dd)
                T.op('dve', lambda e: e.memset(dt_[:], 0.0), writes=['dbgt'])
                T.op('dve', lambda e: e.tensor_copy(out=dt_[:, 0:64], in_=SLOT[:, :]), reads=['dbgt'], writes=['dbgt'])
                T.op('dve', lambda e: e.tensor_copy(out=dt_[:, 64:64 + 512], in_=WIU[:, :, :].rearrange("p r j -> p (r j)")),
                     reads=['dbgt'], writes=['dbgt'])
                T.op('dve', lambda e: e.tensor_copy(out=dt_[:, 576:576 + 256], in_=WID[:, :, :].rearrange("p r j -> p (r j)")),
                     reads=['dbgt'], writes=['dbgt'])
                T.op('dve', lambda e: e.tensor_copy(out=dt_[:, 832:832 + 32], in_=BIU[:, :]), reads=['dbgt'], writes=['dbgt'])
                T.op('dve', lambda e: e.tensor_copy(out=dt_[:, 896:896 + 64], in_=GT[:, :, :].rearrange("p t j -> p (t j)")),
                     reads=['dbgt'], writes=['dbgt'])
                T.dma('sp', dbg_d[:, 0:2048], dt_[:, :], reads=['dbgt'])
            T.finish()
            return nc

        with Scope(A, T) as pe_:
            ugT = [sb("ugT%d" % i, [128, 8, 2048], BF16, stack=pe_) for i in range(2)]
            actT = sb("actT", [128, 8, 2048], BF16, stack=pe_)
            wugh = [sb("wugh%d" % i, [128, 8, 4, 128], BF16, stack=pe_) for i in range(2)]
            wulh = [sb("wulh%d" % i, [128, 8, 4, 128], BF16, stack=pe_) for i in range(2)]
            wdb = sb("wdb", [128, 8, D], BF16, stack=pe_)
            NSTG = 4
            NXS = 6
            stg = [sb("stg%d" % i, [128, D], stack=pe_) for i in range(NSTG)]
            xst = [sb("xst%d" % i, [128, D], BF16, stack=pe_) for i in range(NXS)]
            osb = [sb("osb%d" % i, [128, D], stack=pe_) for i in range(2)]
            bu = [sb("bu%d" % i, [128, 16], stack=pe_) for i in range(2)]
            b7 = [sb("b7%d" % i, [128, 8], stack=pe_) for i in range(2)]
            b1 = [sb("b1%d" % i, [128, 8], stack=pe_) for i in range(2)]
            NZ = 2
            zg = [sb("zg%d" % i, [128, 512], stack=pe_) for i in range(NZ)]
            sg = [sb("sg%d" % i, [128, 512], stack=pe_) for i in range(NZ)]
            zl = [sb("zl%d" % i, [128, 512], stack=pe_) for i in range(NZ)]
            cnts = {'stg': 0, 'xst': 0, 'osb': 0, 'z': 0, 'cast': 0}

            def next_stg():
                s = cnts['stg'] % NSTG
                cnts['stg'] += 1
                return s

            def cast_eng():
                cnts['cast'] += 1
                return 'act' if cnts['cast'] % 2 == 0 else 'dve'

            def load_half(r, h, hb):
                for k in range(8):
                    s = next_stg()
                    T.dma('pool', stg[s][:, :], w_up2[:, :], reads=['WIU'], writes=[('stg', s)],
                          in_offset=bass.IndirectOffsetOnAxis(ap=WIU[:, r, 2 * k + h:2 * k + h + 1], axis=0))
                    v = stg[s][:, :].rearrange("p (a b c) -> p a b c", a=4, b=128, c=2)
                    ce = cast_eng()
                    if ce == 'act':
                        T.op('act', lambda e, v=v, k=k: e.copy(out=wugh[hb][:, k, :, :], in_=v[:, :, :, 0]),
                             reads=[('stg', s)], writes=[('wugh', hb, k)])
                        T.op('dve', lambda e, v=v, k=k: e.tensor_copy(out=wulh[hb][:, k, :, :], in_=v[:, :, :, 1]),
                             reads=[('stg', s)], writes=[('wulh', hb, k)])
                    else:
                        T.op('dve', lambda e, v=v, k=k: e.tensor_copy(out=wugh[hb][:, k, :, :], in_=v[:, :, :, 0]),
                             reads=[('stg', s)], writes=[('wugh', hb, k)])
                        T.op('act', lambda e, v=v, k=k: e.copy(out=wulh[hb][:, k, :, :], in_=v[:, :, :, 1]),
                             reads=[('stg', s)], writes=[('wulh', hb, k)])

            def load_bias(r):
                rb = posof[r] % 2
                T.dma('pool', bu[rb][:, :], b_up_d[:, :], reads=['BIU'], writes=[('bu', rb)],
                      in_offset=bass.IndirectOffsetOnAxis(ap=BIU[:, r:r + 1], axis=0))
                T.op('dve', lambda e: e.tensor_scalar(out=b7[rb][:], in0=bu[rb][:, 0:16:2], scalar1=-1.0, scalar2=7.0,
                                                      op0=ALU.mult, op1=ALU.add), reads=[('bu', rb)], writes=[('b7', rb)])
                T.op('dve', lambda e: e.tensor_scalar(out=b1[rb][:], in0=bu[rb][:, 1:16:2], scalar1=1.0, scalar2=None,
                                                      op0=ALU.add), reads=[('bu', rb)], writes=[('b1', rb)])

            def load_down_chunk(r, k):
                s = next_stg()
                T.dma('pool', stg[s][:, :], w_down2[:, :], reads=['WID'], writes=[('stg', s)],
                      in_offset=bass.IndirectOffsetOnAxis(ap=WID[:, r, k:k + 1], axis=0))
                ce = cast_eng()
                if ce == 'act':
                    T.op('act', lambda e: e.copy(out=wdb[:, k, :], in_=stg[s][:]), reads=[('stg', s)], writes=[('wdb', k)])
                else:
                    T.op('dve', lambda e: e.tensor_copy(out=wdb[:, k, :], in_=stg[s][:]),
                         reads=[('stg', s)], writes=[('wdb', k)])

            def build_ug(r):
                ub = posof[r] % 2
                for blk in range(CR[r] // 128):
                    xb_ = cnts['xst'] % NXS
                    cnts['xst'] += 1
                    row0 = CUM[r] + 128 * blk
                    T.dma('sp', xst[xb_][:], xs_d[row0:row0 + 128, :], writes=[('xst', xb_)])
                    pi = getps()
                    psv = psb[pi][:, :].bitcast(BF16)
                    for k in range(8):
                        T.op('pe', lambda e, k=k, psv=psv, xb_=xb_: e.transpose(
                            out=psv[:, 128 * k:128 * (k + 1)], in_=xst[xb_][:, 128 * k:128 * (k + 1)],
                            identity=identb[:]),
                            reads=[('xst', xb_)] if k == 0 else (), writes=[('ps', pi)] if k == 0 else (),
                            signal=(k == 7))
                    T._record((T.own['pe'], T.cnt['pe']), [('xst', xb_)], [('ps', pi)])
                    dstv = ugT[ub][:, :, 128 * blk:128 * (blk + 1)]
                    srcv = psv.rearrange("p (k s) -> p k s", k=8)
                    if blk % 2 == 0:
                        T.op('act', lambda e, dstv=dstv, srcv=srcv: e.copy(out=dstv, in_=srcv),
                             reads=[('ps', pi)], writes=[('ug', ub, blk)])
                    else:
                        T.op('dve', lambda e, dstv=dstv, srcv=srcv: e.tensor_copy(out=dstv, in_=srcv),
                             reads=[('ps', pi)], writes=[('ug', ub, blk)])

            order = []
            for i_ in range(NE // 2):
                order += [i_, NE - 1 - i_]
            posof = {r_: p_ for p_, r_ in enumerate(order)}
            halves = [(r, h) for r in order for h in range(2)]
            load_bias(order[0])
            load_half(order[0], 0, 0)
            build_ug(order[0])
            for ih, (r, h) in enumerate(halves):
                hb = ih % 2
                ub = posof[r] % 2
                rb = posof[r] % 2
                C = CR[r]
                if ih + 1 < len(halves):
                    r2_, h2_ = halves[ih + 1]
                    if h2_ == 0:
                        load_bias(r2_)
                    load_half(r2_, h2_, (ih + 1) % 2)
                tiles = [(o, min(512, C - o)) for o in range(0, C, 512)]
                for hp4 in range(4):
                    hp_ = 4 * h + hp4
                    for (o, n) in tiles:
                        i3 = cnts['z'] % NZ
                        cnts['z'] += 1
                        ugk = [('ug', ub, b_) for b_ in range(o // 128, (o + n) // 128)]
                        pg = getps()
                        mmgroup(psb[pg][:, 0:n], ('ps', pg),
                                [(wugh[hb][:, k, hp4, :], ugT[ub][:, k, o:o + n]) for k in range(8)],
                                reads=[('wugh', hb, k) for k in range(8)] + ugk)
                        pl = getps()
                        mmgroup(psb[pl][:, 0:n], ('ps', pl),
                                [(wulh[hb][:, k, hp4, :], ugT[ub][:, k, o:o + n]) for k in range(8)],
                                reads=[('wulh', hb, k) for k in range(8)] + ugk)
                        T.op('act', lambda e, i3=i3, pg=pg, rb=rb, hp_=hp_, n=n: e.activation(
                            out=zg[i3][:, 0:n], in_=psb[pg][:, 0:n], func=AF.Relu,
                            bias=b7[rb][:, hp_:hp_ + 1], scale=-1.0),
                            reads=[('ps', pg), ('b7', rb)], writes=[('zg', i3)])
                        T.op('act', lambda e, i3=i3, n=n: e.activation(
                            out=sg[i3][:, 0:n], in_=zg[i3][:, 0:n], func=AF.Silu, bias=float(7.0 * 1.702), scale=-1.702),
                            reads=[('zg', i3)], writes=[('sg', i3)])
                        T.op('dve', lambda e, i3=i3, pl=pl, rb=rb, hp_=hp_, n=n: e.tensor_scalar(
                            out=zl[i3][:, 0:n], in0=psb[pl][:, 0:n], scalar1=b1[rb][:, hp_:hp_ + 1], scalar2=8.0,
                            op0=ALU.add, op1=ALU.min), reads=[('ps', pl), ('b1', rb)], writes=[('zl', i3)])
                        T.op('dve', lambda e, i3=i3, hp_=hp_, o=o, n=n: e.scalar_tensor_tensor(
                            out=actT[:, hp_, o:o + n], in0=zl[i3][:, 0:n], scalar=-6.0, in1=sg[i3][:, 0:n],
                            op0=ALU.max, op1=ALU.mult),
                            reads=[('sg', i3), ('zl', i3)], writes=[('act', hp_, o // 512)])
                    load_down_chunk(r, hp_)
                if h == 0:
                    continue
                if posof[r] + 1 < NE:
                    build_ug(order[posof[r] + 1])
                for blk in range(C // 128):
                    ob = cnts['osb'] % 2
                    cnts['osb'] += 1
                    for n_ in range(2):
                        pi = getps()
                        mmgroup(psb[pi][:, :], ('ps', pi),
                                [(actT[:, k, 128 * blk:128 * (blk + 1)], wdb[:, k, 512 * n_:512 * (n_ + 1)])
                                 for k in range(8)],
                                reads=[('wdb', k) for k in range(8)] + [('act', k, blk // 4) for k in range(8)])
                        if n_ == 0:
                            T.op('act', lambda e, pi=pi, ob=ob: e.copy(out=osb[ob][:, 0:512], in_=psb[pi][:, :]),
                                 reads=[('ps', pi)], writes=[('osb', ob, 0)])
                        else:
                            T.op('dve', lambda e, pi=pi, ob=ob: e.tensor_copy(out=osb[ob][:, 512:1024], in_=psb[pi][:, :]),
                                 reads=[('ps', pi)], writes=[('osb', ob, 1)])
                    row0 = CUM[r] + 128 * blk
                    T.dma('sp', ys_d[row0:row0 + 128, :], osb[ob][:], reads=[('osb', ob, 0), ('osb', ob, 1)])

        yacc, yacc_blk = alloc_long([128, NT, D], F32)
        with Scope(A, T) as pi0:
            b_down = sb("b_down_s", [128, D], stack=pi0)
            gts = [sb("gts%d" % i, [128, 128], stack=pi0) for i in range(2)]
            ygt = [sb("ygt%d" % i, [128, D], stack=pi0) for i in range(3)]
            T.dma('sp', b_down[0:NE, :], b_down_d[:, :], writes=['b_down'])
            ig = 0
            for tt in range(NT):
                b = tt % 2
                pg_ = getps()
                T.op('pe', lambda e, pg_=pg_, tt=tt: e.transpose(
                    out=psb[pg_][0:NE, 0:128], in_=G[:, tt, :], identity=ident[:]),
                    reads=[], writes=[('ps', pg_)])
                T.op('act', lambda e, pg_=pg_, b=b: e.copy(out=gts[b][0:NE, :], in_=psb[pg_][0:NE, 0:128]),
                     reads=[('ps', pg_)], writes=[('gts', b)])
                for n in range(2):
                    pi = getps()
                    mmgroup(psb[pi][:, :], ('ps', pi), [(gts[b][0:NE, :], b_down[0:NE, 512 * n:512 * (n + 1)])],
                            reads=[('gts', b), 'b_down'])
                    T.op('act', lambda e, pi=pi, tt=tt, n=n: e.copy(
                        out=yacc[:, tt, 512 * n:512 * (n + 1)], in_=psb[pi][:, :]),
                        reads=[('ps', pi)], writes=[('y', tt, n)])
                for j in range(4):
                    g3 = ig % 3
                    ig += 1
                    T.dma('pool', ygt[g3][:, :], ys_d[:, :], writes=[('ygt', g3)],
                          in_offset=bass.IndirectOffsetOnAxis(ap=SLOT[:, 4 * tt + j:4 * tt + j + 1], axis=0))
                    T.op('dve', lambda e, g3=g3, tt=tt, j=j: e.scalar_tensor_tensor(
                        out=yacc[:, tt, :], in0=ygt[g3][:], scalar=GT[:, tt, j:j + 1], in1=yacc[:, tt, :],
                        op0=ALU.mult, op1=ALU.add),
                        reads=[('ygt', g3), ('y', tt, 0), ('y', tt, 1)], writes=[('y', tt, 0), ('y', tt, 1)])

        with Scope(A, T) as pf:
            l2g = sb("l2g", [128, D], stack=pf)
            l2b = sb("l2b", [128, D], stack=pf)
            g2row = sb("g2row2", [128, D], stack=pf)
            T.dma('sp', l2g[:], ln2_g_d[0:1, :].partition_broadcast(128), writes=['l2g'])
            T.dma('sp', l2b[:], ln2_b_d[0:1, :].partition_broadcast(128), writes=['l2b'])
            T.dma('sp', g2row[:], g2row_d[:, :], writes=['g2row'])
            h1r = [sb("h1r%d" % i, [128, D], stack=pf) for i in range(2)]
            fo = [sb("fo%d" % i, [128, D], stack=pf) for i in range(2)]
            stt = [sb("stt3%d" % i, [128, 12], stack=pf) for i in range(2)]
            mvt = [sb("mvt3%d" % i, [128, 8], stack=pf) for i in range(2)]
            for tt in range(NT):
                b = tt % 2
                ts_ = slice(128 * tt, 128 * (tt + 1))
                T.dma('sp', h1r[b][:], h1_d[ts_, :], writes=[('h1r', b)])
                T.op('dve', lambda e, tt=tt: e.tensor_tensor(out=yacc[:, tt, :], in0=yacc[:, tt, :], in1=g2row[:],
                                                             op=ALU.mult),
                     reads=[('y', tt, 0), ('y', tt, 1), 'g2row'], writes=[('y', tt, 0), ('y', tt, 1)])
                T.op('dve', lambda e, b=b, tt=tt: e.scalar_tensor_tensor(
                    out=fo[b][:], in0=h1r[b][:], scalar=ALPHA, in1=yacc[:, tt, :], op0=ALU.mult, op1=ALU.add),
                    reads=[('h1r', b), ('y', tt, 0), ('y', tt, 1)], writes=[('fo', b)])
                rs, nm, keys = layernorm_rows(fo[b], ('fo', b), h1r[b], ('h1r', b), mvt[b], ('c', b))
                T.op('act', lambda e, b=b, rs=rs, nm=nm: e.activation(
                    out=fo[b][:], in_=fo[b][:], func=AF.Identity, bias=nm, scale=rs),
                    reads=[('fo', b)] + keys, writes=[('fo', b)])
                T.op('dve', lambda e, b=b: e.tensor_tensor(out=fo[b][:], in0=fo[b][:], in1=l2g[:], op=ALU.mult),
                     reads=[('fo', b), 'l2g'], writes=[('fo', b)])
                T.op('dve', lambda e, b=b: e.tensor_tensor(out=fo[b][:], in0=fo[b][:], in1=l2b[:], op=ALU.add),
                     reads=[('fo', b), 'l2b'], writes=[('fo', b)])
                T.dma('sp', out_d[ts_, :], fo[b][:], reads=[('fo', b)])
        T.finish()
    return nc


def _col(v):
    return np.ascontiguousarray(np.asarray(v, np.float32).reshape(8, 128).T)


def make_in_maps(inputs, cores):
    f = lambda a: np.ascontiguousarray(np.asarray(a, np.float32))
    x = f(inputs['x'])
    c = f(inputs['c'])
    shared = {
        "w_ada": f(inputs['w_ada'][0]),
        "b_ada_col": np.ascontiguousarray(f(inputs['b_ada'][0]).reshape(48, 128).T),
        "b_ada_row": f(inputs['b_ada'][0]).reshape(1, 6 * D),
        "w_in": f(inputs['w_in'][0]),
        "b_in_col": np.ascontiguousarray(f(inputs['b_in'][0]).reshape(48, 128).T),
        "w_cv_dw_col": np.ascontiguousarray(f(inputs['w_cv_dw'][0]).T.reshape(8, 128, 31).transpose(1, 0, 2)),
        "b_cv_dw_col": _col(inputs['b_cv_dw'][0]),
        "ln_cv_g_col": _col(inputs['ln_cv_g'][0]),
        "ln_cv_b_col": _col(inputs['ln_cv_b'][0]),
        "w_cv_out": f(inputs['w_cv_out'][0]),
        "w_lru_conv_col": np.ascontiguousarray(f(inputs['w_lru_conv'][0]).T.reshape(8, 128, 4).transpose(1, 0, 2)),
        "b_lru_conv_col": _col(inputs['b_lru_conv'][0]),
        "w_lru_a_t": np.ascontiguousarray(f(inputs['w_lru_a'][0]).transpose(1, 0, 2)),
        "b_lru_a_col": _col(inputs['b_lru_a'][0]),
        "w_lru_x_t": np.ascontiguousarray(f(inputs['w_lru_x'][0]).transpose(1, 0, 2)),
        "b_lru_x_col": _col(inputs['b_lru_x'][0]),
        "lru_lambda_col": _col(inputs['lru_lambda'][0]),
        "w_lru_out": f(inputs['w_lru_out'][0]),
        "w_o": f(inputs['w_o'][0]),
        "b_o_row": f(inputs['b_o'][0]).reshape(1, D),
        "ln1_g_row": f(inputs['ln1_g'][0]).reshape(1, D),
        "ln1_b_row": f(inputs['ln1_b'][0]).reshape(1, D),
        "w_router_t": np.ascontiguousarray(f(inputs['w_router'][0]).reshape(8, 128, NE).transpose(1, 0, 2)),
        "b_router_row": f(inputs['b_router'][0]).reshape(1, NE),
        "w_up": f(inputs['w_up'][0]),
        "b_up_rows": np.ascontiguousarray(f(inputs['b_up'][0]).reshape(NE, 8, 128, 2).transpose(0, 2, 1, 3)).reshape(NE * 128, 16),
        "w_down": f(inputs['w_down'][0]),
        "b_down": f(inputs['b_down'][0]),
        "ln2_g_row": f(inputs['ln2_g'][0]).reshape(1, D),
        "ln2_b_row": f(inputs['ln2_b'][0]).reshape(1, D),
    }
    maps = []
    for b in cores:
        m = dict(shared)
        m["x"] = np.ascontiguousarray(x[b])
        m["c_col"] = _col(c[b])
        maps.append(m)
    return maps


def kernel(**inputs):
    nc = build()
    maps = make_in_maps(inputs, list(range(8)))
    res = run_bass_kernel_spmd(nc, maps, core_ids=list(range(8)))
    out = np.stack([np.asarray(r["out"], np.float32) for r in res.results], axis=0)
    return out
```

```python
import numpy as np
from contextlib import ExitStack
import concourse.bass as bass
import concourse.mybir as mybir
from concourse.bass_utils import run_bass_kernel_spmd

F32 = mybir.dt.float32
BF16 = mybir.dt.bfloat16
AF = mybir.ActivationFunctionType
ALU = mybir.AluOpType
AX = mybir.AxisListType

S = 2048
D = 1024
NE = 32
ALPHA = float(2.0 ** 0.25)
EPS = 1e-5
NT = S // 128
NQ = S // 512
SEM_ROLL = 30000
CR = [min(2048, -(-(8192 // (r + 1)) // 128) * 128) for r in range(NE)]
CUM = [0]
for _c in CR:
    CUM.append(CUM[-1] + _c)
NSLOT = CUM[-1]
I32 = mybir.dt.int32


class TR:
    def __init__(self, nc, st):
        self.nc = nc
        self.st = st
        self.E = {'pe': nc.tensor, 'act': nc.scalar, 'dve': nc.vector, 'pool': nc.gpsimd, 'sp': nc.sync}
        self.sems = []
        self.own = {}
        self.cnt = {}
        self.seen = {e: {} for e in self.E}
        self.lastw = {}
        self.readers = {}
        self.dslots = {}
        self.drr = {}
        self.ownset = {e: set() for e in self.E}

    def newsem(self, name):
        h = self.st.enter_context(self.nc.semaphore(name))
        self.sems.append(h)
        return len(self.sems) - 1

    def _own(self, e):
        if e not in self.own or self.cnt[e] >= SEM_ROLL:
            self.own[e] = self.newsem("c_%s_%d" % (e, len(self.sems)))
            self.ownset[e].add(self.own[e])
            self.cnt[e] = 0
        return self.own[e]

    def _wait(self, e, tok):
        if tok is None:
            return
        s, v = tok
        if self.seen[e].get(s, 0) >= v:
            return
        self.seen[e][s] = v
        self.E[e].wait_ge(self.sems[s], v)

    def _dep(self, e, tok):
        if tok is None:
            return
        if e == 'pe' and tok[0] in self.ownset['pe']:
            return
        self._wait(e, tok)

    def _deps(self, e, reads, writes):
        for k in reads:
            self._dep(e, self.lastw.get(k))
        for k in writes:
            self._dep(e, self.lastw.get(k))
            r = self.readers.get(k)
            if r:
                for s, v in r.items():
                    self._dep(e, (s, v))

    def _record(self, tok, reads, writes):
        s, v = tok
        for k in reads:
            r = self.readers.setdefault(k, {})
            if r.get(s, 0) < v:
                r[s] = v
        for k in writes:
            self.lastw[k] = tok
            self.readers[k] = {}

    @staticmethod
    def _excl(reads, writes):
        ps = [k for k in reads if isinstance(k, tuple) and k[0] == 'ps']
        if not ps:
            return reads, writes
        return [k for k in reads if not (isinstance(k, tuple) and k[0] == 'ps')], list(writes) + ps

    def op(self, e, fn, reads=(), writes=(), signal=True):
        reads, writes = self._excl(reads, writes)
        self._deps(e, reads, writes)
        s = self._own(e)
        inst = fn(self.E[e])
        if signal:
            self.cnt[e] += 1
            inst.then_inc(self.sems[s], 1)
            tok = (s, self.cnt[e])
        else:
            tok = (s, self.cnt[e] + 1)
        self._record(tok, reads, writes)
        return tok

    def dma(self, q, out, in_, reads=(), writes=(), nslots=12, out_offset=None, in_offset=None):
        self._deps(q, reads, writes)
        if q not in self.dslots:
            self.dslots[q] = [[self.newsem("d_%s_%d" % (q, i)), 0] for i in range(nslots)]
            self.drr[q] = 0
        slot = self.dslots[q][self.drr[q]]
        self.drr[q] = (self.drr[q] + 1) % len(self.dslots[q])
        if slot[1] > 0:
            self._wait(q, (slot[0], slot[1]))
        if out_offset is not None or in_offset is not None:
            inst = self.E[q].indirect_dma_start(out=out, out_offset=out_offset, in_=in_, in_offset=in_offset)
        else:
            inst = self.E[q].dma_start(out=out, in_=in_)
        slot[1] += 16
        inst.then_inc(self.sems[slot[0]], 16)
        tok = (slot[0], slot[1])
        self._record(tok, reads, writes)
        return tok

    def barrier(self):
        toks = []
        for e in self.own:
            if self.cnt[e] > 0:
                toks.append((self.own[e], self.cnt[e]))
        for q in self.dslots:
            for s, v in self.dslots[q]:
                if v > 0:
                    toks.append((s, v))
        for e in self.E:
            for t in toks:
                if e == 'pe' and t[0] == self.own.get(e):
                    continue
                self._wait(e, t)
        self.lastw = {}
        self.readers = {}

    def finish(self):
        for q in self.dslots:
            for s, v in self.dslots[q]:
                if v > 0:
                    self._wait('sp', (s, v))
        for e in self.own:
            if e != 'sp' and self.cnt[e] > 0:
                self._wait('sp', (self.own[e], self.cnt[e]))


class Arena:
    def __init__(self, R, nbytes):
        self.R = R
        self.free = [(0, nbytes)]
        self.pending = []

    def alloc(self, shape, dt):
        esz = 2 if dt == BF16 else 4
        n = esz
        for s in shape[1:]:
            n *= s
        n = (n + 63) // 64 * 64
        for i, (o, l) in enumerate(self.free):
            if l >= n:
                if l == n:
                    self.free.pop(i)
                else:
                    self.free[i] = (o + n, l - n)
                v = self.R[:, o // 4:(o + n) // 4]
                if dt == BF16:
                    v = v.bitcast(BF16)
                tot = 1
                for s in shape[1:]:
                    tot *= s
                v = v[:, 0:tot]
                if len(shape) == 3:
                    v = v.rearrange("p (a b) -> p a b", a=shape[1])
                elif len(shape) == 4:
                    v = v.rearrange("p (a b c) -> p a b c", a=shape[1], b=shape[2])
                return v, (o, n)
        raise RuntimeError("arena full: need %d, free=%s" % (n, self.free))

    def release(self, blk):
        self.pending.append(blk)

    def commit(self):
        fl = sorted(self.free + self.pending)
        self.pending = []
        out = []
        for o, l in fl:
            if out and out[-1][0] + out[-1][1] == o:
                out[-1] = (out[-1][0], out[-1][1] + l)
            else:
                out.append((o, l))
        self.free = out


class Scope:
    def __init__(self, A, T):
        self.A = A
        self.T = T
        self.blks = []

    def __enter__(self):
        return self

    def alloc(self, shape, dt):
        v, b = self.A.alloc(shape, dt)
        self.blks.append(b)
        return v

    def __exit__(self, *a):
        if a[0] is None:
            self.T.barrier()
            for b in self.blks:
                self.A.release(b)
            self.A.commit()
        return False


def build(debug=None):
    nc = bass.Bass("TRN2", target_bir_lowering=False)

    def din(name, shape, dt=F32):
        return nc.dram_tensor(name, list(shape), dt, kind="ExternalInput").ap()

    x_d = din("x", [S, D])
    c_col_d = din("c_col", [128, 8])
    w_ada_d = din("w_ada", [D, 6 * D])
    b_ada_col_d = din("b_ada_col", [128, 48])
    b_ada_row_d = din("b_ada_row", [1, 6 * D])
    w_in_d = din("w_in", [D, 6 * D])
    b_in_col_d = din("b_in_col", [128, 48])
    w_cvdw_d = din("w_cv_dw_col", [128, 8, 31])
    b_cvdw_d = din("b_cv_dw_col", [128, 8])
    ln_cv_g_d = din("ln_cv_g_col", [128, 8])
    ln_cv_b_d = din("ln_cv_b_col", [128, 8])
    w_cv_out_d = din("w_cv_out", [D, D])
    w_lconv_d = din("w_lru_conv_col", [128, 8, 4])
    b_lconv_d = din("b_lru_conv_col", [128, 8])
    w_lru_a_d = din("w_lru_a_t", [128, 8, 128])
    b_lru_a_d = din("b_lru_a_col", [128, 8])
    w_lru_x_d = din("w_lru_x_t", [128, 8, 128])
    b_lru_x_d = din("b_lru_x_col", [128, 8])
    lam_d = din("lru_lambda_col", [128, 8])
    w_lru_out_d = din("w_lru_out", [D, D])
    w_o_d = din("w_o", [D, D])
    b_o_d = din("b_o_row", [1, D])
    ln1_g_d = din("ln1_g_row", [1, D])
    ln1_b_d = din("ln1_b_row", [1, D])
    w_router_d = din("w_router_t", [128, 8, NE])
    b_router_d = din("b_router_row", [1, NE])
    w_up_d = din("w_up", [NE, D, 2 * D])
    b_up_d = din("b_up_rows", [NE * 128, 16])
    w_down_d = din("w_down", [NE, D, D])
    b_down_d = din("b_down", [NE, D])
    ln2_g_d = din("ln2_g_row", [1, D])
    ln2_b_d = din("ln2_b_row", [1, D])
    out_d = nc.dram_tensor("out", [S, D], F32, kind="ExternalOutput").ap()
    h1_d = nc.dram_tensor("h1_scratch", [S, D], F32, kind="Internal").ap()
    g1row_d = nc.dram_tensor("g1row_scratch", [128, D], F32, kind="Internal").ap()
    g1bo_d = nc.dram_tensor("g1bo_scratch", [128, D], F32, kind="Internal").ap()
    g2row_d = nc.dram_tensor("g2row_scratch", [128, D], F32, kind="Internal").ap()
    sh2row_d = nc.dram_tensor("sh2row_scratch", [128, D], F32, kind="Internal").ap()
    sc2prow_d = nc.dram_tensor("sc2prow_scratch", [128, D], F32, kind="Internal").ap()
    u2tok_d = nc.dram_tensor("u2tok_scratch", [S, D], BF16, kind="Internal").ap()
    xs_d = nc.dram_tensor("xs_scratch", [NSLOT, D], BF16, kind="Internal").ap()
    ys_d = nc.dram_tensor("ys_scratch", [NSLOT, D], F32, kind="Internal").ap()
    w_up2 = w_up_d.rearrange("e d (h n) -> (e d h) n", h=2)
    w_down2 = w_down_d.rearrange("e d n -> (e d) n")
    dbg_d = None
    if debug is not None:
        dbg_d = nc.dram_tensor("dbg", [128, 8 * S], F32, kind="ExternalOutput").ap()

    with ExitStack() as st:
        T = TR(nc, st)
        ARENA_BYTES = 194 * 1024
        Rt = st.enter_context(nc.sbuf_tensor("arena", [128, ARENA_BYTES // 4], F32))
        A = Arena(Rt, ARENA_BYTES)

        def sb(name, shape, dt=F32, stack=None):
            if stack is None:
                return st.enter_context(nc.sbuf_tensor(name, list(shape), dt))
            return stack.alloc(list(shape), dt)

        def alloc_long(shape, dt):
            return A.alloc(list(shape), dt)

        def free_long(blk):
            T.barrier()
            A.release(blk)
            A.commit()

        psb = [st.enter_context(nc.psum_tensor("ps%d" % i, [128, 512], F32)) for i in range(8)]
        ps_rr = [0]

        def getps():
            i = ps_rr[0]
            ps_rr[0] = (i + 1) % 8
            return i

        def mmgroup(ps_ap, pskey, pairs, reads=()):
            n = len(pairs)
            tok = None
            for i, (l, r) in enumerate(pairs):
                last = (i == n - 1)
                tok = T.op('pe', lambda e, l=l, r=r, i=i, last=last: e.matmul(
                    ps_ap, lhsT=l, rhs=r, start=(i == 0), stop=last),
                    reads=reads if i == 0 else (), writes=[pskey] if i == 0 else (), signal=last)
            if n > 1:
                T._record(tok, reads, [pskey])
            return tok

        def transpose4(pi, src, k0, rkey):
            for q in range(4):
                k = k0 + q
                T.op('pe', lambda e, q=q, k=k: e.transpose(
                    out=psb[pi][:, 128 * q:128 * (q + 1)], in_=src[:, 128 * k:128 * (k + 1)],
                    identity=ident[:]),
                    reads=[rkey] if q == 0 else (), writes=[('ps', pi)] if q == 0 else (),
                    signal=(q == 3))
            T._record((T.own['pe'], T.cnt['pe']), [rkey], [('ps', pi)])

        ident = sb("ident", [128, 128])
        ones = sb("ones", [128, 128])
        epsc = sb("epsc", [128, 1])
        T.op('pool', lambda e: e.memset(ones[:], 1.0), writes=['ones'])
        T.op('pool', lambda e: e.memset(epsc[:], EPS), writes=['epsc'])
        T.op('pool', lambda e: e.affine_select(out=ident[:], in_=ones[:], pattern=[[-1, 128]],
                                               compare_op=ALU.is_equal, fill=0.0, base=0,
                                               channel_multiplier=1), reads=['ones'], writes=['ident'])

        identb = sb("identb", [128, 128], BF16)
        T.op('pool', lambda e: e.tensor_copy(out=identb[:], in_=ident[:]), reads=['ident'], writes=['identb'])

        def ld_small(name, src, shape):
            t = sb(name, shape)
            T.dma('sp', t[:], src, writes=[name])
            return t

        c_col = ld_small("c_col_s", c_col_d[:, :], [128, 8])
        b_ada_col = ld_small("b_ada_col_s", b_ada_col_d[:, :], [128, 48])
        b_in_col = ld_small("b_in_col_s", b_in_col_d[:, :], [128, 48])
        w_cvdw = ld_small("w_cvdw_s", w_cvdw_d[:, :, :], [128, 8, 31])
        b_cvdw = ld_small("b_cvdw_s", b_cvdw_d[:, :], [128, 8])
        ln_cv_g = ld_small("ln_cv_g_s", ln_cv_g_d[:, :], [128, 8])
        ln_cv_b = ld_small("ln_cv_b_s", ln_cv_b_d[:, :], [128, 8])
        w_lconv = ld_small("w_lconv_s", w_lconv_d[:, :, :], [128, 8, 4])
        b_lconv = ld_small("b_lconv_s", b_lconv_d[:, :], [128, 8])
        b_lru_a = ld_small("b_lru_a_s", b_lru_a_d[:, :], [128, 8])
        b_lru_x = ld_small("b_lru_x_s", b_lru_x_d[:, :], [128, 8])
        lam = ld_small("lam_s", lam_d[:, :], [128, 8])
        w_router = ld_small("w_router_s", w_router_d[:, :, :], [128, 8, NE])
        T.barrier()

        mod_col = sb("mod_col", [128, 48])
        sc1p = sb("sc1p", [128, 8])
        sc2p = sb("sc2p", [128, 8])
        clam = sb("clam", [128, 8])
        clam2 = sb("clam2", [128, 8])
        G = sb("G", [128, NT, NE])
        GT = sb("GT", [128, NT, 4])
        SLOT = sb("SLOT", [128, NT * 4], I32)
        WIU = sb("WIU", [128, NE, 16], I32)
        WID = sb("WID", [128, NE, 8], I32)
        BIU = sb("BIU", [128, NE], I32)

        def dbg_out(view3):
            with Scope(A, T) as dd:
                for k in range(8):
                    dt_ = sb("dbgt", [128, S], stack=dd)
                    T.op('dve', lambda e, k=k, dt_=dt_: e.tensor_copy(out=dt_[:], in_=view3[:, k, :]),
                         writes=[('dbgt', k)])
                    T.dma('sp', dbg_d[:, S * k:S * (k + 1)], dt_[:], reads=[('dbgt', k)])
            T.finish()

        with Scope(A, T) as p0:
            sc = sb("sc", [128, 8], stack=p0)
            scb = sb("scb", [128, 8, 128], stack=p0)
            wa = [sb("wa%d" % i, [128, 8, 128], stack=p0) for i in range(3)]
            wr = [sb("wr%d" % i, [128, 8, 512], stack=p0) for i in range(2)]
            brow = sb("brow", [128, 512], stack=p0)
            bo_b = sb("bo_b", [128, D], stack=p0)
            tmp1 = sb("p0tmp", [128, 8], stack=p0)
            g1row = sb("g1row", [128, D], stack=p0)
            g2row = sb("g2row", [128, D], stack=p0)
            g1bo = sb("g1bo", [128, D], stack=p0)
            sh2row = sb("sh2row", [128, D], stack=p0)
            sc2prow = sb("sc2prow", [128, D], stack=p0)
            T.op('act', lambda e: e.activation(out=sc[:], in_=c_col[:], func=AF.Silu), writes=['sc'])
            for k in range(8):
                T.op('dve', lambda e, k=k: e.tensor_copy(out=scb[:, k, :],
                                                         in_=sc[:, k:k + 1].to_broadcast([128, 128])),
                     reads=['sc'], writes=[('scb', k)])
            T.op('act', lambda e: e.activation(out=tmp1[:], in_=lam[:], func=AF.Exp, scale=-1.0),
                 writes=['p0tmp'])
            T.op('act', lambda e: e.activation(out=tmp1[:], in_=tmp1[:], func=AF.Ln, bias=1.0, scale=1.0),
                 reads=['p0tmp'], writes=['p0tmp'])
            T.op('dve', lambda e: e.tensor_scalar(out=clam[:], in0=tmp1[:], scalar1=-8.0, scalar2=None,
                                                  op0=ALU.mult), reads=['p0tmp'], writes=['clam'])
            T.op('dve', lambda e: e.tensor_scalar(out=clam2[:], in0=tmp1[:], scalar1=-16.0, scalar2=None,
                                                  op0=ALU.mult), reads=['p0tmp'], writes=['clam2'])
            pm = getps()
            for j in range(48):
                slot = j % 3
                T.dma('sp', wa[slot][:, :, :],
                      w_ada_d[:, 128 * j:128 * (j + 1)].rearrange("(k p) n -> p k n", p=128),
                      writes=[('wa', slot)])
                for k in range(8):
                    T.op('pe', lambda e, k=k, j=j, slot=slot: e.matmul(
                        psb[pm][:, j:j + 1], lhsT=wa[slot][:, k, :], rhs=sc[:, k:k + 1],
                        start=(k == 0), stop=(k == 7)),
                        reads=[('wa', slot), 'sc'] if k == 0 else (),
                        writes=[('ps', pm)] if k == 0 else (), signal=(k == 7))
                T._record((T.own['pe'], T.cnt['pe']), [('wa', slot), 'sc'], [('ps', pm)])
            T.op('dve', lambda e: e.tensor_tensor(out=mod_col[:], in0=psb[pm][:, 0:48], in1=b_ada_col[:],
                                                  op=ALU.add), reads=[('ps', pm)], writes=['mod'])
            T.op('dve', lambda e: e.tensor_scalar(out=sc1p[:], in0=mod_col[:, 8:16], scalar1=1.0, scalar2=None,
                                                  op0=ALU.add), reads=['mod'], writes=['sc1p'])
            T.op('dve', lambda e: e.tensor_scalar(out=sc2p[:], in0=mod_col[:, 32:40], scalar1=1.0, scalar2=None,
                                                  op0=ALU.add), reads=['mod'], writes=['sc2p'])
            T.dma('sp', bo_b[:], b_o_d[0:1, :].partition_broadcast(128), writes=['bo_b'])
            i_r = 0
            for sec, dst in ((2, g1row), (5, g2row), (3, sh2row), (4, sc2prow)):
                for n in range(2):
                    off = sec * D + 512 * n
                    slot = i_r % 2
                    i_r += 1
                    T.dma('sp', wr[slot][:, :, :],
                          w_ada_d[:, off:off + 512].rearrange("(k p) n -> p k n", p=128),
                          writes=[('wr', slot)])
                    T.dma('sp', brow[:], b_ada_row_d[0:1, off:off + 512].partition_broadcast(128),
                          writes=['brow'])
                    pi = getps()
                    mmgroup(psb[pi][:, :], ('ps', pi),
                            [(scb[:, k, :], wr[slot][:, k, :]) for k in range(8)],
                            reads=[('wr', slot)] + [('scb', k) for k in range(8)])
                    T.op('dve', lambda e, pi=pi, dst=dst, n=n: e.tensor_tensor(
                        out=dst[:, 512 * n:512 * (n + 1)], in0=psb[pi][:, :], in1=brow[:], op=ALU.add),
                        reads=[('ps', pi), 'brow'], writes=[('grow', sec, n)])
            T.op('dve', lambda e: e.tensor_tensor(out=g1bo[:], in0=g1row[:], in1=bo_b[:], op=ALU.mult),
                 reads=[('grow', 2, 0), ('grow', 2, 1), 'bo_b'], writes=['g1bo'])
            T.dma('sp', g1row_d[:, :], g1row[:], reads=[('grow', 2, 0), ('grow', 2, 1)])
            T.dma('sp', g2row_d[:, :], g2row[:], reads=[('grow', 5, 0), ('grow', 5, 1)])
            T.dma('sp', g1bo_d[:, :], g1bo[:], reads=['g1bo'])
            T.op('dve', lambda e: e.tensor_scalar(out=sc2prow[:], in0=sc2prow[:], scalar1=1.0, scalar2=None, op0=ALU.add),
                 reads=[('grow', 4, 0), ('grow', 4, 1)], writes=[('grow', 4, 0), ('grow', 4, 1)])
            T.dma('sp', sh2row_d[:, :], sh2row[:], reads=[('grow', 3, 0), ('grow', 3, 1)])
            T.dma('sp', sc2prow_d[:, :], sc2prow[:], reads=[('grow', 4, 0), ('grow', 4, 1)])

        if debug == 'p0':
            with Scope(A, T) as dd:
                dt_ = sb("dbgt", [128, 3 * D + 128], stack=dd)
                T.op('dve', lambda e: e.memset(dt_[:], 0.0), writes=['dbgt'])
                T.op('dve', lambda e: e.tensor_copy(out=dt_[:, 0:48], in_=mod_col[:]), reads=['dbgt'], writes=['dbgt'])
                T.op('dve', lambda e: e.tensor_copy(out=dt_[:, 48:56], in_=clam[:]), reads=['dbgt'], writes=['dbgt'])
                T.dma('sp', dt_[:, 128:128 + D], g1row_d[:, :], reads=['dbgt'], writes=['dbgt'])
                T.dma('sp', dt_[:, 128 + D:128 + 2 * D], g2row_d[:, :], reads=['dbgt'], writes=['dbgt'])
                T.dma('sp', dt_[:, 128 + 2 * D:128 + 3 * D], g1bo_d[:, :], reads=['dbgt'], writes=['dbgt'])
                T.dma('sp', dbg_d[:, 0:3 * D + 128], dt_[:, :], reads=['dbgt'])
            T.finish()
            return nc

        def layernorm_rows(xt_ap, key_in, junk, junk_key, mvt, tagi):
            T.op('dve', lambda e: e.tensor_reduce(out=mvt[:, 0:1], in_=xt_ap[:, :], axis=AX.X, op=ALU.add),
                 reads=[key_in], writes=[('mv0', tagi)])
            T.op('act', lambda e: e.activation(out=junk[:, :], in_=xt_ap[:, :], func=AF.Square,
                                               accum_out=mvt[:, 1:2]),
                 reads=[key_in], writes=[junk_key, ('mv1', tagi)])
            T.op('dve', lambda e: e.tensor_scalar(out=mvt[:, 2:3], in0=mvt[:, 0:1], scalar1=1.0 / D, scalar2=None,
                                                  op0=ALU.mult),
                 reads=[('mv0', tagi)], writes=[('mv2', tagi)])
            T.op('dve', lambda e: e.tensor_tensor(out=mvt[:, 3:4], in0=mvt[:, 2:3], in1=mvt[:, 2:3], op=ALU.mult),
                 reads=[('mv2', tagi)], writes=[('mv3', tagi)])
            T.op('dve', lambda e: e.scalar_tensor_tensor(out=mvt[:, 3:4], in0=mvt[:, 1:2], scalar=1.0 / D,
                                                         in1=mvt[:, 3:4], op0=ALU.mult, op1=ALU.subtract),
                 reads=[('mv1', tagi), ('mv3', tagi)], writes=[('mv3', tagi)])
            T.op('act', lambda e: e.activation(out=mvt[:, 3:4], in_=mvt[:, 3:4], func=AF.Sqrt,
                                               bias=epsc[:, 0:1], scale=1.0),
                 reads=[('mv3', tagi)], writes=[('mv3', tagi)])
            T.op('dve', lambda e: e.reciprocal(out=mvt[:, 4:5], in_=mvt[:, 3:4]),
                 reads=[('mv3', tagi)], writes=[('rs', tagi)])
            T.op('dve', lambda e: e.tensor_scalar(out=mvt[:, 5:6], in0=mvt[:, 2:3], scalar1=mvt[:, 4:5],
                                                  scalar2=-1.0, op0=ALU.mult, op1=ALU.mult),
                 reads=[('mv2', tagi), ('rs', tagi)], writes=[('nm', tagi)])
            return mvt[:, 4:5], mvt[:, 5:6], [('rs', tagi), ('nm', tagi)]

        uT, uT_blk = alloc_long([128, 8, S], BF16)
        with Scope(A, T) as p1:
            xt = [sb("xt%d" % i, [128, D], stack=p1) for i in range(3)]
            xn = [sb("xn%d" % i, [128, D], stack=p1) for i in range(2)]
            stt = [sb("stt%d" % i, [128, 12], stack=p1) for i in range(2)]
            mvt = [sb("mvt%d" % i, [128, 8], stack=p1) for i in range(2)]
            for tt in range(NT if debug not in ('p1a', 'p1b') else 1):
                a = tt % 3
                b = tt % 2
                T.dma('sp', xt[a][:], x_d[128 * tt:128 * (tt + 1), :], writes=[('xt', a)])
                rs, nm, keys = layernorm_rows(xt[a], ('xt', a), xn[b], ('xn', b), mvt[b], b)
                T.op('act', lambda e, a=a, b=b, rs=rs, nm=nm: e.activation(
                    out=xn[b][:], in_=xt[a][:], func=AF.Identity, bias=nm, scale=rs),
                    reads=[('xt', a)] + keys, writes=[('xn', b)])
                if debug == 'p1a':
                    T.dma('sp', dbg_d[:, 0:D], xn[b][:], reads=[('xn', b)])
                    T.dma('sp', dbg_d[:, D:D + 8], mvt[b][:], reads=[('xn', b)])
                    break
                for half in range(2):
                    pi = getps()
                    transpose4(pi, xn[b], half * 4, ('xn', b))
                    for q in range(4):
                        k = half * 4 + q
                        dst = uT[:, k, 128 * tt:128 * (tt + 1)]
                        if half == 0:
                            T.op('dve', lambda e, pi=pi, q=q, k=k, dst=dst: e.tensor_scalar(
                                out=dst, in0=psb[pi][:, 128 * q:128 * (q + 1)], scalar1=sc1p[:, k:k + 1],
                                scalar2=mod_col[:, k:k + 1], op0=ALU.mult, op1=ALU.add),
                                reads=[('ps', pi)], writes=[('uT', k, tt)])
                        else:
                            T.op('act', lambda e, pi=pi, q=q, k=k, dst=dst: e.activation(
                                out=dst, in_=psb[pi][:, 128 * q:128 * (q + 1)], func=AF.Identity,
                                bias=mod_col[:, k:k + 1], scale=sc1p[:, k:k + 1]),
                                reads=[('ps', pi)], writes=[('uT', k, tt)])
        if debug == 'p1a':
            T.finish()
            return nc
        if debug in ('uT', 'p1b'):
            dbg_out(uT)
            return nc

        p2 = Scope(A, T)
        wst = [sb("wst%d" % i, [128, 8, 128], stack=p2) for i in range(3)]
        wpb = [sb("wpb%d" % i, [128, 8, 128], BF16, stack=p2) for i in range(4)]
        wcnt = [0, 0]

        def load_panel(src_ap, cast_eng=None):
            s1 = wcnt[0] % 3
            s2 = wcnt[1] % 4
            cast_eng = 'act' if wcnt[0] % 2 == 0 else 'dve'
            wcnt[0] += 1
            wcnt[1] += 1
            T.dma('sp', wst[s1][:, :, :], src_ap.rearrange("(k p) n -> p k n", p=128),
                  writes=[('wst', s1)])
            if cast_eng == 'act':
                T.op('act', lambda e: e.copy(out=wpb[s2][:, :, :], in_=wst[s1][:, :, :]),
                     reads=[('wst', s1)], writes=[('wpb', s2)])
            else:
                T.op(cast_eng, lambda e: e.tensor_copy(out=wpb[s2][:, :, :], in_=wst[s1][:, :, :]),
                     reads=[('wst', s1)], writes=[('wpb', s2)])
            return wpb[s2], ('wpb', s2)

        def inproj(panel_tile, pkey, q):
            pi = getps()
            mmgroup(psb[pi][:, :], ('ps', pi),
                    [(panel_tile[:, k, :], uT[:, k, 512 * q:512 * (q + 1)]) for k in range(8)],
                    reads=[pkey])
            return pi

        ya_act, ya_blk = alloc_long([128, 8, S], BF16)
        with Scope(A, T) as pb:
            yac = sb("yac", [128, 8, S], stack=pb)
            with Scope(A, T) as pb1:
                NG = 2
                glu = [sb("glu%d" % i, [128, 32 + S], BF16, stack=pb1) for i in range(NG)]
                dgs = [sb("dgs%d" % i, [128, 31, 128], BF16, stack=pb1) for i in range(NG)]
                tsig = [sb("tsig%d" % i, [128, 512], stack=pb1) for i in range(2)]
                for i in range(NG):
                    T.op('pool', lambda e, i=i: e.memset(glu[i][:, 0:30], 0.0), writes=[('glu', i, 'pad')])
                it = 0
                for c in range(8):
                    r2 = c % NG
                    pan_v, pk_v = load_panel(w_in_d[:, 128 * c:128 * (c + 1)])
                    pan_g, pk_g = load_panel(w_in_d[:, 1024 + 128 * c:1024 + 128 * (c + 1)])
                    for k in range(31):
                        if k % 2 == 0:
                            T.op('dve', lambda e, r2=r2, c=c, k=k: e.tensor_scalar(
                                out=dgs[r2][:, k, :], in0=identb[:, :], scalar1=w_cvdw[:, c, k:k + 1], scalar2=None,
                                op0=ALU.mult), writes=[('dgs', r2, k)])
                        else:
                            T.op('act', lambda e, r2=r2, c=c, k=k: e.activation(
                                out=dgs[r2][:, k, :], in_=identb[:, :], func=AF.Copy, scale=w_cvdw[:, c, k:k + 1]),
                                writes=[('dgs', r2, k)])
                    for q in range(NQ):
                        i2 = it % 2
                        it += 1
                        pv = inproj(pan_v, pk_v, q)
                        pg = inproj(pan_g, pk_g, q)
                        T.op('act', lambda e, i2=i2, pg=pg, c=c: e.activation(
                            out=tsig[i2][:], in_=psb[pg][:, :], func=AF.Sigmoid,
                            bias=b_in_col[:, 8 + c:9 + c], scale=1.0),
                            reads=[('ps', pg)], writes=[('tsig', i2)])
                        T.op('dve', lambda e, i2=i2, pv=pv, c=c, r2=r2, q=q: e.scalar_tensor_tensor(
                            out=glu[r2][:, 30 + 512 * q:30 + 512 * (q + 1)], in0=psb[pv][:, :],
                            scalar=b_in_col[:, c:c + 1], in1=tsig[i2][:], op0=ALU.add, op1=ALU.mult),
                            reads=[('ps', pv), ('tsig', i2)], writes=[('glu', r2, q)])
                    rk = [('glu', r2, q) for q in range(NQ)] + [('glu', r2, 'pad')] + [('dgs', r2, k) for k in range(31)]
                    for q in range(NQ):
                        pc_ = getps()
                        mmgroup(psb[pc_][:, :], ('ps', pc_),
                                [(dgs[r2][:, k, :], glu[r2][:, k + 512 * q:k + 512 * (q + 1)]) for k in range(31)],
                                reads=rk)
                        T.op('act', lambda e, pc_=pc_, c=c, q=q: e.activation(
                            out=yac[:, c, 512 * q:512 * (q + 1)], in_=psb[pc_][:, :], func=AF.Identity,
                            bias=b_cvdw[:, c:c + 1], scale=1.0),
                            reads=[('ps', pc_)], writes=[('yac', c, q)])
            with Scope(A, T) as pb2:
                tsq = [sb("tsq%d" % i, [128, 512], stack=pb2) for i in range(3)]
                mean = sb("cmean", [128, 512], stack=pb2)
                var = sb("cvar", [128, 512], stack=pb2)
                rstd = sb("crstd", [128, 512], stack=pb2)
                nmr = sb("cnmr", [128, 512], stack=pb2)
                tn = [sb("tn%d" % i, [128, 512], stack=pb2) for i in range(3)]
                isq = 0
                itn = 0
                for q in range(NQ):
                    sl = slice(512 * q, 512 * (q + 1))
                    p_s = getps()
                    mmgroup(psb[p_s][:, :], ('ps', p_s), [(ones[:, :], yac[:, c, sl]) for c in range(8)],
                            reads=[])
                    p_q = getps()
                    for c in range(8):
                        j = isq % 3
                        isq += 1
                        T.op('act', lambda e, j=j, c=c, sl=sl: e.activation(
                            out=tsq[j][:], in_=yac[:, c, sl], func=AF.Square),
                            reads=[], writes=[('tsq', j)])
                        T.op('pe', lambda e, j=j, c=c, p_q=p_q: e.matmul(
                            psb[p_q][:, :], lhsT=ones[:, :], rhs=tsq[j][:], start=(c == 0), stop=(c == 7)),
                            reads=[('tsq', j)], writes=[('ps', p_q)], signal=True)
                    T.op('act', lambda e, p_s=p_s: e.activation(
                        out=mean[:], in_=psb[p_s][:, :], func=AF.Identity, scale=1.0 / D),
                        reads=[('ps', p_s)], writes=['cmean'])
                    T.op('dve', lambda e: e.tensor_tensor(out=var[:], in0=mean[:], in1=mean[:], op=ALU.mult),
                         reads=['cmean'], writes=['cvar'])
                    T.op('dve', lambda e, p_q=p_q: e.scalar_tensor_tensor(
                        out=var[:], in0=psb[p_q][:, :], scalar=1.0 / D, in1=var[:],
                        op0=ALU.mult, op1=ALU.subtract),
                        reads=[('ps', p_q), 'cvar'], writes=['cvar'])
                    T.op('act', lambda e: e.activation(out=var[:], in_=var[:], func=AF.Sqrt,
                                                       bias=epsc[:, 0:1], scale=1.0),
                         reads=['cvar'], writes=['cvar'])
                    T.op('dve', lambda e: e.reciprocal(out=rstd[:], in_=var[:]),
                         reads=['cvar'], writes=['crstd'])
                    T.op('dve', lambda e: e.scalar_tensor_tensor(
                        out=nmr[:], in0=mean[:], scalar=-1.0, in1=rstd[:], op0=ALU.mult, op1=ALU.mult),
                        reads=['cmean', 'crstd'], writes=['cnmr'])
                    for c in range(8):
                        j = itn % 3
                        itn += 1
                        T.op('dve', lambda e, j=j, c=c, sl=sl: e.tensor_tensor(
                            out=tn[j][:], in0=yac[:, c, sl], in1=rstd[:], op=ALU.mult),
                            reads=['crstd'], writes=[('tn', j)])
                        T.op('dve', lambda e, j=j: e.tensor_tensor(
                            out=tn[j][:], in0=tn[j][:], in1=nmr[:], op=ALU.add),
                            reads=[('tn', j), 'cnmr'], writes=[('tn', j)])
                        T.op('act', lambda e, j=j, c=c, sl=sl: e.activation(
                            out=ya_act[:, c, sl], in_=tn[j][:], func=AF.Silu,
                            bias=ln_cv_b[:, c:c + 1], scale=ln_cv_g[:, c:c + 1]),
                            reads=[('tn', j)], writes=[('ya_act', c, q)])
        if debug == 'ya_act':
            dbg_out(ya_act)
            return nc

        yb_act, yb_blk = alloc_long([128, 8, S], BF16)
        with Scope(A, T) as pa:
            wla = sb("wla", [128, 8, 128], BF16, stack=pa)
            wlx = sb("wlx", [128, 8, 128], BF16, stack=pa)
            with Scope(A, T) as pa0:
                wla_s = sb("wla_s", [128, 8, 128], stack=pa0)
                wlx_s = sb("wlx_s", [128, 8, 128], stack=pa0)
                T.dma('sp', wla_s[:, :, :], w_lru_a_d[:, :, :], writes=['wla_s'])
                T.dma('sp', wlx_s[:, :, :], w_lru_x_d[:, :, :], writes=['wlx_s'])
                T.op('dve', lambda e: e.tensor_copy(out=wla[:, :, :], in_=wla_s[:, :, :]),
                     reads=['wla_s'], writes=['wla'])
                T.op('act', lambda e: e.copy(out=wlx[:, :, :], in_=wlx_s[:, :, :]),
                     reads=['wlx_s'], writes=['wlx'])
            GSQ = float(np.sqrt(0.044715))
            bgs = sb("bgs", [128, 8], stack=pa)
            T.op('dve', lambda e: e.tensor_scalar(out=bgs[:], in0=b_in_col[:, 24:32], scalar1=GSQ, scalar2=None,
                                                  op0=ALU.mult), writes=['bgs'])
            NB = 1
            ybp = [sb("ybp%d" % i, [128, 3 + S], stack=pa) for i in range(NB)]
            ybc = [sb("ybc%d" % i, [128, S], stack=pa) for i in range(NB)]
            ybcb = [sb("ybcb%d" % i, [128, S], BF16, stack=pa) for i in range(NB)]
            hh = [sb("hh%d" % i, [128, 512], stack=pa) for i in range(2)]
            NTMP = 2
            tga = [sb("tga%d" % i, [128, 512], stack=pa) for i in range(NTMP)]
            tgx = [sb("tgx%d" % i, [128, 512], stack=pa) for i in range(NTMP)]
            ta = [sb("ta%d" % i, [128, 512], stack=pa) for i in range(NTMP)]
            tm = [sb("tm%d" % i, [128, 512], stack=pa) for i in range(NTMP)]
            tu = [sb("tu%d" % i, [128, 512], stack=pa) for i in range(NTMP)]
            tz = [sb("tz%d" % i, [128, 512], stack=pa) for i in range(NTMP)]
            tp = [sb("tp%d" % i, [128, 512], stack=pa) for i in range(NTMP)]
            tsg = [sb("tsg%d" % i, [128, 512], stack=pa) for i in range(NTMP)]
            for i in range(NB):
                T.op('pool', lambda e, i=i: e.memset(ybp[i][:, 0:3], 0.0), writes=[('ybp', i, 'pad')])
            it = 0
            hcnt = 0
            for c in range(8):
                r2 = c % NB
                pan, pkey = load_panel(w_in_d[:, 2048 + 128 * c:2048 + 128 * (c + 1)])
                for q in range(NQ):
                    pi = inproj(pan, pkey, q)
                    T.op('act', lambda e, pi=pi, q=q, r2=r2, c=c: e.activation(
                        out=ybp[r2][:, 3 + 512 * q:3 + 512 * (q + 1)], in_=psb[pi][:, :],
                        func=AF.Identity, bias=b_in_col[:, 16 + c:17 + c], scale=1.0),
                        reads=[('ps', pi)], writes=[('ybp', r2, q)])
                rk = [('ybp', r2, q) for q in range(NQ)] + [('ybp', r2, 'pad')]
                T.op('dve', lambda e, r2=r2, c=c: e.tensor_scalar(
                    out=ybc[r2][:, :], in0=ybp[r2][:, 0:S], scalar1=w_lconv[:, c, 0:1],
                    scalar2=b_lconv[:, c:c + 1], op0=ALU.mult, op1=ALU.add),
                    reads=rk, writes=[('ybc', r2)])
                for kk in range(1, 4):
                    T.op('dve', lambda e, r2=r2, c=c, kk=kk: e.scalar_tensor_tensor(
                        out=ybc[r2][:, :], in0=ybp[r2][:, kk:kk + S], scalar=w_lconv[:, c, kk:kk + 1],
                        in1=ybc[r2][:, :], op0=ALU.mult, op1=ALU.add),
                        reads=rk + [('ybc', r2)], writes=[('ybc', r2)])
                T.op('act', lambda e, r2=r2: e.copy(out=ybcb[r2][:, :], in_=ybc[r2][:, :]),
                     reads=[('ybc', r2)], writes=[('ybcb', r2)])
                pan_g, pkey_g = load_panel(w_in_d[:, 3072 + 128 * c:3072 + 128 * (c + 1)])
                for q in range(NQ):
                    i3 = it % NTMP
                    it += 1
                    sl = slice(512 * q, 512 * (q + 1))
                    pa_i = getps()
                    mmgroup(psb[pa_i][:, :], ('ps', pa_i), [(wla[:, c, :], ybcb[r2][:, sl])],
                            reads=[('ybcb', r2), 'wla'])
                    px_i = getps()
                    mmgroup(psb[px_i][:, :], ('ps', px_i), [(wlx[:, c, :], ybcb[r2][:, sl])],
                            reads=[('ybcb', r2), 'wlx'])
                    T.op('act', lambda e, i3=i3, pa_i=pa_i, c=c: e.activation(
                        out=tga[i3][:], in_=psb[pa_i][:, :], func=AF.Sigmoid,
                        bias=b_lru_a[:, c:c + 1], scale=1.0),
                        reads=[('ps', pa_i)], writes=[('tga', i3)])
                    T.op('act', lambda e, i3=i3, px_i=px_i, c=c: e.activation(
                        out=tgx[i3][:], in_=psb[px_i][:, :], func=AF.Sigmoid,
                        bias=b_lru_x[:, c:c + 1], scale=1.0),
                        reads=[('ps', px_i)], writes=[('tgx', i3)])
                    T.op('act', lambda e, i3=i3, c=c: e.activation(
                        out=ta[i3][:], in_=tga[i3][:], func=AF.Exp, scale=clam[:, c:c + 1]),
                        reads=[('tga', i3)], writes=[('ta', i3)])
                    T.op('act', lambda e, i3=i3, c=c: e.activation(
                        out=tm[i3][:], in_=tga[i3][:], func=AF.Exp, scale=clam2[:, c:c + 1]),
                        reads=[('tga', i3)], writes=[('tm', i3)])
                    T.op('act', lambda e, i3=i3: e.activation(
                        out=tm[i3][:], in_=tm[i3][:], func=AF.Sqrt, bias=1.0, scale=-1.0),
                        reads=[('tm', i3)], writes=[('tm', i3)])
                    if q == 0:
                        T.op('dve', lambda e, i3=i3: e.memset(tm[i3][:, 0:1], 1.0),
                             reads=[('tm', i3)], writes=[('tm', i3)])
                    T.op('dve', lambda e, i3=i3, r2=r2, sl=sl: e.tensor_tensor(
                        out=tu[i3][:], in0=tgx[i3][:], in1=ybc[r2][:, sl], op=ALU.mult),
                        reads=[('tgx', i3), ('ybc', r2)], writes=[('tu', i3)])
                    T.op('dve', lambda e, i3=i3: e.tensor_tensor(
                        out=tu[i3][:], in0=tu[i3][:], in1=tm[i3][:], op=ALU.mult),
                        reads=[('tu', i3), ('tm', i3)], writes=[('tu', i3)])
                    hcur = hcnt % 2
                    hprev = (hcnt - 1) % 2
                    hcnt += 1
                    if q == 0:
                        T.op('dve', lambda e, i3=i3, hcur=hcur: e.tensor_tensor_scan(
                            out=hh[hcur][:], data0=ta[i3][:], data1=tu[i3][:], initial=0.0,
                            op0=ALU.mult, op1=ALU.add),
                            reads=[('ta', i3), ('tu', i3)], writes=[('hh', hcur)])
                    else:
                        T.op('dve', lambda e, i3=i3, hcur=hcur, hprev=hprev: e.tensor_tensor_scan(
                            out=hh[hcur][:], data0=ta[i3][:], data1=tu[i3][:],
                            initial=hh[hprev][:, 511:512], op0=ALU.mult, op1=ALU.add),
                            reads=[('ta', i3), ('tu', i3), ('hh', hprev)], writes=[('hh', hcur)])
                    pg_i = inproj(pan_g, pkey_g, q)
                    T.op('act', lambda e, i3=i3, pg_i=pg_i, c=c: e.activation(
                        out=tz[i3][:], in_=psb[pg_i][:, :], func=AF.Identity,
                        bias=b_in_col[:, 24 + c:25 + c], scale=1.0),
                        reads=[('ps', pg_i)], writes=[('tz', i3)])
                    T.op('act', lambda e, i3=i3, pg_i=pg_i, c=c: e.activation(
                        out=tp[i3][:], in_=psb[pg_i][:, :], func=AF.Square,
                        bias=bgs[:, c:c + 1], scale=GSQ),
                        reads=[('ps', pg_i)], writes=[('tp', i3)])
                    T.op('dve', lambda e, i3=i3: e.scalar_tensor_tensor(
                        out=tp[i3][:], in0=tp[i3][:], scalar=1.0, in1=tz[i3][:], op0=ALU.add, op1=ALU.mult),
                        reads=[('tp', i3), ('tz', i3)], writes=[('tp', i3)])
                    T.op('act', lambda e, i3=i3: e.activation(
                        out=tsg[i3][:], in_=tp[i3][:], func=AF.Sigmoid,
                        scale=float(2.0 * np.sqrt(2.0 / np.pi))),
                        reads=[('tp', i3)], writes=[('tsg', i3)])
                    T.op('dve', lambda e, i3=i3: e.tensor_tensor(
                        out=tsg[i3][:], in0=tsg[i3][:], in1=tz[i3][:], op=ALU.mult),
                        reads=[('tsg', i3), ('tz', i3)], writes=[('tsg', i3)])
                    T.op('dve', lambda e, i3=i3, hcur=hcur, c=c, sl=sl: e.tensor_tensor(
                        out=yb_act[:, c, sl], in0=hh[hcur][:], in1=tsg[i3][:], op=ALU.mult),
                        reads=[('hh', hcur), ('tsg', i3)], writes=[('yb_act', c, q)])
        if debug == 'yb_act':
            dbg_out(yb_act)
            return nc

        merged, merged_blk = alloc_long([128, 8, S], BF16)
        with Scope(A, T) as pc:
            tga_ = [sb("mga%d" % i, [128, 512], stack=pc) for i in range(2)]
            tgb_ = [sb("mgb%d" % i, [128, 512], stack=pc) for i in range(2)]
            tm1 = [sb("mm1%d" % i, [128, 512], stack=pc) for i in range(2)]
            tm2 = [sb("mm2%d" % i, [128, 512], stack=pc) for i in range(2)]
            it = 0
            for m in range(8):
                pan_a, pk_a = load_panel(w_cv_out_d[:, 128 * m:128 * (m + 1)])
                pan_b, pk_b = load_panel(w_lru_out_d[:, 128 * m:128 * (m + 1)], cast_eng='act')
                pan_ga, pk_ga = load_panel(w_in_d[:, 4096 + 128 * m:4096 + 128 * (m + 1)])
                pan_gb, pk_gb = load_panel(w_in_d[:, 5120 + 128 * m:5120 + 128 * (m + 1)], cast_eng='act')
                for q in range(NQ):
                    i2 = it % 2
                    it += 1
                    sl = slice(512 * q, 512 * (q + 1))
                    p1i = getps()
                    mmgroup(psb[p1i][:, :], ('ps', p1i),
                            [(pan_a[:, k, :], ya_act[:, k, sl]) for k in range(8)], reads=[pk_a])
                    p2i = getps()
                    mmgroup(psb[p2i][:, :], ('ps', p2i),
                            [(pan_b[:, k, :], yb_act[:, k, sl]) for k in range(8)], reads=[pk_b])
                    p3i = inproj(pan_ga, pk_ga, q)
                    p4i = inproj(pan_gb, pk_gb, q)
                    T.op('act', lambda e, i2=i2, p3i=p3i, m=m: e.activation(
                        out=tga_[i2][:], in_=psb[p3i][:, :], func=AF.Sigmoid,
                        bias=b_in_col[:, 32 + m:33 + m], scale=1.0),
                        reads=[('ps', p3i)], writes=[('mga', i2)])
                    T.op('act', lambda e, i2=i2, p4i=p4i, m=m: e.activation(
                        out=tgb_[i2][:], in_=psb[p4i][:, :], func=AF.Sigmoid,
                        bias=b_in_col[:, 40 + m:41 + m], scale=1.0),
                        reads=[('ps', p4i)], writes=[('mgb', i2)])
                    T.op('dve', lambda e, i2=i2, p1i=p1i: e.tensor_tensor(
                        out=tm1[i2][:], in0=psb[p1i][:, :], in1=tga_[i2][:], op=ALU.mult),
                        reads=[('ps', p1i), ('mga', i2)], writes=[('mm1', i2)])
                    T.op('dve', lambda e, i2=i2, p2i=p2i: e.tensor_tensor(
                        out=tm2[i2][:], in0=psb[p2i][:, :], in1=tgb_[i2][:], op=ALU.mult),
                        reads=[('ps', p2i), ('mgb', i2)], writes=[('mm2', i2)])
                    T.op('dve', lambda e, i2=i2, m=m, sl=sl: e.tensor_tensor(
                        out=merged[:, m, sl], in0=tm1[i2][:], in1=tm2[i2][:], op=ALU.add),
                        reads=[('mm1', i2), ('mm2', i2)], writes=[('merged', m, q)])
        p2.__exit__(None, None, None)
        T.barrier()
        for blk in (uT_blk, ya_blk, yb_blk):
            A.release(blk)
        A.commit()

        if debug == 'merged':
            dbg_out(merged)
            return nc

        OH, OH_blk = alloc_long([128, NT, 4, NE], F32)
        with Scope(A, T) as pd:
            wob = sb("wob", [128, 8, D], BF16, stack=pd)
            with Scope(A, T) as pd0:
                wos = [sb("wos%d" % i, [128, D], stack=pd0) for i in range(2)]
                for k in range(8):
                    s1 = k % 2
                    T.dma('sp', wos[s1][:], w_o_d[128 * k:128 * (k + 1), :], writes=[('wos', s1)])
                    if k % 2 == 0:
                        T.op('dve', lambda e, k=k, s1=s1: e.tensor_copy(out=wob[:, k, :], in_=wos[s1][:]),
                             reads=[('wos', s1)], writes=[('wob', k)])
                    else:
                        T.op('act', lambda e, k=k, s1=s1: e.copy(out=wob[:, k, :], in_=wos[s1][:]),
                             reads=[('wos', s1)], writes=[('wob', k)])
            l1g = sb("l1g", [128, D], stack=pd)
            l1b = sb("l1b", [128, D], stack=pd)
            brt = sb("brt", [128, NE], stack=pd)
            g1row = sb("g1row2", [128, D], stack=pd)
            g1bo = sb("g1bo2", [128, D], stack=pd)
            T.dma('sp', l1g[:], ln1_g_d[0:1, :].partition_broadcast(128), writes=['l1g'])
            T.dma('sp', l1b[:], ln1_b_d[0:1, :].partition_broadcast(128), writes=['l1b'])
            T.dma('sp', brt[:], b_router_d[0:1, :].partition_broadcast(128), writes=['brt'])
            T.dma('sp', g1row[:], g1row_d[:, :], writes=['g1row'])
            T.dma('sp', g1bo[:], g1bo_d[:, :], writes=['g1bo'])
            sh2row = sb("sh2row2", [128, D], stack=pd)
            sc2prow = sb("sc2prow2", [128, D], stack=pd)
            T.dma('sp', sh2row[:], sh2row_d[:, :], writes=['sh2row'])
            T.dma('sp', sc2prow[:], sc2prow_d[:, :], writes=['sc2prow'])
            u2m = [sb("u2m%d" % i, [128, D], stack=pd) for i in range(2)]
            u2k = [sb("u2k%d" % i, [128, D], BF16, stack=pd) for i in range(2)]
            e4 = [sb("e4%d" % i, [128, 8], stack=pd) for i in range(2)]
            xt = [sb("xr%d" % i, [128, D], stack=pd) for i in range(2)]
            hp = [sb("hp%d" % i, [128, D], stack=pd) for i in range(2)]
            tq = [sb("tq%d" % i, [128, D], stack=pd) for i in range(2)]
            h1t = [sb("h1t%d" % i, [128, D], stack=pd) for i in range(2)]
            xn2 = [sb("xn2%d" % i, [128, D], stack=pd) for i in range(2)]
            u2f = [sb("u2f%d" % i, [128, 8, 128], stack=pd) for i in range(2)]
            stt = [sb("stt2%d" % i, [128, 12], stack=pd) for i in range(4)]
            mvt = [sb("mvt2%d" % i, [128, 8], stack=pd) for i in range(4)]
            lg = [sb("lg%d" % i, [128, NE], stack=pd) for i in range(2)]
            mx8 = [sb("mx8%d" % i, [128, 8], stack=pd) for i in range(2)]
            ex = [sb("ex%d" % i, [128, NE], stack=pd) for i in range(2)]
            msk = [sb("msk%d" % i, [128, NE], stack=pd) for i in range(2)]
            ssum = [sb("ssum%d" % i, [128, 2], stack=pd) for i in range(2)]
            for tt in range(NT):
                b = tt % 2
                ts_ = slice(128 * tt, 128 * (tt + 1))
                T.dma('sp', xt[b][:], x_d[ts_, :], writes=[('xr', b)])
                pm_ = []
                for n in range(2):
                    pi = getps()
                    mmgroup(psb[pi][:, :], ('ps', pi),
                            [(merged[:, k, ts_], wob[:, k, 512 * n:512 * (n + 1)]) for k in range(8)],
                            reads=[('wob', k) for k in range(8)])
                    pm_.append(pi)
                T.op('dve', lambda e, b=b: e.scalar_tensor_tensor(
                    out=hp[b][:], in0=xt[b][:], scalar=ALPHA, in1=g1bo[:], op0=ALU.mult, op1=ALU.add),
                    reads=[('xr', b), 'g1bo'], writes=[('hp', b)])
                for n in range(2):
                    T.op('dve', lambda e, b=b, n=n, pi=pm_[n]: e.tensor_tensor(
                        out=tq[b][:, 512 * n:512 * (n + 1)], in0=psb[pi][:, :],
                        in1=g1row[:, 512 * n:512 * (n + 1)], op=ALU.mult),
                        reads=[('ps', pm_[n]), 'g1row'], writes=[('tq', b, n)])
                T.op('dve', lambda e, b=b: e.tensor_tensor(out=hp[b][:], in0=hp[b][:], in1=tq[b][:], op=ALU.add),
                     reads=[('hp', b), ('tq', b, 0), ('tq', b, 1)], writes=[('hp', b)])
                rs, nm, keys = layernorm_rows(hp[b], ('hp', b), h1t[b], ('h1t', b), mvt[b], ('a', b))
                T.op('act', lambda e, b=b, rs=rs, nm=nm: e.activation(
                    out=h1t[b][:], in_=hp[b][:], func=AF.Identity, bias=nm, scale=rs),
                    reads=[('hp', b)] + keys, writes=[('h1t', b)])
                T.op('dve', lambda e, b=b: e.tensor_tensor(out=h1t[b][:], in0=h1t[b][:], in1=l1g[:], op=ALU.mult),
                     reads=[('h1t', b), 'l1g'], writes=[('h1t', b)])
                T.op('dve', lambda e, b=b: e.tensor_tensor(out=h1t[b][:], in0=h1t[b][:], in1=l1b[:], op=ALU.add),
                     reads=[('h1t', b), 'l1b'], writes=[('h1t', b)])
                T.dma('sp', h1_d[ts_, :], h1t[b][:], reads=[('h1t', b)])
                rs2, nm2, keys2 = layernorm_rows(h1t[b], ('h1t', b), xn2[b], ('xn2', b), mvt[2 + b], ('b', b))
                T.op('act', lambda e, b=b, rs2=rs2, nm2=nm2: e.activation(
                    out=xn2[b][:], in_=h1t[b][:], func=AF.Identity, bias=nm2, scale=rs2),
                    reads=[('h1t', b)] + keys2, writes=[('xn2', b)])
                for half in range(2):
                    pi = getps()
                    transpose4(pi, xn2[b], half * 4, ('xn2', b))
                    for q in range(4):
                        k = half * 4 + q
                        if half == 0:
                            T.op('dve', lambda e, pi=pi, q=q, k=k, b=b: e.tensor_scalar(
                                out=u2f[b][:, k, :], in0=psb[pi][:, 128 * q:128 * (q + 1)],
                                scalar1=sc2p[:, k:k + 1], scalar2=mod_col[:, 24 + k:25 + k],
                                op0=ALU.mult, op1=ALU.add),
                                reads=[('ps', pi)], writes=[('u2f', b, k)])
                        else:
                            T.op('act', lambda e, pi=pi, q=q, k=k, b=b: e.activation(
                                out=u2f[b][:, k, :], in_=psb[pi][:, 128 * q:128 * (q + 1)], func=AF.Identity,
                                bias=mod_col[:, 24 + k:25 + k], scale=sc2p[:, k:k + 1]),
                                reads=[('ps', pi)], writes=[('u2f', b, k)])
                T.op('dve', lambda e, b=b: e.tensor_tensor(out=u2m[b][:], in0=xn2[b][:], in1=sc2prow[:], op=ALU.mult),
                     reads=[('xn2', b), 'sc2prow'], writes=[('u2m', b)])
                T.op('dve', lambda e, b=b: e.tensor_tensor(out=u2k[b][:], in0=u2m[b][:], in1=sh2row[:], op=ALU.add),
                     reads=[('u2m', b), 'sh2row'], writes=[('u2k', b)])
                T.dma('sp', u2tok_d[ts_, :], u2k[b][:], reads=[('u2k', b)])
                pl = getps()
                mmgroup(psb[pl][:, 0:NE], ('ps', pl),
                        [(u2f[b][:, k, :], w_router[:, k, :]) for k in range(8)],
                        reads=[('u2f', b, k) for k in range(8)])
                T.op('dve', lambda e, b=b, pl=pl: e.tensor_tensor(
                    out=lg[b][:], in0=psb[pl][:, 0:NE], in1=brt[:], op=ALU.add),
                    reads=[('ps', pl), 'brt'], writes=[('lg', b)])
                T.op('dve', lambda e, b=b: e.max(out=mx8[b][:], in_=lg[b][:]),
                     reads=[('lg', b)], writes=[('mx8', b)])
                T.op('dve', lambda e, b=b: e.tensor_scalar(
                    out=msk[b][:], in0=lg[b][:], scalar1=mx8[b][:, 3:4], scalar2=None, op0=ALU.is_ge),
                    reads=[('lg', b), ('mx8', b)], writes=[('msk', b)])
                T.op('dve', lambda e, b=b: e.tensor_scalar(
                    out=ex[b][:], in0=lg[b][:], scalar1=mx8[b][:, 0:1], scalar2=None, op0=ALU.subtract),
                    reads=[('lg', b), ('mx8', b)], writes=[('ex', b)])
                T.op('act', lambda e, b=b: e.activation(out=ex[b][:], in_=ex[b][:], func=AF.Exp),
                     reads=[('ex', b)], writes=[('ex', b)])
                T.op('dve', lambda e, b=b: e.tensor_tensor(out=ex[b][:], in0=ex[b][:], in1=msk[b][:], op=ALU.mult),
                     reads=[('ex', b), ('msk', b)], writes=[('ex', b)])
                T.op('dve', lambda e, b=b: e.tensor_reduce(out=ssum[b][:, 0:1], in_=ex[b][:], axis=AX.X, op=ALU.add),
                     reads=[('ex', b)], writes=[('ssum', b)])
                T.op('dve', lambda e, b=b: e.reciprocal(out=ssum[b][:, 1:2], in_=ssum[b][:, 0:1]),
                     reads=[('ssum', b)], writes=[('ssum', b)])
                T.op('dve', lambda e, b=b, tt=tt: e.tensor_scalar(
                    out=G[:, tt, :], in0=ex[b][:], scalar1=ssum[b][:, 1:2], scalar2=None, op0=ALU.mult),
                    reads=[('ex', b), ('ssum', b)], writes=[('G', tt)])
                for j in range(4):
                    T.op('dve', lambda e, b=b, tt=tt, j=j: e.tensor_scalar(
                        out=OH[:, tt, j, :], in0=lg[b][:], scalar1=mx8[b][:, j:j + 1], scalar2=None,
                        op0=ALU.is_equal), reads=[('lg', b), ('mx8', b)], writes=[('OH', tt, j)])
                T.op('dve', lambda e, b=b: e.tensor_scalar(
                    out=e4[b][:, 6:7], in0=mx8[b][:, 0:1], scalar1=-1.0, scalar2=None, op0=ALU.mult),
                    reads=[('mx8', b)], writes=[('e4n', b)])
                T.op('act', lambda e, b=b: e.activation(out=e4[b][:, 0:4], in_=mx8[b][:, 0:4], func=AF.Exp,
                                                        bias=e4[b][:, 6:7], scale=1.0),
                     reads=[('mx8', b), ('e4n', b)], writes=[('e4', b)])
                T.op('dve', lambda e, b=b: e.tensor_reduce(out=e4[b][:, 4:5], in_=e4[b][:, 0:4], axis=AX.X, op=ALU.add),
                     reads=[('e4', b)], writes=[('e4s', b)])
                T.op('dve', lambda e, b=b: e.reciprocal(out=e4[b][:, 5:6], in_=e4[b][:, 4:5]),
                     reads=[('e4s', b)], writes=[('e4r', b)])
                T.op('dve', lambda e, b=b, tt=tt: e.tensor_scalar(
                    out=GT[:, tt, :], in0=e4[b][:, 0:4], scalar1=e4[b][:, 5:6], scalar2=float(1.0 / 1.702),
                    op0=ALU.mult, op1=ALU.mult), reads=[('e4', b), ('e4r', b)], writes=[('GT', tt)])
        free_long(merged_blk)

        if debug == 'h1':
            with Scope(A, T) as dd:
                for tt in range(NT):
                    dt_ = sb("dbgt", [128, D], stack=dd)
                    T.dma('sp', dt_[:], h1_d[128 * tt:128 * (tt + 1), :], writes=[('dbgt', tt)])
                    T.dma('sp', dbg_d[:, D * tt:D * (tt + 1)], dt_[:], reads=[('dbgt', tt)])
            T.finish()
            return nc
        if debug == 'G':
            with Scope(A, T) as dd:
                dt_ = sb("dbgt", [128, 8 * S], stack=dd)
                T.op('dve', lambda e: e.memset(dt_[:], 0.0), writes=['dbgt'])
                T.op('dve', lambda e: e.tensor_copy(out=dt_[:, 0:NT * NE], in_=G[:, :, :].rearrange("p t e -> p (t e)")),
                     reads=['dbgt'], writes=['dbgt'])
                T.op('dve', lambda e: e.tensor_copy(out=dt_[:, 1024:1024 + NT * 4], in_=GT[:, :, :].rearrange("p t j -> p (t j)")),
                     reads=['dbgt'], writes=['dbgt'])
                T.dma('sp', dbg_d[:, :], dt_[:, :], reads=['dbgt'])
            T.finish()
            return nc

        with Scope(A, T) as pr:
            MK = sb("MK", [128, NT, NE], stack=pr)
            T.op('dve', lambda e: e.tensor_tensor(out=MK[:, :, :], in0=OH[:, :, 0, :], in1=OH[:, :, 1, :], op=ALU.add),
                 writes=['MK'])
            T.op('dve', lambda e: e.tensor_tensor(out=MK[:, :, :], in0=MK[:, :, :], in1=OH[:, :, 2, :], op=ALU.add),
                 reads=['MK'], writes=['MK'])
            T.op('dve', lambda e: e.tensor_tensor(out=MK[:, :, :], in0=MK[:, :, :], in1=OH[:, :, 3, :], op=ALU.add),
                 reads=['MK'], writes=['MK'])
            erow = sb("erow", [128, NE], stack=pr)
            ecol = sb("ecol", [128, 1], stack=pr)
            iou = sb("iou", [128, 16], stack=pr)
            iod = sb("iod", [128, 8], stack=pr)
            r31row = sb("r31row", [128, NE], stack=pr)
            r31col = sb("r31col", [128, 1], stack=pr)
            T.op('pool', lambda e: e.iota(out=erow[:], pattern=[[1, NE]], base=0, channel_multiplier=0,
                                          allow_small_or_imprecise_dtypes=True), writes=['erow'])
            T.op('pool', lambda e: e.iota(out=ecol[:], pattern=[[0, 1]], base=0, channel_multiplier=1,
                                          allow_small_or_imprecise_dtypes=True), writes=['ecol'])
            T.op('pool', lambda e: e.iota(out=iou[:], pattern=[[256, 8], [1, 2]], base=0, channel_multiplier=2,
                                          allow_small_or_imprecise_dtypes=True), writes=['iou'])
            T.op('pool', lambda e: e.iota(out=iod[:], pattern=[[128, 8]], base=0, channel_multiplier=1,
                                          allow_small_or_imprecise_dtypes=True), writes=['iod'])
            T.op('dve', lambda e: e.tensor_scalar(out=r31row[:], in0=erow[:], scalar1=-1.0, scalar2=31.0,
                                                  op0=ALU.mult, op1=ALU.add), reads=['erow'], writes=['r31row'])
            T.op('dve', lambda e: e.tensor_scalar(out=r31col[:], in0=ecol[:], scalar1=-1.0, scalar2=31.0,
                                                  op0=ALU.mult, op1=ALU.add), reads=['ecol'], writes=['r31col'])
            pc_ = getps()
            mmgroup(psb[pc_][0:NE, 0:1], ('ps', pc_), [(MK[:, tt, :], ones[:, 0:1]) for tt in range(NT)], reads=['MK'])
            pw_ = getps()
            mmgroup(psb[pw_][0:NE, 0:NE], ('ps', pw_), [(ones[:, 0:NE], MK[:, tt, :]) for tt in range(NT)], reads=['MK'])
            keyc = sb("keyc", [128, 1], stack=pr)
            keyr = sb("keyr", [128, NE], stack=pr)
            gtm = sb("gtm", [128, NE], stack=pr)
            rankc = sb("rankc", [128, 1], stack=pr)
            Pm = sb("Pm", [128, NE], stack=pr)
            cumrow = sb("cumrow", [128, NE], stack=pr)
            basec = sb("basec", [128, 1], stack=pr)
            diagB = sb("diagB", [128, NE], stack=pr)
            ecolb = sb("ecolb", [128, 128], stack=pr)
            pib = sb("pib", [128, NE], stack=pr)
            Um = sb("Um", [128, 128], stack=pr)
            T.op('dve', lambda e: e.scalar_tensor_tensor(out=keyc[0:NE, :], in0=psb[pc_][0:NE, 0:1], scalar=32.0,
                                                         in1=r31col[0:NE, :], op0=ALU.mult, op1=ALU.add),
                 reads=[('ps', pc_), 'r31col'], writes=['keyc'])
            T.op('dve', lambda e: e.scalar_tensor_tensor(out=keyr[0:NE, :], in0=psb[pw_][0:NE, 0:NE], scalar=32.0,
                                                         in1=r31row[0:NE, :], op0=ALU.mult, op1=ALU.add),
                 reads=[('ps', pw_), 'r31row'], writes=['keyr'])
            T.op('dve', lambda e: e.tensor_scalar(out=gtm[0:NE, :], in0=keyr[0:NE, :], scalar1=keyc[0:NE, 0:1],
                                                  scalar2=None, op0=ALU.is_gt),
                 reads=['keyr', 'keyc'], writes=['gtm'])
            T.op('dve', lambda e: e.tensor_reduce(out=rankc[0:NE, :], in_=gtm[0:NE, :], axis=AX.X, op=ALU.add),
                 reads=['gtm'], writes=['rankc'])
            T.op('dve', lambda e: e.tensor_scalar(out=Pm[0:NE, :], in0=erow[0:NE, :], scalar1=rankc[0:NE, 0:1],
                                                  scalar2=None, op0=ALU.is_equal),
                 reads=['erow', 'rankc'], writes=['Pm'])
            for r in range(NE):
                T.op('dve', lambda e, r=r: e.memset(cumrow[:, r:r + 1], float(CUM[r])), writes=[('cumrow', r)])
            T.op('dve', lambda e: e.tensor_tensor(out=gtm[0:NE, :], in0=Pm[0:NE, :], in1=cumrow[0:NE, :], op=ALU.mult),
                 reads=['Pm', 'gtm'] + [('cumrow', r) for r in range(NE)], writes=['gtm'])
            T.op('dve', lambda e: e.tensor_reduce(out=basec[0:NE, :], in_=gtm[0:NE, :], axis=AX.X, op=ALU.add),
                 reads=['gtm'], writes=['basec'])
            T.op('dve', lambda e: e.tensor_scalar(out=diagB[0:NE, :], in0=ident[0:NE, 0:NE], scalar1=basec[0:NE, 0:1],
                                                  scalar2=None, op0=ALU.mult), reads=['basec'], writes=['diagB'])
            T.op('dve', lambda e: e.tensor_copy(out=ecolb[0:NE, :], in_=ecol[0:NE, 0:1].to_broadcast([NE, 128])),
                 reads=['ecol'], writes=['ecolb'])
            pp_ = getps()
            mmgroup(psb[pp_][:, 0:NE], ('ps', pp_), [(ecolb[0:NE, :], Pm[0:NE, :])], reads=['ecolb', 'Pm'])
            T.op('dve', lambda e: e.tensor_copy(out=pib[:], in_=psb[pp_][:, 0:NE]), reads=[('ps', pp_)], writes=['pib'])
            T.op('pool', lambda e: e.affine_select(out=Um[:], in_=ones[:], pattern=[[1, 128]], compare_op=ALU.is_gt,
                                                   fill=0.0, base=0, channel_multiplier=-1), writes=['Um'])
            slotf = sb("slotf", [128, NT * 4], stack=pr)
            tsl = [sb("tsl%d" % i, [128, NE], stack=pr) for i in range(2)]
            isl = 0
            for tt in range(NT):
                ps_ = getps()
                pairs = [(ones[:, :], MK[:, tp, :]) for tp in range(tt)] + [(Um[:, :], MK[:, tt, :])] + \
                        [(ones[0:NE, :], diagB[0:NE, :])]
                mmgroup(psb[ps_][:, 0:NE], ('ps', ps_), pairs, reads=['MK', 'Um', 'diagB'])
                for j in range(4):
                    i2 = isl % 2
                    isl += 1
                    T.op('dve', lambda e, i2=i2, tt=tt, j=j, ps_=ps_: e.tensor_tensor(
                        out=tsl[i2][:], in0=psb[ps_][:, 0:NE], in1=OH[:, tt, j, :], op=ALU.mult),
                        reads=[('ps', ps_)], writes=[('tsl', i2)])
                    T.op('dve', lambda e, i2=i2, tt=tt, j=j: e.tensor_reduce(
                        out=slotf[:, 4 * tt + j:4 * tt + j + 1], in_=tsl[i2][:], axis=AX.X, op=ALU.add),
                        reads=[('tsl', i2)], writes=[('slotf', tt, j)])
            T.op('dve', lambda e: e.tensor_copy(out=SLOT[:, :], in_=slotf[:, :]),
                 reads=[('slotf', tt, j) for tt in range(NT) for j in range(4)], writes=['SLOT'])
            wif = sb("wif", [128, NE, 16], stack=pr)
            wdf = sb("wdf", [128, NE, 8], stack=pr)
            bif = sb("bif", [128, NE], stack=pr)
            pib2 = sb("pib2", [128, NE], stack=pr)
            T.op('dve', lambda e: e.tensor_scalar(out=pib2[:], in0=pib[:], scalar1=2048.0, scalar2=None, op0=ALU.mult),
                 reads=['pib'], writes=['pib2'])
            for r in range(NE):
                T.op('dve', lambda e, r=r: e.tensor_scalar(out=wif[:, r, :], in0=iou[:], scalar1=pib2[:, r:r + 1],
                                                           scalar2=None, op0=ALU.add),
                     reads=['iou', 'pib2'], writes=[('wif', r)])
            T.op('dve', lambda e: e.tensor_copy(out=WIU[:, :, :], in_=wif[:, :, :]),
                 reads=[('wif', r) for r in range(NE)], writes=['WIU'])
            T.op('dve', lambda e: e.tensor_scalar(out=pib2[:], in0=pib[:], scalar1=1024.0, scalar2=None, op0=ALU.mult),
                 reads=['pib', 'pib2'] + [('wif', r) for r in range(NE)], writes=['pib2'])
            for r in range(NE):
                T.op('dve', lambda e, r=r: e.tensor_scalar(out=wdf[:, r, :], in0=iod[:], scalar1=pib2[:, r:r + 1],
                                                           scalar2=None, op0=ALU.add),
                     reads=['iod', 'pib2'], writes=[('wdf', r)])
            T.op('dve', lambda e: e.tensor_copy(out=WID[:, :, :], in_=wdf[:, :, :]),
                 reads=[('wdf', r) for r in range(NE)], writes=['WID'])
            T.op('dve', lambda e: e.scalar_tensor_tensor(out=bif[:], in0=pib[:], scalar=128.0,
                                                         in1=ecol[:, 0:1].to_broadcast([128, NE]),
                                                         op0=ALU.mult, op1=ALU.add),
                 reads=['pib', 'ecol'], writes=['bif'])
            T.op('dve', lambda e: e.tensor_copy(out=BIU[:, :], in_=bif[:, :]), reads=['bif'], writes=['BIU'])
            utile = [sb("utile%d" % i, [128, D], BF16, stack=pr) for i in range(2)]
            for tt in range(NT):
                b = tt % 2
                T.dma('sp', utile[b][:], u2tok_d[128 * tt:128 * (tt + 1), :], writes=[('utile', b)])
                for j in range(4):
                    T.dma('pool', xs_d[:, :], utile[b][:, :], reads=[('utile', b), 'SLOT'], writes=[('xs', tt, j)],
                          out_offset=bass.IndirectOffsetOnAxis(ap=SLOT[:, 4 * tt + j:4 * tt + j + 1], axis=0))
        free_long(OH_blk)
        if debug == 'route':
            with Scope(A, T) as dd:
                dt_ = sb("dbgt", [128, 2048], stack=dd)
                T.op('dve', lambda e: e.memset(dt_[:], 0.0), writes=['dbgt'])
                T.op('dve', lambda e: e.tensor_copy(out=dt_[:, 0:64], in_=SLOT[:, :]), reads=['dbgt'], writes=['dbgt'])
                T.op('dve', lambda e: e.tensor_copy(out=dt_[:, 64:64 + 512], in_=WIU[:, :, :].rearrange("p r j -> p (r j)")),
                     reads=['dbgt'], writes=['dbgt'])
                T.op('dve', lambda e: e.tensor_copy(out=dt_[:, 576:576 + 256], in_=WID[:, :, :].rearrange("p r j -> p (r j)")),
                     reads=['dbgt'], writes=['dbgt'])
                T.op('dve', lambda e: e.tensor_copy(out=dt_[:, 832:832 + 32], in_=BIU[:, :]), reads=['dbgt'], writes=['dbgt'])
                T.op('dve', lambda e: e.tensor_copy(out=dt_[:, 896:896 + 64], in_=GT[:, :, :].rearrange("p t j -> p (t j)")),
                     reads=['dbgt'], writes=['dbgt'])
                T.dma('sp', dbg_d[:, 0:2048], dt_[:, :], reads=['dbgt'])
            T.finish()
            return nc

        with Scope(A, T) as pe_:
            ugT = [sb("ugT0", [128, 8, 2048], BF16, stack=pe_), sb("ugT1", [128, 8, 512], BF16, stack=pe_)]
            actT = sb("actT", [128, 8, 2048], BF16, stack=pe_)
            NHB = 3
            wugh = [sb("wugh%d" % i, [128, 8, 4, 128], BF16, stack=pe_) for i in range(NHB)]
            wulh = [sb("wulh%d" % i, [128, 8, 4, 128], BF16, stack=pe_) for i in range(NHB)]
            wdb = sb("wdb", [128, 8, D], BF16, stack=pe_)
            NSTG = 4
            NXS = 6
            stg = [sb("stg%d" % i, [128, D], stack=pe_) for i in range(NSTG)]
            xst = [sb("xst%d" % i, [128, D], BF16, stack=pe_) for i in range(NXS)]
            osb = [sb("osb%d" % i, [128, D], stack=pe_) for i in range(2)]
            bu = [sb("bu%d" % i, [128, 16], stack=pe_) for i in range(2)]
            b7 = [sb("b7%d" % i, [128, 8], stack=pe_) for i in range(2)]
            b1 = [sb("b1%d" % i, [128, 8], stack=pe_) for i in range(2)]
            NZ = 2
            zg = [sb("zg%d" % i, [128, 512], stack=pe_) for i in range(NZ)]
            sg = [sb("sg%d" % i, [128, 512], stack=pe_) for i in range(NZ)]
            zl = [sb("zl%d" % i, [128, 512], stack=pe_) for i in range(NZ)]
            cnts = {'stg': 0, 'xst': 0, 'osb': 0, 'z': 0, 'cast': 0}

            def next_stg():
                s = cnts['stg'] % NSTG
                cnts['stg'] += 1
                return s

            def cast_eng():
                cnts['cast'] += 1
                return 'act' if cnts['cast'] % 2 == 0 else 'dve'

            def load_half(r, h, hb):
                for k in range(8):
                    s = next_stg()
                    T.dma('pool', stg[s][:, :], w_up2[:, :], reads=['WIU'], writes=[('stg', s)],
                          in_offset=bass.IndirectOffsetOnAxis(ap=WIU[:, r, 2 * k + h:2 * k + h + 1], axis=0))
                    v = stg[s][:, :].rearrange("p (a b c) -> p a b c", a=4, b=128, c=2)
                    ce = cast_eng()
                    if ce == 'act':
                        T.op('act', lambda e, v=v, k=k: e.copy(out=wugh[hb][:, k, :, :], in_=v[:, :, :, 0]),
                             reads=[('stg', s)], writes=[('wugh', hb, k)])
                        T.op('dve', lambda e, v=v, k=k: e.tensor_copy(out=wulh[hb][:, k, :, :], in_=v[:, :, :, 1]),
                             reads=[('stg', s)], writes=[('wulh', hb, k)])
                    else:
                        T.op('dve', lambda e, v=v, k=k: e.tensor_copy(out=wugh[hb][:, k, :, :], in_=v[:, :, :, 0]),
                             reads=[('stg', s)], writes=[('wugh', hb, k)])
                        T.op('act', lambda e, v=v, k=k: e.copy(out=wulh[hb][:, k, :, :], in_=v[:, :, :, 1]),
                             reads=[('stg', s)], writes=[('wulh', hb, k)])

            def load_bias(r):
                rb = posof[r] % 2
                T.dma('pool', bu[rb][:, :], b_up_d[:, :], reads=['BIU'], writes=[('bu', rb)],
                      in_offset=bass.IndirectOffsetOnAxis(ap=BIU[:, r:r + 1], axis=0))
                T.op('dve', lambda e: e.tensor_scalar(out=b7[rb][:], in0=bu[rb][:, 0:16:2], scalar1=-1.0, scalar2=7.0,
                                                      op0=ALU.mult, op1=ALU.add), reads=[('bu', rb)], writes=[('b7', rb)])
                T.op('dve', lambda e: e.tensor_scalar(out=b1[rb][:], in0=bu[rb][:, 1:16:2], scalar1=1.0, scalar2=None,
                                                      op0=ALU.add), reads=[('bu', rb)], writes=[('b1', rb)])

            def load_down_chunk(r, k):
                s = next_stg()
                T.dma('pool', stg[s][:, :], w_down2[:, :], reads=['WID'], writes=[('stg', s)],
                      in_offset=bass.IndirectOffsetOnAxis(ap=WID[:, r, k:k + 1], axis=0))
                ce = cast_eng()
                if ce == 'act':
                    T.op('act', lambda e: e.copy(out=wdb[:, k, :], in_=stg[s][:]), reads=[('stg', s)], writes=[('wdb', k)])
                else:
                    T.op('dve', lambda e: e.tensor_copy(out=wdb[:, k, :], in_=stg[s][:]),
                         reads=[('stg', s)], writes=[('wdb', k)])

            def build_ug(r):
                ub = posof[r] % 2
                for blk in range(CR[r] // 128):
                    xb_ = cnts['xst'] % NXS
                    cnts['xst'] += 1
                    row0 = CUM[r] + 128 * blk
                    T.dma('sp', xst[xb_][:], xs_d[row0:row0 + 128, :], writes=[('xst', xb_)])
                    pi = getps()
                    psv = psb[pi][:, :].bitcast(BF16)
                    for k in range(8):
                        T.op('pe', lambda e, k=k, psv=psv, xb_=xb_: e.transpose(
                            out=psv[:, 128 * k:128 * (k + 1)], in_=xst[xb_][:, 128 * k:128 * (k + 1)],
                            identity=identb[:]),
                            reads=[('xst', xb_)] if k == 0 else (), writes=[('ps', pi)] if k == 0 else (),
                            signal=(k == 7))
                    T._record((T.own['pe'], T.cnt['pe']), [('xst', xb_)], [('ps', pi)])
                    dstv = ugT[ub][:, :, 128 * blk:128 * (blk + 1)]
                    srcv = psv.rearrange("p (k s) -> p k s", k=8)
                    if blk % 2 == 0:
                        T.op('act', lambda e, dstv=dstv, srcv=srcv: e.copy(out=dstv, in_=srcv),
                             reads=[('ps', pi)], writes=[('ug', ub, blk)])
                    else:
                        T.op('dve', lambda e, dstv=dstv, srcv=srcv: e.tensor_copy(out=dstv, in_=srcv),
                             reads=[('ps', pi)], writes=[('ug', ub, blk)])

            order = []
            for i_ in range(NE // 2):
                order += [i_, NE - 1 - i_]
            posof = {r_: p_ for p_, r_ in enumerate(order)}
            halves = [(r, h) for r in order for h in range(2)]
            assert all(CR[r_] <= 512 for r_ in order[1::2])
            load_bias(order[0])
            load_half(order[0], 0, 0)
            load_half(order[0], 1, 1)
            build_ug(order[0])
            for ih, (r, h) in enumerate(halves):
                hb = ih % NHB
                ub = posof[r] % 2
                rb = posof[r] % 2
                C = CR[r]
                if ih + 2 < len(halves):
                    r2_, h2_ = halves[ih + 2]
                    if h2_ == 0:
                        load_bias(r2_)
                    load_half(r2_, h2_, (ih + 2) % NHB)
                tiles = [(o, min(512, C - o)) for o in range(0, C, 512)]
                for hp4 in range(4):
                    hp_ = 4 * h + hp4
                    for (o, n) in tiles:
                        i3 = cnts['z'] % NZ
                        cnts['z'] += 1
                        ugk = [('ug', ub, b_) for b_ in range(o // 128, (o + n) // 128)]
                        pg = getps()
                        mmgroup(psb[pg][:, 0:n], ('ps', pg),
                                [(wugh[hb][:, k, hp4, :], ugT[ub][:, k, o:o + n]) for k in range(8)],
                                reads=[('wugh', hb, k) for k in range(8)] + ugk)
                        pl = getps()
                        mmgroup(psb[pl][:, 0:n], ('ps', pl),
                                [(wulh[hb][:, k, hp4, :], ugT[ub][:, k, o:o + n]) for k in range(8)],
                                reads=[('wulh', hb, k) for k in range(8)] + ugk)
                        T.op('act', lambda e, i3=i3, pg=pg, rb=rb, hp_=hp_, n=n: e.activation(
                            out=zg[i3][:, 0:n], in_=psb[pg][:, 0:n], func=AF.Relu,
                            bias=b7[rb][:, hp_:hp_ + 1], scale=-1.0),
                            reads=[('ps', pg), ('b7', rb)], writes=[('zg', i3)])
                        T.op('act', lambda e, i3=i3, n=n: e.activation(
                            out=sg[i3][:, 0:n], in_=zg[i3][:, 0:n], func=AF.Silu, bias=float(7.0 * 1.702), scale=-1.702),
                            reads=[('zg', i3)], writes=[('sg', i3)])
                        T.op('dve', lambda e, i3=i3, pl=pl, rb=rb, hp_=hp_, n=n: e.tensor_scalar(
                            out=zl[i3][:, 0:n], in0=psb[pl][:, 0:n], scalar1=b1[rb][:, hp_:hp_ + 1], scalar2=8.0,
                            op0=ALU.add, op1=ALU.min), reads=[('ps', pl), ('b1', rb)], writes=[('zl', i3)])
                        T.op('dve', lambda e, i3=i3, hp_=hp_, o=o, n=n: e.scalar_tensor_tensor(
                            out=actT[:, hp_, o:o + n], in0=zl[i3][:, 0:n], scalar=-6.0, in1=sg[i3][:, 0:n],
                            op0=ALU.max, op1=ALU.mult),
                            reads=[('sg', i3), ('zl', i3)], writes=[('act', hp_, o // 512)])
                    load_down_chunk(r, hp_)
                if h == 0:
                    continue
                if posof[r] + 1 < NE:
                    build_ug(order[posof[r] + 1])
                for blk in range(C // 128):
                    ob = cnts['osb'] % 2
                    cnts['osb'] += 1
                    for n_ in range(2):
                        pi = getps()
                        mmgroup(psb[pi][:, :], ('ps', pi),
                                [(actT[:, k, 128 * blk:128 * (blk + 1)], wdb[:, k, 512 * n_:512 * (n_ + 1)])
                                 for k in range(8)],
                                reads=[('wdb', k) for k in range(8)] + [('act', k, blk // 4) for k in range(8)])
                        if n_ == 0:
                            T.op('act', lambda e, pi=pi, ob=ob: e.copy(out=osb[ob][:, 0:512], in_=psb[pi][:, :]),
                                 reads=[('ps', pi)], writes=[('osb', ob, 0)])
                        else:
                            T.op('dve', lambda e, pi=pi, ob=ob: e.tensor_copy(out=osb[ob][:, 512:1024], in_=psb[pi][:, :]),
                                 reads=[('ps', pi)], writes=[('osb', ob, 1)])
                    row0 = CUM[r] + 128 * blk
                    T.dma('sp', ys_d[row0:row0 + 128, :], osb[ob][:], reads=[('osb', ob, 0), ('osb', ob, 1)])

        yacc, yacc_blk = alloc_long([128, NT, D], F32)
        with Scope(A, T) as pi0:
            b_down = sb("b_down_s", [128, D], stack=pi0)
            gts = [sb("gts%d" % i, [128, 128], stack=pi0) for i in range(2)]
            ygt = [sb("ygt%d" % i, [128, D], stack=pi0) for i in range(3)]
            T.dma('sp', b_down[0:NE, :], b_down_d[:, :], writes=['b_down'])
            l2g = sb("l2g", [128, D], stack=pi0)
            l2b = sb("l2b", [128, D], stack=pi0)
            g2row = sb("g2row2", [128, D], stack=pi0)
            T.dma('sp', l2g[:], ln2_g_d[0:1, :].partition_broadcast(128), writes=['l2g'])
            T.dma('sp', l2b[:], ln2_b_d[0:1, :].partition_broadcast(128), writes=['l2b'])
            T.dma('sp', g2row[:], g2row_d[:, :], writes=['g2row'])
            h1r = [sb("h1r%d" % i, [128, D], stack=pi0) for i in range(2)]
            fo = [sb("fo%d" % i, [128, D], stack=pi0) for i in range(2)]
            mvt = [sb("mvt3%d" % i, [128, 8], stack=pi0) for i in range(2)]

            def p4_tile(tt):
                b = tt % 2
                ts_ = slice(128 * tt, 128 * (tt + 1))
                T.dma('sp', h1r[b][:], h1_d[ts_, :], writes=[('h1r', b)])
                T.op('dve', lambda e: e.tensor_tensor(out=yacc[:, tt, :], in0=yacc[:, tt, :], in1=g2row[:], op=ALU.mult),
                     reads=[('y', tt, 0), ('y', tt, 1), 'g2row'], writes=[('y', tt, 0), ('y', tt, 1)])
                T.op('dve', lambda e: e.scalar_tensor_tensor(
                    out=fo[b][:], in0=h1r[b][:], scalar=ALPHA, in1=yacc[:, tt, :], op0=ALU.mult, op1=ALU.add),
                    reads=[('h1r', b), ('y', tt, 0), ('y', tt, 1)], writes=[('fo', b)])
                rs, nm, keys = layernorm_rows(fo[b], ('fo', b), h1r[b], ('h1r', b), mvt[b], ('c', b))
                T.op('act', lambda e: e.activation(out=fo[b][:], in_=fo[b][:], func=AF.Identity, bias=nm, scale=rs),
                     reads=[('fo', b)] + keys, writes=[('fo', b)])
                T.op('dve', lambda e: e.tensor_tensor(out=fo[b][:], in0=fo[b][:], in1=l2g[:], op=ALU.mult),
                     reads=[('fo', b), 'l2g'], writes=[('fo', b)])
                T.op('dve', lambda e: e.tensor_tensor(out=fo[b][:], in0=fo[b][:], in1=l2b[:], op=ALU.add),
                     reads=[('fo', b), 'l2b'], writes=[('fo', b)])
                T.dma('sp', out_d[ts_, :], fo[b][:], reads=[('fo', b)])

            ig = 0
            for tt in range(NT):
                b = tt % 2
                pg_ = getps()
                T.op('pe', lambda e, pg_=pg_, tt=tt: e.transpose(
                    out=psb[pg_][0:NE, 0:128], in_=G[:, tt, :], identity=ident[:]),
                    reads=[], writes=[('ps', pg_)])
                T.op('act', lambda e, pg_=pg_, b=b: e.copy(out=gts[b][0:NE, :], in_=psb[pg_][0:NE, 0:128]),
                     reads=[('ps', pg_)], writes=[('gts', b)])
                for n in range(2):
                    pi = getps()
                    mmgroup(psb[pi][:, :], ('ps', pi), [(gts[b][0:NE, :], b_down[0:NE, 512 * n:512 * (n + 1)])],
                            reads=[('gts', b), 'b_down'])
                    T.op('act', lambda e, pi=pi, tt=tt, n=n: e.copy(
                        out=yacc[:, tt, 512 * n:512 * (n + 1)], in_=psb[pi][:, :]),
                        reads=[('ps', pi)], writes=[('y', tt, n)])
                for j in range(4):
                    g3 = ig % 3
                    ig += 1
                    T.dma('pool', ygt[g3][:, :], ys_d[:, :], writes=[('ygt', g3)],
                          in_offset=bass.IndirectOffsetOnAxis(ap=SLOT[:, 4 * tt + j:4 * tt + j + 1], axis=0))
                    T.op('dve', lambda e, g3=g3, tt=tt, j=j: e.scalar_tensor_tensor(
                        out=yacc[:, tt, :], in0=ygt[g3][:], scalar=GT[:, tt, j:j + 1], in1=yacc[:, tt, :],
                        op0=ALU.mult, op1=ALU.add),
                        reads=[('ygt', g3), ('y', tt, 0), ('y', tt, 1)], writes=[('y', tt, 0), ('y', tt, 1)])
                if tt >= 1:
                    p4_tile(tt - 1)
            p4_tile(NT - 1)

        T.finish()
    return nc


def _col(v):
    return np.ascontiguousarray(np.asarray(v, np.float32).reshape(8, 128).T)


def make_in_maps(inputs, cores):
    f = lambda a: np.ascontiguousarray(np.asarray(a, np.float32))
    x = f(inputs['x'])
    c = f(inputs['c'])
    shared = {
        "w_ada": f(inputs['w_ada'][0]),
        "b_ada_col": np.ascontiguousarray(f(inputs['b_ada'][0]).reshape(48, 128).T),
        "b_ada_row": f(inputs['b_ada'][0]).reshape(1, 6 * D),
        "w_in": f(inputs['w_in'][0]),
        "b_in_col": np.ascontiguousarray(f(inputs['b_in'][0]).reshape(48, 128).T),
        "w_cv_dw_col": np.ascontiguousarray(f(inputs['w_cv_dw'][0]).T.reshape(8, 128, 31).transpose(1, 0, 2)),
        "b_cv_dw_col": _col(inputs['b_cv_dw'][0]),
        "ln_cv_g_col": _col(inputs['ln_cv_g'][0]),
        "ln_cv_b_col": _col(inputs['ln_cv_b'][0]),
        "w_cv_out": f(inputs['w_cv_out'][0]),
        "w_lru_conv_col": np.ascontiguousarray(f(inputs['w_lru_conv'][0]).T.reshape(8, 128, 4).transpose(1, 0, 2)),
        "b_lru_conv_col": _col(inputs['b_lru_conv'][0]),
        "w_lru_a_t": np.ascontiguousarray(f(inputs['w_lru_a'][0]).transpose(1, 0, 2)),
        "b_lru_a_col": _col(inputs['b_lru_a'][0]),
        "w_lru_x_t": np.ascontiguousarray(f(inputs['w_lru_x'][0]).transpose(1, 0, 2)),
        "b_lru_x_col": _col(inputs['b_lru_x'][0]),
        "lru_lambda_col": _col(inputs['lru_lambda'][0]),
        "w_lru_out": f(inputs['w_lru_out'][0]),
        "w_o": f(inputs['w_o'][0]),
        "b_o_row": f(inputs['b_o'][0]).reshape(1, D),
        "ln1_g_row": f(inputs['ln1_g'][0]).reshape(1, D),
        "ln1_b_row": f(inputs['ln1_b'][0]).reshape(1, D),
        "w_router_t": np.ascontiguousarray(f(inputs['w_router'][0]).reshape(8, 128, NE).transpose(1, 0, 2)),
        "b_router_row": f(inputs['b_router'][0]).reshape(1, NE),
        "w_up": f(inputs['w_up'][0]),
        "b_up_rows": np.ascontiguousarray(f(inputs['b_up'][0]).reshape(NE, 8, 128, 2).transpose(0, 2, 1, 3)).reshape(NE * 128, 16),
        "w_down": f(inputs['w_down'][0]),
        "b_down": f(inputs['b_down'][0]),
        "ln2_g_row": f(inputs['ln2_g'][0]).reshape(1, D),
        "ln2_b_row": f(inputs['ln2_b'][0]).reshape(1, D),
    }
    maps = []
    for b in cores:
        m = dict(shared)
        m["x"] = np.ascontiguousarray(x[b])
        m["c_col"] = _col(c[b])
        maps.append(m)
    return maps


def kernel(**inputs):
    nc = build()
    maps = make_in_maps(inputs, list(range(8)))
    res = run_bass_kernel_spmd(nc, maps, core_ids=list(range(8)))
    out = np.stack([np.asarray(r["out"], np.float32) for r in res.results], axis=0)
    return out
```

```python
import numpy as np
from contextlib import ExitStack
import concourse.bass as bass
import concourse.mybir as mybir
from concourse.bass_utils import run_bass_kernel_spmd

F32 = mybir.dt.float32
BF16 = mybir.dt.bfloat16
AF = mybir.ActivationFunctionType
ALU = mybir.AluOpType
AX = mybir.AxisListType

S = 2048
D = 1024
NE = 32
ALPHA = float(2.0 ** 0.25)
EPS = 1e-5
NT = S // 128
NQ = S // 512
SEM_ROLL = 30000
CR = [min(2048, -(-(8192 // (r + 1)) // 128) * 128) for r in range(NE)]
CUM = [0]
for _c in CR:
    CUM.append(CUM[-1] + _c)
NSLOT = CUM[-1]
I32 = mybir.dt.int32


class TR:
    def __init__(self, nc, st):
        self.nc = nc
        self.st = st
        self.E = {'pe': nc.tensor, 'act': nc.scalar, 'dve': nc.vector, 'pool': nc.gpsimd, 'sp': nc.sync}
        self.sems = []
        self.own = {}
        self.cnt = {}
        self.seen = {e: {} for e in self.E}
        self.lastw = {}
        self.readers = {}
        self.dslots = {}
        self.drr = {}
        self.ownset = {e: set() for e in self.E}

    def newsem(self, name):
        h = self.st.enter_context(self.nc.semaphore(name))
        self.sems.append(h)
        return len(self.sems) - 1

    def _own(self, e):
        if e not in self.own or self.cnt[e] >= SEM_ROLL:
            self.own[e] = self.newsem("c_%s_%d" % (e, len(self.sems)))
            self.ownset[e].add(self.own[e])
            self.cnt[e] = 0
        return self.own[e]

    def _wait(self, e, tok):
        if tok is None:
            return
        s, v = tok
        if self.seen[e].get(s, 0) >= v:
            return
        self.seen[e][s] = v
        self.E[e].wait_ge(self.sems[s], v)

    def _dep(self, e, tok):
        if tok is None:
            return
        if e == 'pe' and tok[0] in self.ownset['pe']:
            return
        self._wait(e, tok)

    def _deps(self, e, reads, writes):
        for k in reads:
            self._dep(e, self.lastw.get(k))
        for k in writes:
            self._dep(e, self.lastw.get(k))
            r = self.readers.get(k)
            if r:
                for s, v in r.items():
                    self._dep(e, (s, v))

    def _record(self, tok, reads, writes):
        s, v = tok
        for k in reads:
            r = self.readers.setdefault(k, {})
            if r.get(s, 0) < v:
                r[s] = v
        for k in writes:
            self.lastw[k] = tok
            self.readers[k] = {}

    @staticmethod
    def _excl(reads, writes):
        ps = [k for k in reads if isinstance(k, tuple) and k[0] == 'ps']
        if not ps:
            return reads, writes
        return [k for k in reads if not (isinstance(k, tuple) and k[0] == 'ps')], list(writes) + ps

    def op(self, e, fn, reads=(), writes=(), signal=True):
        reads, writes = self._excl(reads, writes)
        self._deps(e, reads, writes)
        s = self._own(e)
        inst = fn(self.E[e])
        if signal:
            self.cnt[e] += 1
            inst.then_inc(self.sems[s], 1)
            tok = (s, self.cnt[e])
        else:
            tok = (s, self.cnt[e] + 1)
        self._record(tok, reads, writes)
        return tok

    def dma(self, q, out, in_, reads=(), writes=(), nslots=12, out_offset=None, in_offset=None):
        self._deps(q, reads, writes)
        if q not in self.dslots:
            self.dslots[q] = [[self.newsem("d_%s_%d" % (q, i)), 0] for i in range(nslots)]
            self.drr[q] = 0
        slot = self.dslots[q][self.drr[q]]
        self.drr[q] = (self.drr[q] + 1) % len(self.dslots[q])
        if slot[1] > 0:
            self._wait(q, (slot[0], slot[1]))
        if out_offset is not None or in_offset is not None:
            inst = self.E[q].indirect_dma_start(out=out, out_offset=out_offset, in_=in_, in_offset=in_offset)
        else:
            inst = self.E[q].dma_start(out=out, in_=in_)
        slot[1] += 16
        inst.then_inc(self.sems[slot[0]], 16)
        tok = (slot[0], slot[1])
        self._record(tok, reads, writes)
        return tok

    def barrier(self):
        toks = []
        for e in self.own:
            if self.cnt[e] > 0:
                toks.append((self.own[e], self.cnt[e]))
        for q in self.dslots:
            for s, v in self.dslots[q]:
                if v > 0:
                    toks.append((s, v))
        for e in self.E:
            for t in toks:
                if e == 'pe' and t[0] == self.own.get(e):
                    continue
                self._wait(e, t)
        self.lastw = {}
        self.readers = {}

    def finish(self):
        for q in self.dslots:
            for s, v in self.dslots[q]:
                if v > 0:
                    self._wait('sp', (s, v))
        for e in self.own:
            if e != 'sp' and self.cnt[e] > 0:
                self._wait('sp', (self.own[e], self.cnt[e]))


class Arena:
    def __init__(self, R, nbytes):
        self.R = R
        self.free = [(0, nbytes)]
        self.pending = []

    def alloc(self, shape, dt):
        esz = 2 if dt == BF16 else 4
        n = esz
        for s in shape[1:]:
            n *= s
        n = (n + 63) // 64 * 64
        for i, (o, l) in enumerate(self.free):
            if l >= n:
                if l == n:
                    self.free.pop(i)
                else:
                    self.free[i] = (o + n, l - n)
                v = self.R[:, o // 4:(o + n) // 4]
                if dt == BF16:
                    v = v.bitcast(BF16)
                tot = 1
                for s in shape[1:]:
                    tot *= s
                v = v[:, 0:tot]
                if len(shape) == 3:
                    v = v.rearrange("p (a b) -> p a b", a=shape[1])
                elif len(shape) == 4:
                    v = v.rearrange("p (a b c) -> p a b c", a=shape[1], b=shape[2])
                return v, (o, n)
        raise RuntimeError("arena full: need %d, free=%s" % (n, self.free))

    def release(self, blk):
        self.pending.append(blk)

    def commit(self):
        fl = sorted(self.free + self.pending)
        self.pending = []
        out = []
        for o, l in fl:
            if out and out[-1][0] + out[-1][1] == o:
                out[-1] = (out[-1][0], out[-1][1] + l)
            else:
                out.append((o, l))
        self.free = out


class Scope:
    def __init__(self, A, T):
        self.A = A
        self.T = T
        self.blks = []

    def __enter__(self):
        return self

    def alloc(self, shape, dt):
        v, b = self.A.alloc(shape, dt)
        self.blks.append(b)
        return v

    def __exit__(self, *a):
        if a[0] is None:
            self.T.barrier()
            for b in self.blks:
                self.A.release(b)
            self.A.commit()
        return False


def build(debug=None):
    nc = bass.Bass("TRN2", target_bir_lowering=False)

    def din(name, shape, dt=F32):
        return nc.dram_tensor(name, list(shape), dt, kind="ExternalInput").ap()

    x_d = din("x", [S, D])
    c_col_d = din("c_col", [128, 8])
    w_ada_d = din("w_ada", [D, 6 * D])
    b_ada_col_d = din("b_ada_col", [128, 48])
    b_ada_row_d = din("b_ada_row", [1, 6 * D])
    w_in_d = din("w_in", [D, 6 * D])
    b_in_col_d = din("b_in_col", [128, 48])
    w_cvdw_d = din("w_cv_dw_col", [128, 8, 31])
    b_cvdw_d = din("b_cv_dw_col", [128, 8])
    ln_cv_g_d = din("ln_cv_g_col", [128, 8])
    ln_cv_b_d = din("ln_cv_b_col", [128, 8])
    w_cv_out_d = din("w_cv_out", [D, D])
    w_lconv_d = din("w_lru_conv_col", [128, 8, 4])
    b_lconv_d = din("b_lru_conv_col", [128, 8])
    w_lru_a_d = din("w_lru_a_t", [128, 8, 128])
    b_lru_a_d = din("b_lru_a_col", [128, 8])
    w_lru_x_d = din("w_lru_x_t", [128, 8, 128])
    b_lru_x_d = din("b_lru_x_col", [128, 8])
    lam_d = din("lru_lambda_col", [128, 8])
    w_lru_out_d = din("w_lru_out", [D, D])
    w_o_d = din("w_o", [D, D])
    b_o_d = din("b_o_row", [1, D])
    ln1_g_d = din("ln1_g_row", [1, D])
    ln1_b_d = din("ln1_b_row", [1, D])
    w_router_d = din("w_router_t", [128, 8, NE])
    b_router_d = din("b_router_row", [1, NE])
    w_up_d = din("w_up", [NE, D, 2 * D])
    b_up_d = din("b_up_rows", [NE * 128, 16])
    w_down_d = din("w_down", [NE, D, D])
    b_down_d = din("b_down", [NE, D])
    ln2_g_d = din("ln2_g_row", [1, D])
    ln2_b_d = din("ln2_b_row", [1, D])
    out_d = nc.dram_tensor("out", [S, D], F32, kind="ExternalOutput").ap()
    h1_d = nc.dram_tensor("h1_scratch", [S, D], F32, kind="Internal").ap()
    g1row_d = nc.dram_tensor("g1row_scratch", [128, D], F32, kind="Internal").ap()
    g1bo_d = nc.dram_tensor("g1bo_scratch", [128, D], F32, kind="Internal").ap()
    g2row_d = nc.dram_tensor("g2row_scratch", [128, D], F32, kind="Internal").ap()
    sh2row_d = nc.dram_tensor("sh2row_scratch", [128, D], F32, kind="Internal").ap()
    sc2prow_d = nc.dram_tensor("sc2prow_scratch", [128, D], F32, kind="Internal").ap()
    u2tok_d = nc.dram_tensor("u2tok_scratch", [S, D], BF16, kind="Internal").ap()
    xs_d = nc.dram_tensor("xs_scratch", [NSLOT, D], BF16, kind="Internal").ap()
    ys_d = nc.dram_tensor("ys_scratch", [NSLOT, D], F32, kind="Internal").ap()
    w_up2 = w_up_d.rearrange("e d (h n) -> (e d h) n", h=2)
    w_down2 = w_down_d.rearrange("e d n -> (e d) n")
    dbg_d = None
    if debug is not None:
        dbg_d = nc.dram_tensor("dbg", [128, 8 * S], F32, kind="ExternalOutput").ap()

    with ExitStack() as st:
        T = TR(nc, st)
        ARENA_BYTES = 194 * 1024
        Rt = st.enter_context(nc.sbuf_tensor("arena", [128, ARENA_BYTES // 4], F32))
        A = Arena(Rt, ARENA_BYTES)

        def sb(name, shape, dt=F32, stack=None):
            if stack is None:
                return st.enter_context(nc.sbuf_tensor(name, list(shape), dt))
            return stack.alloc(list(shape), dt)

        def alloc_long(shape, dt):
            return A.alloc(list(shape), dt)

        def free_long(blk):
            T.barrier()
            A.release(blk)
            A.commit()

        psb = [st.enter_context(nc.psum_tensor("ps%d" % i, [128, 512], F32)) for i in range(8)]
        ps_rr = [0]

        def getps():
            i = ps_rr[0]
            ps_rr[0] = (i + 1) % 8
            return i

        def mmgroup(ps_ap, pskey, pairs, reads=()):
            n = len(pairs)
            tok = None
            for i, (l, r) in enumerate(pairs):
                last = (i == n - 1)
                tok = T.op('pe', lambda e, l=l, r=r, i=i, last=last: e.matmul(
                    ps_ap, lhsT=l, rhs=r, start=(i == 0), stop=last),
                    reads=reads if i == 0 else (), writes=[pskey] if i == 0 else (), signal=last)
            if n > 1:
                T._record(tok, reads, [pskey])
            return tok

        def transpose4(pi, src, k0, rkey):
            for q in range(4):
                k = k0 + q
                T.op('pe', lambda e, q=q, k=k: e.transpose(
                    out=psb[pi][:, 128 * q:128 * (q + 1)], in_=src[:, 128 * k:128 * (k + 1)],
                    identity=ident[:]),
                    reads=[rkey] if q == 0 else (), writes=[('ps', pi)] if q == 0 else (),
                    signal=(q == 3))
            T._record((T.own['pe'], T.cnt['pe']), [rkey], [('ps', pi)])

        ident = sb("ident", [128, 128])
        ones = sb("ones", [128, 128])
        epsc = sb("epsc", [128, 1])
        T.op('pool', lambda e: e.memset(ones[:], 1.0), writes=['ones'])
        T.op('pool', lambda e: e.memset(epsc[:], EPS), writes=['epsc'])
        T.op('pool', lambda e: e.affine_select(out=ident[:], in_=ones[:], pattern=[[-1, 128]],
                                               compare_op=ALU.is_equal, fill=0.0, base=0,
                                               channel_multiplier=1), reads=['ones'], writes=['ident'])

        identb = sb("identb", [128, 128], BF16)
        T.op('pool', lambda e: e.tensor_copy(out=identb[:], in_=ident[:]), reads=['ident'], writes=['identb'])

        def ld_small(name, src, shape):
            t = sb(name, shape)
            T.dma('sp', t[:], src, writes=[name])
            return t

        c_col = ld_small("c_col_s", c_col_d[:, :], [128, 8])
        b_ada_col = ld_small("b_ada_col_s", b_ada_col_d[:, :], [128, 48])
        b_in_col = ld_small("b_in_col_s", b_in_col_d[:, :], [128, 48])
        w_cvdw = ld_small("w_cvdw_s", w_cvdw_d[:, :, :], [128, 8, 31])
        b_cvdw = ld_small("b_cvdw_s", b_cvdw_d[:, :], [128, 8])
        ln_cv_g = ld_small("ln_cv_g_s", ln_cv_g_d[:, :], [128, 8])
        ln_cv_b = ld_small("ln_cv_b_s", ln_cv_b_d[:, :], [128, 8])
        w_lconv = ld_small("w_lconv_s", w_lconv_d[:, :, :], [128, 8, 4])
        b_lconv = ld_small("b_lconv_s", b_lconv_d[:, :], [128, 8])
        b_lru_a = ld_small("b_lru_a_s", b_lru_a_d[:, :], [128, 8])
        b_lru_x = ld_small("b_lru_x_s", b_lru_x_d[:, :], [128, 8])
        lam = ld_small("lam_s", lam_d[:, :], [128, 8])
        w_router = ld_small("w_router_s", w_router_d[:, :, :], [128, 8, NE])
        T.barrier()

        mod_col = sb("mod_col", [128, 48])
        sc1p = sb("sc1p", [128, 8])
        sc2p = sb("sc2p", [128, 8])
        clam = sb("clam", [128, 8])
        clam2 = sb("clam2", [128, 8])
        G = sb("G", [128, NT, NE])
        GT = sb("GT", [128, NT, 4])
        SLOT = sb("SLOT", [128, NT * 4], I32)
        WIU = sb("WIU", [128, NE, 16], I32)
        WID = sb("WID", [128, NE, 8], I32)
        BIU = sb("BIU", [128, NE], I32)

        def dbg_out(view3):
            with Scope(A, T) as dd:
                for k in range(8):
                    dt_ = sb("dbgt", [128, S], stack=dd)
                    T.op('dve', lambda e, k=k, dt_=dt_: e.tensor_copy(out=dt_[:], in_=view3[:, k, :]),
                         writes=[('dbgt', k)])
                    T.dma('sp', dbg_d[:, S * k:S * (k + 1)], dt_[:], reads=[('dbgt', k)])
            T.finish()

        with Scope(A, T) as p0:
            sc = sb("sc", [128, 8], stack=p0)
            scb = sb("scb", [128, 8, 128], stack=p0)
            wa = [sb("wa%d" % i, [128, 8, 128], stack=p0) for i in range(3)]
            wr = [sb("wr%d" % i, [128, 8, 512], stack=p0) for i in range(2)]
            brow = sb("brow", [128, 512], stack=p0)
            bo_b = sb("bo_b", [128, D], stack=p0)
            tmp1 = sb("p0tmp", [128, 8], stack=p0)
            g1row = sb("g1row", [128, D], stack=p0)
            g2row = sb("g2row", [128, D], stack=p0)
            g1bo = sb("g1bo", [128, D], stack=p0)
            sh2row = sb("sh2row", [128, D], stack=p0)
            sc2prow = sb("sc2prow", [128, D], stack=p0)
            T.op('act', lambda e: e.activation(out=sc[:], in_=c_col[:], func=AF.Silu), writes=['sc'])
            for k in range(8):
                T.op('dve', lambda e, k=k: e.tensor_copy(out=scb[:, k, :],
                                                         in_=sc[:, k:k + 1].to_broadcast([128, 128])),
                     reads=['sc'], writes=[('scb', k)])
            T.op('act', lambda e: e.activation(out=tmp1[:], in_=lam[:], func=AF.Exp, scale=-1.0),
                 writes=['p0tmp'])
            T.op('act', lambda e: e.activation(out=tmp1[:], in_=tmp1[:], func=AF.Ln, bias=1.0, scale=1.0),
                 reads=['p0tmp'], writes=['p0tmp'])
            T.op('dve', lambda e: e.tensor_scalar(out=clam[:], in0=tmp1[:], scalar1=-8.0, scalar2=None,
                                                  op0=ALU.mult), reads=['p0tmp'], writes=['clam'])
            T.op('dve', lambda e: e.tensor_scalar(out=clam2[:], in0=tmp1[:], scalar1=-16.0, scalar2=None,
                                                  op0=ALU.mult), reads=['p0tmp'], writes=['clam2'])
            pm = getps()
            for j in range(48):
                slot = j % 3
                T.dma('sp', wa[slot][:, :, :],
                      w_ada_d[:, 128 * j:128 * (j + 1)].rearrange("(k p) n -> p k n", p=128),
                      writes=[('wa', slot)])
                for k in range(8):
                    T.op('pe', lambda e, k=k, j=j, slot=slot: e.matmul(
                        psb[pm][:, j:j + 1], lhsT=wa[slot][:, k, :], rhs=sc[:, k:k + 1],
                        start=(k == 0), stop=(k == 7)),
                        reads=[('wa', slot), 'sc'] if k == 0 else (),
                        writes=[('ps', pm)] if k == 0 else (), signal=(k == 7))
                T._record((T.own['pe'], T.cnt['pe']), [('wa', slot), 'sc'], [('ps', pm)])
            T.op('dve', lambda e: e.tensor_tensor(out=mod_col[:], in0=psb[pm][:, 0:48], in1=b_ada_col[:],
                                                  op=ALU.add), reads=[('ps', pm)], writes=['mod'])
            T.op('dve', lambda e: e.tensor_scalar(out=sc1p[:], in0=mod_col[:, 8:16], scalar1=1.0, scalar2=None,
                                                  op0=ALU.add), reads=['mod'], writes=['sc1p'])
            T.op('dve', lambda e: e.tensor_scalar(out=sc2p[:], in0=mod_col[:, 32:40], scalar1=1.0, scalar2=None,
                                                  op0=ALU.add), reads=['mod'], writes=['sc2p'])
            T.dma('sp', bo_b[:], b_o_d[0:1, :].partition_broadcast(128), writes=['bo_b'])
            i_r = 0
            for sec, dst in ((2, g1row), (5, g2row), (3, sh2row), (4, sc2prow)):
                for n in range(2):
                    off = sec * D + 512 * n
                    slot = i_r % 2
                    i_r += 1
                    T.dma('sp', wr[slot][:, :, :],
                          w_ada_d[:, off:off + 512].rearrange("(k p) n -> p k n", p=128),
                          writes=[('wr', slot)])
                    T.dma('sp', brow[:], b_ada_row_d[0:1, off:off + 512].partition_broadcast(128),
                          writes=['brow'])
                    pi = getps()
                    mmgroup(psb[pi][:, :], ('ps', pi),
                            [(scb[:, k, :], wr[slot][:, k, :]) for k in range(8)],
                            reads=[('wr', slot)] + [('scb', k) for k in range(8)])
                    T.op('dve', lambda e, pi=pi, dst=dst, n=n: e.tensor_tensor(
                        out=dst[:, 512 * n:512 * (n + 1)], in0=psb[pi][:, :], in1=brow[:], op=ALU.add),
                        reads=[('ps', pi), 'brow'], writes=[('grow', sec, n)])
            T.op('dve', lambda e: e.tensor_tensor(out=g1bo[:], in0=g1row[:], in1=bo_b[:], op=ALU.mult),
                 reads=[('grow', 2, 0), ('grow', 2, 1), 'bo_b'], writes=['g1bo'])
            T.dma('sp', g1row_d[:, :], g1row[:], reads=[('grow', 2, 0), ('grow', 2, 1)])
            T.dma('sp', g2row_d[:, :], g2row[:], reads=[('grow', 5, 0), ('grow', 5, 1)])
            T.dma('sp', g1bo_d[:, :], g1bo[:], reads=['g1bo'])
            T.op('dve', lambda e: e.tensor_scalar(out=sc2prow[:], in0=sc2prow[:], scalar1=1.0, scalar2=None, op0=ALU.add),
                 reads=[('grow', 4, 0), ('grow', 4, 1)], writes=[('grow', 4, 0), ('grow', 4, 1)])
            T.dma('sp', sh2row_d[:, :], sh2row[:], reads=[('grow', 3, 0), ('grow', 3, 1)])
            T.dma('sp', sc2prow_d[:, :], sc2prow[:], reads=[('grow', 4, 0), ('grow', 4, 1)])

        if debug == 'p0':
            with Scope(A, T) as dd:
                dt_ = sb("dbgt", [128, 3 * D + 128], stack=dd)
                T.op('dve', lambda e: e.memset(dt_[:], 0.0), writes=['dbgt'])
                T.op('dve', lambda e: e.tensor_copy(out=dt_[:, 0:48], in_=mod_col[:]), reads=['dbgt'], writes=['dbgt'])
                T.op('dve', lambda e: e.tensor_copy(out=dt_[:, 48:56], in_=clam[:]), reads=['dbgt'], writes=['dbgt'])
                T.dma('sp', dt_[:, 128:128 + D], g1row_d[:, :], reads=['dbgt'], writes=['dbgt'])
                T.dma('sp', dt_[:, 128 + D:128 + 2 * D], g2row_d[:, :], reads=['dbgt'], writes=['dbgt'])
                T.dma('sp', dt_[:, 128 + 2 * D:128 + 3 * D], g1bo_d[:, :], reads=['dbgt'], writes=['dbgt'])
                T.dma('sp', dbg_d[:, 0:3 * D + 128], dt_[:, :], reads=['dbgt'])
            T.finish()
            return nc

        def layernorm_rows(xt_ap, key_in, junk, junk_key, mvt, tagi):
            T.op('dve', lambda e: e.tensor_reduce(out=mvt[:, 0:1], in_=xt_ap[:, :], axis=AX.X, op=ALU.add),
                 reads=[key_in], writes=[('mv0', tagi)])
            T.op('act', lambda e: e.activation(out=junk[:, :], in_=xt_ap[:, :], func=AF.Square,
                                               accum_out=mvt[:, 1:2]),
                 reads=[key_in], writes=[junk_key, ('mv1', tagi)])
            T.op('dve', lambda e: e.tensor_scalar(out=mvt[:, 2:3], in0=mvt[:, 0:1], scalar1=1.0 / D, scalar2=None,
                                                  op0=ALU.mult),
                 reads=[('mv0', tagi)], writes=[('mv2', tagi)])
            T.op('dve', lambda e: e.tensor_tensor(out=mvt[:, 3:4], in0=mvt[:, 2:3], in1=mvt[:, 2:3], op=ALU.mult),
                 reads=[('mv2', tagi)], writes=[('mv3', tagi)])
            T.op('dve', lambda e: e.scalar_tensor_tensor(out=mvt[:, 3:4], in0=mvt[:, 1:2], scalar=1.0 / D,
                                                         in1=mvt[:, 3:4], op0=ALU.mult, op1=ALU.subtract),
                 reads=[('mv1', tagi), ('mv3', tagi)], writes=[('mv3', tagi)])
            T.op('act', lambda e: e.activation(out=mvt[:, 3:4], in_=mvt[:, 3:4], func=AF.Sqrt,
                                               bias=epsc[:, 0:1], scale=1.0),
                 reads=[('mv3', tagi)], writes=[('mv3', tagi)])
            T.op('dve', lambda e: e.reciprocal(out=mvt[:, 4:5], in_=mvt[:, 3:4]),
                 reads=[('mv3', tagi)], writes=[('rs', tagi)])
            T.op('dve', lambda e: e.tensor_scalar(out=mvt[:, 5:6], in0=mvt[:, 2:3], scalar1=mvt[:, 4:5],
                                                  scalar2=-1.0, op0=ALU.mult, op1=ALU.mult),
                 reads=[('mv2', tagi), ('rs', tagi)], writes=[('nm', tagi)])
            return mvt[:, 4:5], mvt[:, 5:6], [('rs', tagi), ('nm', tagi)]

        uT, uT_blk = alloc_long([128, 8, S], BF16)
        with Scope(A, T) as p1:
            xt = [sb("xt%d" % i, [128, D], stack=p1) for i in range(3)]
            xn = [sb("xn%d" % i, [128, D], stack=p1) for i in range(2)]
            stt = [sb("stt%d" % i, [128, 12], stack=p1) for i in range(2)]
            mvt = [sb("mvt%d" % i, [128, 8], stack=p1) for i in range(2)]
            for tt in range(NT if debug not in ('p1a', 'p1b') else 1):
                a = tt % 3
                b = tt % 2
                T.dma('sp', xt[a][:], x_d[128 * tt:128 * (tt + 1), :], writes=[('xt', a)])
                rs, nm, keys = layernorm_rows(xt[a], ('xt', a), xn[b], ('xn', b), mvt[b], b)
                T.op('act', lambda e, a=a, b=b, rs=rs, nm=nm: e.activation(
                    out=xn[b][:], in_=xt[a][:], func=AF.Identity, bias=nm, scale=rs),
                    reads=[('xt', a)] + keys, writes=[('xn', b)])
                if debug == 'p1a':
                    T.dma('sp', dbg_d[:, 0:D], xn[b][:], reads=[('xn', b)])
                    T.dma('sp', dbg_d[:, D:D + 8], mvt[b][:], reads=[('xn', b)])
                    break
                for half in range(2):
                    pi = getps()
                    transpose4(pi, xn[b], half * 4, ('xn', b))
                    for q in range(4):
                        k = half * 4 + q
                        dst = uT[:, k, 128 * tt:128 * (tt + 1)]
                        if half == 0:
                            T.op('dve', lambda e, pi=pi, q=q, k=k, dst=dst: e.tensor_scalar(
                                out=dst, in0=psb[pi][:, 128 * q:128 * (q + 1)], scalar1=sc1p[:, k:k + 1],
                                scalar2=mod_col[:, k:k + 1], op0=ALU.mult, op1=ALU.add),
                                reads=[('ps', pi)], writes=[('uT', k, tt)])
                        else:
                            T.op('act', lambda e, pi=pi, q=q, k=k, dst=dst: e.activation(
                                out=dst, in_=psb[pi][:, 128 * q:128 * (q + 1)], func=AF.Identity,
                                bias=mod_col[:, k:k + 1], scale=sc1p[:, k:k + 1]),
                                reads=[('ps', pi)], writes=[('uT', k, tt)])
        if debug == 'p1a':
            T.finish()
            return nc
        if debug in ('uT', 'p1b'):
            dbg_out(uT)
            return nc

        p2 = Scope(A, T)
        wst = [sb("wst%d" % i, [128, 8, 128], stack=p2) for i in range(3)]
        wpb = [sb("wpb%d" % i, [128, 8, 128], BF16, stack=p2) for i in range(4)]
        wcnt = [0, 0]

        def load_panel(src_ap, cast_eng=None):
            s1 = wcnt[0] % 3
            s2 = wcnt[1] % 4
            cast_eng = 'act' if wcnt[0] % 2 == 0 else 'dve'
            wcnt[0] += 1
            wcnt[1] += 1
            T.dma('sp', wst[s1][:, :, :], src_ap.rearrange("(k p) n -> p k n", p=128),
                  writes=[('wst', s1)])
            if cast_eng == 'act':
                T.op('act', lambda e: e.copy(out=wpb[s2][:, :, :], in_=wst[s1][:, :, :]),
                     reads=[('wst', s1)], writes=[('wpb', s2)])
            else:
                T.op(cast_eng, lambda e: e.tensor_copy(out=wpb[s2][:, :, :], in_=wst[s1][:, :, :]),
                     reads=[('wst', s1)], writes=[('wpb', s2)])
            return wpb[s2], ('wpb', s2)

        def inproj(panel_tile, pkey, q):
            pi = getps()
            mmgroup(psb[pi][:, :], ('ps', pi),
                    [(panel_tile[:, k, :], uT[:, k, 512 * q:512 * (q + 1)]) for k in range(8)],
                    reads=[pkey])
            return pi

        ya_act, ya_blk = alloc_long([128, 8, S], BF16)
        with Scope(A, T) as pb:
            yac = sb("yac", [128, 8, S], stack=pb)
            with Scope(A, T) as pb1:
                NG = 2
                glu = [sb("glu%d" % i, [128, 32 + S], BF16, stack=pb1) for i in range(NG)]
                dgs = [sb("dgs%d" % i, [128, 31, 128], BF16, stack=pb1) for i in range(NG)]
                tsig = [sb("tsig%d" % i, [128, 512], stack=pb1) for i in range(2)]
                for i in range(NG):
                    T.op('pool', lambda e, i=i: e.memset(glu[i][:, 0:30], 0.0), writes=[('glu', i, 'pad')])
                it = 0
                for c in range(8):
                    r2 = c % NG
                    pan_v, pk_v = load_panel(w_in_d[:, 128 * c:128 * (c + 1)])
                    pan_g, pk_g = load_panel(w_in_d[:, 1024 + 128 * c:1024 + 128 * (c + 1)])
                    for k in range(31):
                        if k % 2 == 0:
                            T.op('dve', lambda e, r2=r2, c=c, k=k: e.tensor_scalar(
                                out=dgs[r2][:, k, :], in0=identb[:, :], scalar1=w_cvdw[:, c, k:k + 1], scalar2=None,
                                op0=ALU.mult), writes=[('dgs', r2, k)])
                        else:
                            T.op('act', lambda e, r2=r2, c=c, k=k: e.activation(
                                out=dgs[r2][:, k, :], in_=identb[:, :], func=AF.Copy, scale=w_cvdw[:, c, k:k + 1]),
                                writes=[('dgs', r2, k)])
                    for q in range(NQ):
                        i2 = it % 2
                        it += 1
                        pv = inproj(pan_v, pk_v, q)
                        pg = inproj(pan_g, pk_g, q)
                        T.op('act', lambda e, i2=i2, pg=pg, c=c: e.activation(
                            out=tsig[i2][:], in_=psb[pg][:, :], func=AF.Sigmoid,
                            bias=b_in_col[:, 8 + c:9 + c], scale=1.0),
                            reads=[('ps', pg)], writes=[('tsig', i2)])
                        T.op('dve', lambda e, i2=i2, pv=pv, c=c, r2=r2, q=q: e.scalar_tensor_tensor(
                            out=glu[r2][:, 30 + 512 * q:30 + 512 * (q + 1)], in0=psb[pv][:, :],
                            scalar=b_in_col[:, c:c + 1], in1=tsig[i2][:], op0=ALU.add, op1=ALU.mult),
                            reads=[('ps', pv), ('tsig', i2)], writes=[('glu', r2, q)])
                    rk = [('glu', r2, q) for q in range(NQ)] + [('glu', r2, 'pad')] + [('dgs', r2, k) for k in range(31)]
                    for q in range(NQ):
                        pc_ = getps()
                        mmgroup(psb[pc_][:, :], ('ps', pc_),
                                [(dgs[r2][:, k, :], glu[r2][:, k + 512 * q:k + 512 * (q + 1)]) for k in range(31)],
                                reads=rk)
                        T.op('act', lambda e, pc_=pc_, c=c, q=q: e.activation(
                            out=yac[:, c, 512 * q:512 * (q + 1)], in_=psb[pc_][:, :], func=AF.Identity,
                            bias=b_cvdw[:, c:c + 1], scale=1.0),
                            reads=[('ps', pc_)], writes=[('yac', c, q)])
            with Scope(A, T) as pb2:
                tsq = [sb("tsq%d" % i, [128, 512], stack=pb2) for i in range(3)]
                mean = sb("cmean", [128, 512], stack=pb2)
                var = sb("cvar", [128, 512], stack=pb2)
                rstd = sb("crstd", [128, 512], stack=pb2)
                nmr = sb("cnmr", [128, 512], stack=pb2)
                tn = [sb("tn%d" % i, [128, 512], stack=pb2) for i in range(3)]
                isq = 0
                itn = 0
                for q in range(NQ):
                    sl = slice(512 * q, 512 * (q + 1))
                    p_s = getps()
                    mmgroup(psb[p_s][:, :], ('ps', p_s), [(ones[:, :], yac[:, c, sl]) for c in range(8)],
                            reads=[])
                    p_q = getps()
                    for c in range(8):
                        j = isq % 3
                        isq += 1
                        T.op('act', lambda e, j=j, c=c, sl=sl: e.activation(
                            out=tsq[j][:], in_=yac[:, c, sl], func=AF.Square),
                            reads=[], writes=[('tsq', j)])
                        T.op('pe', lambda e, j=j, c=c, p_q=p_q: e.matmul(
                            psb[p_q][:, :], lhsT=ones[:, :], rhs=tsq[j][:], start=(c == 0), stop=(c == 7)),
                            reads=[('tsq', j)], writes=[('ps', p_q)], signal=True)
                    T.op('act', lambda e, p_s=p_s: e.activation(
                        out=mean[:], in_=psb[p_s][:, :], func=AF.Identity, scale=1.0 / D),
                        reads=[('ps', p_s)], writes=['cmean'])
                    T.op('dve', lambda e: e.tensor_tensor(out=var[:], in0=mean[:], in1=mean[:], op=ALU.mult),
                         reads=['cmean'], writes=['cvar'])
                    T.op('dve', lambda e, p_q=p_q: e.scalar_tensor_tensor(
                        out=var[:], in0=psb[p_q][:, :], scalar=1.0 / D, in1=var[:],
                        op0=ALU.mult, op1=ALU.subtract),
                        reads=[('ps', p_q), 'cvar'], writes=['cvar'])
                    T.op('act', lambda e: e.activation(out=var[:], in_=var[:], func=AF.Sqrt,
                                                       bias=epsc[:, 0:1], scale=1.0),
                         reads=['cvar'], writes=['cvar'])
                    T.op('dve', lambda e: e.reciprocal(out=rstd[:], in_=var[:]),
                         reads=['cvar'], writes=['crstd'])
                    T.op('dve', lambda e: e.scalar_tensor_tensor(
                        out=nmr[:], in0=mean[:], scalar=-1.0, in1=rstd[:], op0=ALU.mult, op1=ALU.mult),
                        reads=['cmean', 'crstd'], writes=['cnmr'])
                    for c in range(8):
                        j = itn % 3
                        itn += 1
                        T.op('dve', lambda e, j=j, c=c, sl=sl: e.tensor_tensor(
                            out=tn[j][:], in0=yac[:, c, sl], in1=rstd[:], op=ALU.mult),
                            reads=['crstd'], writes=[('tn', j)])
                        T.op('dve', lambda e, j=j: e.tensor_tensor(
                            out=tn[j][:], in0=tn[j][:], in1=nmr[:], op=ALU.add),
                            reads=[('tn', j), 'cnmr'], writes=[('tn', j)])
                        T.op('act', lambda e, j=j, c=c, sl=sl: e.activation(
                            out=ya_act[:, c, sl], in_=tn[j][:], func=AF.Silu,
                            bias=ln_cv_b[:, c:c + 1], scale=ln_cv_g[:, c:c + 1]),
                            reads=[('tn', j)], writes=[('ya_act', c, q)])
        if debug == 'ya_act':
            dbg_out(ya_act)
            return nc

        yb_act, yb_blk = alloc_long([128, 8, S], BF16)
        with Scope(A, T) as pa:
            wla = sb("wla", [128, 8, 128], BF16, stack=pa)
            wlx = sb("wlx", [128, 8, 128], BF16, stack=pa)
            with Scope(A, T) as pa0:
                wla_s = sb("wla_s", [128, 8, 128], stack=pa0)
                wlx_s = sb("wlx_s", [128, 8, 128], stack=pa0)
                T.dma('sp', wla_s[:, :, :], w_lru_a_d[:, :, :], writes=['wla_s'])
                T.dma('sp', wlx_s[:, :, :], w_lru_x_d[:, :, :], writes=['wlx_s'])
                T.op('dve', lambda e: e.tensor_copy(out=wla[:, :, :], in_=wla_s[:, :, :]),
                     reads=['wla_s'], writes=['wla'])
                T.op('act', lambda e: e.copy(out=wlx[:, :, :], in_=wlx_s[:, :, :]),
                     reads=['wlx_s'], writes=['wlx'])
            GSQ = float(np.sqrt(0.044715))
            bgs = sb("bgs", [128, 8], stack=pa)
            T.op('dve', lambda e: e.tensor_scalar(out=bgs[:], in0=b_in_col[:, 24:32], scalar1=GSQ, scalar2=None,
                                                  op0=ALU.mult), writes=['bgs'])
            hba = sb("hba", [128, 8], stack=pa)
            hbx = sb("hbx", [128, 8], stack=pa)
            hcl = sb("hcl", [128, 8], stack=pa)
            T.op('dve', lambda e: e.tensor_scalar(out=hba[:], in0=b_lru_a[:], scalar1=0.5, scalar2=None, op0=ALU.mult),
                 writes=['hba'])
            T.op('dve', lambda e: e.tensor_scalar(out=hbx[:], in0=b_lru_x[:], scalar1=0.5, scalar2=None, op0=ALU.mult),
                 writes=['hbx'])
            T.op('dve', lambda e: e.tensor_scalar(out=hcl[:], in0=clam[:], scalar1=0.5, scalar2=None, op0=ALU.mult),
                 writes=['hcl'])
            NB = 1
            ybp = [sb("ybp%d" % i, [128, 3 + S], stack=pa) for i in range(NB)]
            ybc = [sb("ybc%d" % i, [128, S], stack=pa) for i in range(NB)]
            ybcb = [sb("ybcb%d" % i, [128, S], BF16, stack=pa) for i in range(NB)]
            hh = [sb("hh%d" % i, [128, 512], stack=pa) for i in range(2)]
            thx = sb("thx", [128, S], stack=pa)
            tac = sb("tac", [128, S], stack=pa)
            tmc = sb("tmc", [128, S], stack=pa)
            NTMP = 2
            tga = [sb("tga%d" % i, [128, 512], stack=pa) for i in range(NTMP)]
            tu = [sb("tu%d" % i, [128, 512], stack=pa) for i in range(NTMP)]
            tz = [sb("tz%d" % i, [128, 512], stack=pa) for i in range(NTMP)]
            tp = [sb("tp%d" % i, [128, 512], stack=pa) for i in range(NTMP)]
            tsg = [sb("tsg%d" % i, [128, 512], stack=pa) for i in range(NTMP)]
            for i in range(NB):
                T.op('pool', lambda e, i=i: e.memset(ybp[i][:, 0:3], 0.0), writes=[('ybp', i, 'pad')])
            it = 0
            hcnt = 0
            CG = float(np.sqrt(2.0 / np.pi))
            for c in range(8):
                r2 = c % NB
                pan, pkey = load_panel(w_in_d[:, 2048 + 128 * c:2048 + 128 * (c + 1)])
                for q in range(NQ):
                    pi = inproj(pan, pkey, q)
                    T.op('act', lambda e, pi=pi, q=q, r2=r2, c=c: e.activation(
                        out=ybp[r2][:, 3 + 512 * q:3 + 512 * (q + 1)], in_=psb[pi][:, :],
                        func=AF.Identity, bias=b_in_col[:, 16 + c:17 + c], scale=1.0),
                        reads=[('ps', pi)], writes=[('ybp', r2, q)])
                rk = [('ybp', r2, q) for q in range(NQ)] + [('ybp', r2, 'pad')]
                T.op('dve', lambda e, r2=r2, c=c: e.tensor_scalar(
                    out=ybc[r2][:, :], in0=ybp[r2][:, 0:S], scalar1=w_lconv[:, c, 0:1],
                    scalar2=b_lconv[:, c:c + 1], op0=ALU.mult, op1=ALU.add),
                    reads=rk, writes=[('ybc', r2)])
                for kk in range(1, 4):
                    T.op('dve', lambda e, r2=r2, c=c, kk=kk: e.scalar_tensor_tensor(
                        out=ybc[r2][:, :], in0=ybp[r2][:, kk:kk + S], scalar=w_lconv[:, c, kk:kk + 1],
                        in1=ybc[r2][:, :], op0=ALU.mult, op1=ALU.add),
                        reads=rk + [('ybc', r2)], writes=[('ybc', r2)])
                T.op('act', lambda e, r2=r2: e.copy(out=ybcb[r2][:, :], in_=ybc[r2][:, :]),
                     reads=[('ybc', r2)], writes=[('ybcb', r2)])
                pan_g, pkey_g = load_panel(w_in_d[:, 3072 + 128 * c:3072 + 128 * (c + 1)])
                for q in range(NQ):
                    i3 = it % NTMP
                    it += 1
                    sl = slice(512 * q, 512 * (q + 1))
                    pa_i = getps()
                    mmgroup(psb[pa_i][:, :], ('ps', pa_i), [(wla[:, c, :], ybcb[r2][:, sl])],
                            reads=[('ybcb', r2), 'wla'])
                    px_i = getps()
                    mmgroup(psb[px_i][:, :], ('ps', px_i), [(wlx[:, c, :], ybcb[r2][:, sl])],
                            reads=[('ybcb', r2), 'wlx'])
                    T.op('act', lambda e, i3=i3, pa_i=pa_i, c=c: e.activation(
                        out=tga[i3][:], in_=psb[pa_i][:, :], func=AF.Tanh, bias=hba[:, c:c + 1], scale=0.5),
                        reads=[('ps', pa_i), 'hba'], writes=[('tga', i3)])
                    T.op('act', lambda e, px_i=px_i, c=c, sl=sl: e.activation(
                        out=thx[:, sl], in_=psb[px_i][:, :], func=AF.Tanh, bias=hbx[:, c:c + 1], scale=0.5),
                        reads=[('ps', px_i), 'hbx'], writes=[('thx', q)])
                    T.op('act', lambda e, i3=i3, c=c, sl=sl: e.activation(
                        out=tac[:, sl], in_=tga[i3][:], func=AF.Exp, bias=hcl[:, c:c + 1], scale=hcl[:, c:c + 1]),
                        reads=[('tga', i3), 'hcl'], writes=[('tac', q)])
                    T.op('act', lambda e, i3=i3, c=c, sl=sl: e.activation(
                        out=tmc[:, sl], in_=tga[i3][:], func=AF.Exp, bias=clam[:, c:c + 1], scale=clam[:, c:c + 1]),
                        reads=[('tga', i3)], writes=[('tmc', q)])
                T.op('act', lambda e: e.activation(out=tmc[:, :], in_=tmc[:, :], func=AF.Sqrt, bias=0.25, scale=-0.25),
                     reads=[('tmc', q) for q in range(NQ)], writes=[('tmc', q) for q in range(NQ)])
                T.op('dve', lambda e: e.memset(tmc[:, 0:1], 0.5), reads=[('tmc', 0)], writes=[('tmc', 0)])
                for q in range(NQ):
                    i3 = it % NTMP
                    it += 1
                    sl = slice(512 * q, 512 * (q + 1))
                    T.op('dve', lambda e, i3=i3, r2=r2, sl=sl: e.scalar_tensor_tensor(
                        out=tu[i3][:], in0=thx[:, sl], scalar=1.0, in1=ybc[r2][:, sl], op0=ALU.add, op1=ALU.mult),
                        reads=[('thx', q), ('ybc', r2)], writes=[('tu', i3)])
                    T.op('dve', lambda e, i3=i3, sl=sl: e.tensor_tensor(
                        out=tu[i3][:], in0=tu[i3][:], in1=tmc[:, sl], op=ALU.mult),
                        reads=[('tu', i3), ('tmc', q)], writes=[('tu', i3)])
                    hcur = hcnt % 2
                    hprev = (hcnt - 1) % 2
                    hcnt += 1
                    if q == 0:
                        T.op('dve', lambda e, i3=i3, hcur=hcur, sl=sl: e.tensor_tensor_scan(
                            out=hh[hcur][:], data0=tac[:, sl], data1=tu[i3][:], initial=0.0,
                            op0=ALU.mult, op1=ALU.add),
                            reads=[('tac', q), ('tu', i3)], writes=[('hh', hcur)])
                    else:
                        T.op('dve', lambda e, i3=i3, hcur=hcur, hprev=hprev, sl=sl: e.tensor_tensor_scan(
                            out=hh[hcur][:], data0=tac[:, sl], data1=tu[i3][:],
                            initial=hh[hprev][:, 511:512], op0=ALU.mult, op1=ALU.add),
                            reads=[('tac', q), ('tu', i3), ('hh', hprev)], writes=[('hh', hcur)])
                    pg_i = inproj(pan_g, pkey_g, q)
                    T.op('act', lambda e, i3=i3, pg_i=pg_i, c=c: e.activation(
                        out=tz[i3][:], in_=psb[pg_i][:, :], func=AF.Identity,
                        bias=b_in_col[:, 24 + c:25 + c], scale=1.0),
                        reads=[('ps', pg_i)], writes=[('tz', i3)])
                    T.op('act', lambda e, i3=i3, pg_i=pg_i, c=c: e.activation(
                        out=tp[i3][:], in_=psb[pg_i][:, :], func=AF.Square,
                        bias=bgs[:, c:c + 1], scale=GSQ),
                        reads=[('ps', pg_i)], writes=[('tp', i3)])
                    T.op('dve', lambda e, i3=i3: e.scalar_tensor_tensor(
                        out=tp[i3][:], in0=tp[i3][:], scalar=1.0, in1=tz[i3][:], op0=ALU.add, op1=ALU.mult),
                        reads=[('tp', i3), ('tz', i3)], writes=[('tp', i3)])
                    T.op('act', lambda e, i3=i3: e.activation(
                        out=tsg[i3][:], in_=tp[i3][:], func=AF.Tanh, scale=CG),
                        reads=[('tp', i3)], writes=[('tsg', i3)])
                    T.op('dve', lambda e, i3=i3: e.scalar_tensor_tensor(
                        out=tsg[i3][:], in0=tsg[i3][:], scalar=1.0, in1=tz[i3][:], op0=ALU.add, op1=ALU.mult),
                        reads=[('tsg', i3), ('tz', i3)], writes=[('tsg', i3)])
                    T.op('dve', lambda e, i3=i3, hcur=hcur, c=c, sl=sl: e.scalar_tensor_tensor(
                        out=yb_act[:, c, sl], in0=tsg[i3][:], scalar=0.5, in1=hh[hcur][:], op0=ALU.mult, op1=ALU.mult),
                        reads=[('hh', hcur), ('tsg', i3)], writes=[('yb_act', c, q)])
        if debug == 'yb_act':
            dbg_out(yb_act)
            return nc

        merged, merged_blk = alloc_long([128, 8, S], BF16)
        with Scope(A, T) as pc:
            tga_ = [sb("mga%d" % i, [128, 512], stack=pc) for i in range(2)]
            tgb_ = [sb("mgb%d" % i, [128, 512], stack=pc) for i in range(2)]
            tm1 = [sb("mm1%d" % i, [128, 512], stack=pc) for i in range(2)]
            tm2 = [sb("mm2%d" % i, [128, 512], stack=pc) for i in range(2)]
            it = 0
            for m in range(8):
                pan_a, pk_a = load_panel(w_cv_out_d[:, 128 * m:128 * (m + 1)])
                pan_b, pk_b = load_panel(w_lru_out_d[:, 128 * m:128 * (m + 1)], cast_eng='act')
                pan_ga, pk_ga = load_panel(w_in_d[:, 4096 + 128 * m:4096 + 128 * (m + 1)])
                pan_gb, pk_gb = load_panel(w_in_d[:, 5120 + 128 * m:5120 + 128 * (m + 1)], cast_eng='act')
                for q in range(NQ):
                    i2 = it % 2
                    it += 1
                    sl = slice(512 * q, 512 * (q + 1))
                    p1i = getps()
                    mmgroup(psb[p1i][:, :], ('ps', p1i),
                            [(pan_a[:, k, :], ya_act[:, k, sl]) for k in range(8)], reads=[pk_a])
                    p2i = getps()
                    mmgroup(psb[p2i][:, :], ('ps', p2i),
                            [(pan_b[:, k, :], yb_act[:, k, sl]) for k in range(8)], reads=[pk_b])
                    p3i = inproj(pan_ga, pk_ga, q)
                    p4i = inproj(pan_gb, pk_gb, q)
                    T.op('act', lambda e, i2=i2, p3i=p3i, m=m: e.activation(
                        out=tga_[i2][:], in_=psb[p3i][:, :], func=AF.Sigmoid,
                        bias=b_in_col[:, 32 + m:33 + m], scale=1.0),
                        reads=[('ps', p3i)], writes=[('mga', i2)])
                    T.op('act', lambda e, i2=i2, p4i=p4i, m=m: e.activation(
                        out=tgb_[i2][:], in_=psb[p4i][:, :], func=AF.Sigmoid,
                        bias=b_in_col[:, 40 + m:41 + m], scale=1.0),
                        reads=[('ps', p4i)], writes=[('mgb', i2)])
                    T.op('dve', lambda e, i2=i2, p1i=p1i: e.tensor_tensor(
                        out=tm1[i2][:], in0=psb[p1i][:, :], in1=tga_[i2][:], op=ALU.mult),
                        reads=[('ps', p1i), ('mga', i2)], writes=[('mm1', i2)])
                    T.op('dve', lambda e, i2=i2, p2i=p2i: e.tensor_tensor(
                        out=tm2[i2][:], in0=psb[p2i][:, :], in1=tgb_[i2][:], op=ALU.mult),
                        reads=[('ps', p2i), ('mgb', i2)], writes=[('mm2', i2)])
                    T.op('dve', lambda e, i2=i2, m=m, sl=sl: e.tensor_tensor(
                        out=merged[:, m, sl], in0=tm1[i2][:], in1=tm2[i2][:], op=ALU.add),
                        reads=[('mm1', i2), ('mm2', i2)], writes=[('merged', m, q)])
        p2.__exit__(None, None, None)
        T.barrier()
        for blk in (uT_blk, ya_blk, yb_blk):
            A.release(blk)
        A.commit()

        if debug == 'merged':
            dbg_out(merged)
            return nc

        OH, OH_blk = alloc_long([128, NT, 4, NE], F32)
        with Scope(A, T) as pd:
            wob = sb("wob", [128, 8, D], BF16, stack=pd)
            with Scope(A, T) as pd0:
                wos = [sb("wos%d" % i, [128, D], stack=pd0) for i in range(2)]
                for k in range(8):
                    s1 = k % 2
                    T.dma('sp', wos[s1][:], w_o_d[128 * k:128 * (k + 1), :], writes=[('wos', s1)])
                    if k % 2 == 0:
                        T.op('dve', lambda e, k=k, s1=s1: e.tensor_copy(out=wob[:, k, :], in_=wos[s1][:]),
                             reads=[('wos', s1)], writes=[('wob', k)])
                    else:
                        T.op('act', lambda e, k=k, s1=s1: e.copy(out=wob[:, k, :], in_=wos[s1][:]),
                             reads=[('wos', s1)], writes=[('wob', k)])
            l1g = sb("l1g", [128, D], stack=pd)
            l1b = sb("l1b", [128, D], stack=pd)
            brt = sb("brt", [128, NE], stack=pd)
            g1row = sb("g1row2", [128, D], stack=pd)
            g1bo = sb("g1bo2", [128, D], stack=pd)
            T.dma('sp', l1g[:], ln1_g_d[0:1, :].partition_broadcast(128), writes=['l1g'])
            T.dma('sp', l1b[:], ln1_b_d[0:1, :].partition_broadcast(128), writes=['l1b'])
            T.dma('sp', brt[:], b_router_d[0:1, :].partition_broadcast(128), writes=['brt'])
            T.dma('sp', g1row[:], g1row_d[:, :], writes=['g1row'])
            T.dma('sp', g1bo[:], g1bo_d[:, :], writes=['g1bo'])
            sh2row = sb("sh2row2", [128, D], stack=pd)
            sc2prow = sb("sc2prow2", [128, D], stack=pd)
            T.dma('sp', sh2row[:], sh2row_d[:, :], writes=['sh2row'])
            T.dma('sp', sc2prow[:], sc2prow_d[:, :], writes=['sc2prow'])
            u2m = [sb("u2m%d" % i, [128, D], stack=pd) for i in range(2)]
            u2k = [sb("u2k%d" % i, [128, D], BF16, stack=pd) for i in range(2)]
            e4 = [sb("e4%d" % i, [128, 8], stack=pd) for i in range(2)]
            xt = [sb("xr%d" % i, [128, D], stack=pd) for i in range(2)]
            hp = [sb("hp%d" % i, [128, D], stack=pd) for i in range(2)]
            tq = [sb("tq%d" % i, [128, D], stack=pd) for i in range(2)]
            h1t = [sb("h1t%d" % i, [128, D], stack=pd) for i in range(2)]
            xn2 = [sb("xn2%d" % i, [128, D], stack=pd) for i in range(2)]
            u2f = [sb("u2f%d" % i, [128, 8, 128], stack=pd) for i in range(2)]
            stt = [sb("stt2%d" % i, [128, 12], stack=pd) for i in range(4)]
            mvt = [sb("mvt2%d" % i, [128, 8], stack=pd) for i in range(4)]
            lg = [sb("lg%d" % i, [128, NE], stack=pd) for i in range(2)]
            mx8 = [sb("mx8%d" % i, [128, 8], stack=pd) for i in range(2)]
            ex = [sb("ex%d" % i, [128, NE], stack=pd) for i in range(2)]
            msk = [sb("msk%d" % i, [128, NE], stack=pd) for i in range(2)]
            ssum = [sb("ssum%d" % i, [128, 2], stack=pd) for i in range(2)]
            for tt in range(NT):
                b = tt % 2
                ts_ = slice(128 * tt, 128 * (tt + 1))
                T.dma('sp', xt[b][:], x_d[ts_, :], writes=[('xr', b)])
                pm_ = []
                for n in range(2):
                    pi = getps()
                    mmgroup(psb[pi][:, :], ('ps', pi),
                            [(merged[:, k, ts_], wob[:, k, 512 * n:512 * (n + 1)]) for k in range(8)],
                            reads=[('wob', k) for k in range(8)])
                    pm_.append(pi)
                T.op('dve', lambda e, b=b: e.scalar_tensor_tensor(
                    out=hp[b][:], in0=xt[b][:], scalar=ALPHA, in1=g1bo[:], op0=ALU.mult, op1=ALU.add),
                    reads=[('xr', b), 'g1bo'], writes=[('hp', b)])
                for n in range(2):
                    T.op('dve', lambda e, b=b, n=n, pi=pm_[n]: e.tensor_tensor(
                        out=tq[b][:, 512 * n:512 * (n + 1)], in0=psb[pi][:, :],
                        in1=g1row[:, 512 * n:512 * (n + 1)], op=ALU.mult),
                        reads=[('ps', pm_[n]), 'g1row'], writes=[('tq', b, n)])
                T.op('dve', lambda e, b=b: e.tensor_tensor(out=hp[b][:], in0=hp[b][:], in1=tq[b][:], op=ALU.add),
                     reads=[('hp', b), ('tq', b, 0), ('tq', b, 1)], writes=[('hp', b)])
                rs, nm, keys = layernorm_rows(hp[b], ('hp', b), h1t[b], ('h1t', b), mvt[b], ('a', b))
                T.op('act', lambda e, b=b, rs=rs, nm=nm: e.activation(
                    out=h1t[b][:], in_=hp[b][:], func=AF.Identity, bias=nm, scale=rs),
                    reads=[('hp', b)] + keys, writes=[('h1t', b)])
                T.op('dve', lambda e, b=b: e.tensor_tensor(out=h1t[b][:], in0=h1t[b][:], in1=l1g[:], op=ALU.mult),
                     reads=[('h1t', b), 'l1g'], writes=[('h1t', b)])
                T.op('dve', lambda e, b=b: e.tensor_tensor(out=h1t[b][:], in0=h1t[b][:], in1=l1b[:], op=ALU.add),
                     reads=[('h1t', b), 'l1b'], writes=[('h1t', b)])
                T.dma('sp', h1_d[ts_, :], h1t[b][:], reads=[('h1t', b)])
                rs2, nm2, keys2 = layernorm_rows(h1t[b], ('h1t', b), xn2[b], ('xn2', b), mvt[2 + b], ('b', b))
                T.op('act', lambda e, b=b, rs2=rs2, nm2=nm2: e.activation(
                    out=xn2[b][:], in_=h1t[b][:], func=AF.Identity, bias=nm2, scale=rs2),
                    reads=[('h1t', b)] + keys2, writes=[('xn2', b)])
                for half in range(2):
                    pi = getps()
                    transpose4(pi, xn2[b], half * 4, ('xn2', b))
                    for q in range(4):
                        k = half * 4 + q
                        if half == 0:
                            T.op('dve', lambda e, pi=pi, q=q, k=k, b=b: e.tensor_scalar(
                                out=u2f[b][:, k, :], in0=psb[pi][:, 128 * q:128 * (q + 1)],
                                scalar1=sc2p[:, k:k + 1], scalar2=mod_col[:, 24 + k:25 + k],
                                op0=ALU.mult, op1=ALU.add),
                                reads=[('ps', pi)], writes=[('u2f', b, k)])
                        else:
                            T.op('act', lambda e, pi=pi, q=q, k=k, b=b: e.activation(
                                out=u2f[b][:, k, :], in_=psb[pi][:, 128 * q:128 * (q + 1)], func=AF.Identity,
                                bias=mod_col[:, 24 + k:25 + k], scale=sc2p[:, k:k + 1]),
                                reads=[('ps', pi)], writes=[('u2f', b, k)])
                T.op('dve', lambda e, b=b: e.tensor_tensor(out=u2m[b][:], in0=xn2[b][:], in1=sc2prow[:], op=ALU.mult),
                     reads=[('xn2', b), 'sc2prow'], writes=[('u2m', b)])
                T.op('dve', lambda e, b=b: e.tensor_tensor(out=u2k[b][:], in0=u2m[b][:], in1=sh2row[:], op=ALU.add),
                     reads=[('u2m', b), 'sh2row'], writes=[('u2k', b)])
                T.dma('sp', u2tok_d[ts_, :], u2k[b][:], reads=[('u2k', b)])
                pl = getps()
                mmgroup(psb[pl][:, 0:NE], ('ps', pl),
                        [(u2f[b][:, k, :], w_router[:, k, :]) for k in range(8)],
                        reads=[('u2f', b, k) for k in range(8)])
                T.op('dve', lambda e, b=b, pl=pl: e.tensor_tensor(
                    out=lg[b][:], in0=psb[pl][:, 0:NE], in1=brt[:], op=ALU.add),
                    reads=[('ps', pl), 'brt'], writes=[('lg', b)])
                T.op('dve', lambda e, b=b: e.max(out=mx8[b][:], in_=lg[b][:]),
                     reads=[('lg', b)], writes=[('mx8', b)])
                T.op('dve', lambda e, b=b: e.tensor_scalar(
                    out=msk[b][:], in0=lg[b][:], scalar1=mx8[b][:, 3:4], scalar2=None, op0=ALU.is_ge),
                    reads=[('lg', b), ('mx8', b)], writes=[('msk', b)])
                T.op('dve', lambda e, b=b: e.tensor_scalar(
                    out=ex[b][:], in0=lg[b][:], scalar1=mx8[b][:, 0:1], scalar2=None, op0=ALU.subtract),
                    reads=[('lg', b), ('mx8', b)], writes=[('ex', b)])
                T.op('act', lambda e, b=b: e.activation(out=ex[b][:], in_=ex[b][:], func=AF.Exp),
                     reads=[('ex', b)], writes=[('ex', b)])
                T.op('dve', lambda e, b=b: e.tensor_tensor(out=ex[b][:], in0=ex[b][:], in1=msk[b][:], op=ALU.mult),
                     reads=[('ex', b), ('msk', b)], writes=[('ex', b)])
                T.op('dve', lambda e, b=b: e.tensor_reduce(out=ssum[b][:, 0:1], in_=ex[b][:], axis=AX.X, op=ALU.add),
                     reads=[('ex', b)], writes=[('ssum', b)])
                T.op('dve', lambda e, b=b: e.reciprocal(out=ssum[b][:, 1:2], in_=ssum[b][:, 0:1]),
                     reads=[('ssum', b)], writes=[('ssum', b)])
                T.op('dve', lambda e, b=b, tt=tt: e.tensor_scalar(
                    out=G[:, tt, :], in0=ex[b][:], scalar1=ssum[b][:, 1:2], scalar2=None, op0=ALU.mult),
                    reads=[('ex', b), ('ssum', b)], writes=[('G', tt)])
                for j in range(4):
                    T.op('dve', lambda e, b=b, tt=tt, j=j: e.tensor_scalar(
                        out=OH[:, tt, j, :], in0=lg[b][:], scalar1=mx8[b][:, j:j + 1], scalar2=None,
                        op0=ALU.is_equal), reads=[('lg', b), ('mx8', b)], writes=[('OH', tt, j)])
                T.op('dve', lambda e, b=b: e.tensor_scalar(
                    out=e4[b][:, 6:7], in0=mx8[b][:, 0:1], scalar1=-1.0, scalar2=None, op0=ALU.mult),
                    reads=[('mx8', b)], writes=[('e4n', b)])
                T.op('act', lambda e, b=b: e.activation(out=e4[b][:, 0:4], in_=mx8[b][:, 0:4], func=AF.Exp,
                                                        bias=e4[b][:, 6:7], scale=1.0),
                     reads=[('mx8', b), ('e4n', b)], writes=[('e4', b)])
                T.op('dve', lambda e, b=b: e.tensor_reduce(out=e4[b][:, 4:5], in_=e4[b][:, 0:4], axis=AX.X, op=ALU.add),
                     reads=[('e4', b)], writes=[('e4s', b)])
                T.op('dve', lambda e, b=b: e.reciprocal(out=e4[b][:, 5:6], in_=e4[b][:, 4:5]),
                     reads=[('e4s', b)], writes=[('e4r', b)])
                T.op('dve', lambda e, b=b, tt=tt: e.tensor_scalar(
                    out=GT[:, tt, :], in0=e4[b][:, 0:4], scalar1=e4[b][:, 5:6], scalar2=float(1.0 / 1.702),
                    op0=ALU.mult, op1=ALU.mult), reads=[('e4', b), ('e4r', b)], writes=[('GT', tt)])
        free_long(merged_blk)

        if debug == 'h1':
            with Scope(A, T) as dd:
                for tt in range(NT):
                    dt_ = sb("dbgt", [128, D], stack=dd)
                    T.dma('sp', dt_[:], h1_d[128 * tt:128 * (tt + 1), :], writes=[('dbgt', tt)])
                    T.dma('sp', dbg_d[:, D * tt:D * (tt + 1)], dt_[:], reads=[('dbgt', tt)])
            T.finish()
            return nc
        if debug == 'G':
            with Scope(A, T) as dd:
                dt_ = sb("dbgt", [128, 8 * S], stack=dd)
                T.op('dve', lambda e: e.memset(dt_[:], 0.0), writes=['dbgt'])
                T.op('dve', lambda e: e.tensor_copy(out=dt_[:, 0:NT * NE], in_=G[:, :, :].rearrange("p t e -> p (t e)")),
                     reads=['dbgt'], writes=['dbgt'])
                T.op('dve', lambda e: e.tensor_copy(out=dt_[:, 1024:1024 + NT * 4], in_=GT[:, :, :].rearrange("p t j -> p (t j)")),
                     reads=['dbgt'], writes=['dbgt'])
                T.dma('sp', dbg_d[:, :], dt_[:, :], reads=['dbgt'])
            T.finish()
            return nc

        with Scope(A, T) as pr:
            MK = sb("MK", [128, NT, NE], stack=pr)
            T.op('dve', lambda e: e.tensor_tensor(out=MK[:, :, :], in0=OH[:, :, 0, :], in1=OH[:, :, 1, :], op=ALU.add),
                 writes=['MK'])
            T.op('dve', lambda e: e.tensor_tensor(out=MK[:, :, :], in0=MK[:, :, :], in1=OH[:, :, 2, :], op=ALU.add),
                 reads=['MK'], writes=['MK'])
            T.op('dve', lambda e: e.tensor_tensor(out=MK[:, :, :], in0=MK[:, :, :], in1=OH[:, :, 3, :], op=ALU.add),
                 reads=['MK'], writes=['MK'])
            erow = sb("erow", [128, NE], stack=pr)
            ecol = sb("ecol", [128, 1], stack=pr)
            iou = sb("iou", [128, 16], stack=pr)
            iod = sb("iod", [128, 8], stack=pr)
            r31row = sb("r31row", [128, NE], stack=pr)
            r31col = sb("r31col", [128, 1], stack=pr)
            T.op('pool', lambda e: e.iota(out=erow[:], pattern=[[1, NE]], base=0, channel_multiplier=0,
                                          allow_small_or_imprecise_dtypes=True), writes=['erow'])
            T.op('pool', lambda e: e.iota(out=ecol[:], pattern=[[0, 1]], base=0, channel_multiplier=1,
                                          allow_small_or_imprecise_dtypes=True), writes=['ecol'])
            T.op('pool', lambda e: e.iota(out=iou[:], pattern=[[256, 8], [1, 2]], base=0, channel_multiplier=2,
                                          allow_small_or_imprecise_dtypes=True), writes=['iou'])
            T.op('pool', lambda e: e.iota(out=iod[:], pattern=[[128, 8]], base=0, channel_multiplier=1,
                                          allow_small_or_imprecise_dtypes=True), writes=['iod'])
            T.op('dve', lambda e: e.tensor_scalar(out=r31row[:], in0=erow[:], scalar1=-1.0, scalar2=31.0,
                                                  op0=ALU.mult, op1=ALU.add), reads=['erow'], writes=['r31row'])
            T.op('dve', lambda e: e.tensor_scalar(out=r31col[:], in0=ecol[:], scalar1=-1.0, scalar2=31.0,
                                                  op0=ALU.mult, op1=ALU.add), reads=['ecol'], writes=['r31col'])
            pc_ = getps()
            mmgroup(psb[pc_][0:NE, 0:1], ('ps', pc_), [(MK[:, tt, :], ones[:, 0:1]) for tt in range(NT)], reads=['MK'])
            pw_ = getps()
            mmgroup(psb[pw_][0:NE, 0:NE], ('ps', pw_), [(ones[:, 0:NE], MK[:, tt, :]) for tt in range(NT)], reads=['MK'])
            keyc = sb("keyc", [128, 1], stack=pr)
            keyr = sb("keyr", [128, NE], stack=pr)
            gtm = sb("gtm", [128, NE], stack=pr)
            rankc = sb("rankc", [128, 1], stack=pr)
            Pm = sb("Pm", [128, NE], stack=pr)
            cumrow = sb("cumrow", [128, NE], stack=pr)
            basec = sb("basec", [128, 1], stack=pr)
            diagB = sb("diagB", [128, NE], stack=pr)
            ecolb = sb("ecolb", [128, 128], stack=pr)
            pib = sb("pib", [128, NE], stack=pr)
            Um = sb("Um", [128, 128], stack=pr)
            T.op('dve', lambda e: e.scalar_tensor_tensor(out=keyc[0:NE, :], in0=psb[pc_][0:NE, 0:1], scalar=32.0,
                                                         in1=r31col[0:NE, :], op0=ALU.mult, op1=ALU.add),
                 reads=[('ps', pc_), 'r31col'], writes=['keyc'])
            T.op('dve', lambda e: e.scalar_tensor_tensor(out=keyr[0:NE, :], in0=psb[pw_][0:NE, 0:NE], scalar=32.0,
                                                         in1=r31row[0:NE, :], op0=ALU.mult, op1=ALU.add),
                 reads=[('ps', pw_), 'r31row'], writes=['keyr'])
            T.op('dve', lambda e: e.tensor_scalar(out=gtm[0:NE, :], in0=keyr[0:NE, :], scalar1=keyc[0:NE, 0:1],
                                                  scalar2=None, op0=ALU.is_gt),
                 reads=['keyr', 'keyc'], writes=['gtm'])
            T.op('dve', lambda e: e.tensor_reduce(out=rankc[0:NE, :], in_=gtm[0:NE, :], axis=AX.X, op=ALU.add),
                 reads=['gtm'], writes=['rankc'])
            T.op('dve', lambda e: e.tensor_scalar(out=Pm[0:NE, :], in0=erow[0:NE, :], scalar1=rankc[0:NE, 0:1],
                                                  scalar2=None, op0=ALU.is_equal),
                 reads=['erow', 'rankc'], writes=['Pm'])
            for r in range(NE):
                T.op('dve', lambda e, r=r: e.memset(cumrow[:, r:r + 1], float(CUM[r])), writes=[('cumrow', r)])
            T.op('dve', lambda e: e.tensor_tensor(out=gtm[0:NE, :], in0=Pm[0:NE, :], in1=cumrow[0:NE, :], op=ALU.mult),
                 reads=['Pm', 'gtm'] + [('cumrow', r) for r in range(NE)], writes=['gtm'])
            T.op('dve', lambda e: e.tensor_reduce(out=basec[0:NE, :], in_=gtm[0:NE, :], axis=AX.X, op=ALU.add),
                 reads=['gtm'], writes=['basec'])
            T.op('dve', lambda e: e.tensor_scalar(out=diagB[0:NE, :], in0=ident[0:NE, 0:NE], scalar1=basec[0:NE, 0:1],
                                                  scalar2=None, op0=ALU.mult), reads=['basec'], writes=['diagB'])
            T.op('dve', lambda e: e.tensor_copy(out=ecolb[0:NE, :], in_=ecol[0:NE, 0:1].to_broadcast([NE, 128])),
                 reads=['ecol'], writes=['ecolb'])
            pp_ = getps()
            mmgroup(psb[pp_][:, 0:NE], ('ps', pp_), [(ecolb[0:NE, :], Pm[0:NE, :])], reads=['ecolb', 'Pm'])
            T.op('dve', lambda e: e.tensor_copy(out=pib[:], in_=psb[pp_][:, 0:NE]), reads=[('ps', pp_)], writes=['pib'])
            T.op('pool', lambda e: e.affine_select(out=Um[:], in_=ones[:], pattern=[[1, 128]], compare_op=ALU.is_gt,
                                                   fill=0.0, base=0, channel_multiplier=-1), writes=['Um'])
            slotf = sb("slotf", [128, NT * 4], stack=pr)
            tsl = [sb("tsl%d" % i, [128, NE], stack=pr) for i in range(2)]
            isl = 0
            for tt in range(NT):
                ps_ = getps()
                pairs = [(ones[:, :], MK[:, tp, :]) for tp in range(tt)] + [(Um[:, :], MK[:, tt, :])] + \
                        [(ones[0:NE, :], diagB[0:NE, :])]
                mmgroup(psb[ps_][:, 0:NE], ('ps', ps_), pairs, reads=['MK', 'Um', 'diagB'])
                for j in range(4):
                    i2 = isl % 2
                    isl += 1
                    T.op('dve', lambda e, i2=i2, tt=tt, j=j, ps_=ps_: e.tensor_tensor(
                        out=tsl[i2][:], in0=psb[ps_][:, 0:NE], in1=OH[:, tt, j, :], op=ALU.mult),
                        reads=[('ps', ps_)], writes=[('tsl', i2)])
                    T.op('dve', lambda e, i2=i2, tt=tt, j=j: e.tensor_reduce(
                        out=slotf[:, 4 * tt + j:4 * tt + j + 1], in_=tsl[i2][:], axis=AX.X, op=ALU.add),
                        reads=[('tsl', i2)], writes=[('slotf', tt, j)])
            T.op('dve', lambda e: e.tensor_copy(out=SLOT[:, :], in_=slotf[:, :]),
                 reads=[('slotf', tt, j) for tt in range(NT) for j in range(4)], writes=['SLOT'])
            wif = sb("wif", [128, NE, 16], stack=pr)
            wdf = sb("wdf", [128, NE, 8], stack=pr)
            bif = sb("bif", [128, NE], stack=pr)
            pib2 = sb("pib2", [128, NE], stack=pr)
            T.op('dve', lambda e: e.tensor_scalar(out=pib2[:], in0=pib[:], scalar1=2048.0, scalar2=None, op0=ALU.mult),
                 reads=['pib'], writes=['pib2'])
            for r in range(NE):
                T.op('dve', lambda e, r=r: e.tensor_scalar(out=wif[:, r, :], in0=iou[:], scalar1=pib2[:, r:r + 1],
                                                           scalar2=None, op0=ALU.add),
                     reads=['iou', 'pib2'], writes=[('wif', r)])
            T.op('dve', lambda e: e.tensor_copy(out=WIU[:, :, :], in_=wif[:, :, :]),
                 reads=[('wif', r) for r in range(NE)], writes=['WIU'])
            T.op('dve', lambda e: e.tensor_scalar(out=pib2[:], in0=pib[:], scalar1=1024.0, scalar2=None, op0=ALU.mult),
                 reads=['pib', 'pib2'] + [('wif', r) for r in range(NE)], writes=['pib2'])
            for r in range(NE):
                T.op('dve', lambda e, r=r: e.tensor_scalar(out=wdf[:, r, :], in0=iod[:], scalar1=pib2[:, r:r + 1],
                                                           scalar2=None, op0=ALU.add),
                     reads=['iod', 'pib2'], writes=[('wdf', r)])
            T.op('dve', lambda e: e.tensor_copy(out=WID[:, :, :], in_=wdf[:, :, :]),
                 reads=[('wdf', r) for r in range(NE)], writes=['WID'])
            T.op('dve', lambda e: e.scalar_tensor_tensor(out=bif[:], in0=pib[:], scalar=128.0,
                                                         in1=ecol[:, 0:1].to_broadcast([128, NE]),
                                                         op0=ALU.mult, op1=ALU.add),
                 reads=['pib', 'ecol'], writes=['bif'])
            T.op('dve', lambda e: e.tensor_copy(out=BIU[:, :], in_=bif[:, :]), reads=['bif'], writes=['BIU'])
            utile = [sb("utile%d" % i, [128, D], BF16, stack=pr) for i in range(2)]
            for tt in range(NT):
                b = tt % 2
                T.dma('sp', utile[b][:], u2tok_d[128 * tt:128 * (tt + 1), :], writes=[('utile', b)])
                for j in range(4):
                    T.dma('pool', xs_d[:, :], utile[b][:, :], reads=[('utile', b), 'SLOT'], writes=[('xs', tt, j)],
                          out_offset=bass.IndirectOffsetOnAxis(ap=SLOT[:, 4 * tt + j:4 * tt + j + 1], axis=0))
        free_long(OH_blk)
        if debug == 'route':
            with Scope(A, T) as dd:
                dt_ = sb("dbgt", [128, 2048], stack=dd)
                T.op('dve', lambda e: e.memset(dt_[:], 0.0), writes=['dbgt'])
                T.op('dve', lambda e: e.tensor_copy(out=dt_[:, 0:64], in_=SLOT[:, :]), reads=['dbgt'], writes=['dbgt'])
                T.op('dve', lambda e: e.tensor_copy(out=dt_[:, 64:64 + 512], in_=WIU[:, :, :].rearrange("p r j -> p (r j)")),
                     reads=['dbgt'], writes=['dbgt'])
                T.op('dve', lambda e: e.tensor_copy(out=dt_[:, 576:576 + 256], in_=WID[:, :, :].rearrange("p r j -> p (r j)")),
                     reads=['dbgt'], writes=['dbgt'])
                T.op('dve', lambda e: e.tensor_copy(out=dt_[:, 832:832 + 32], in_=BIU[:, :]), reads=['dbgt'], writes=['dbgt'])
                T.op('dve', lambda e: e.tensor_copy(out=dt_[:, 896:896 + 64], in_=GT[:, :, :].rearrange("p t j -> p (t j)")),
                     reads=['dbgt'], writes=['dbgt'])
                T.dma('sp', dbg_d[:, 0:2048], dt_[:, :], reads=['dbgt'])
            T.finish()
            return nc

        with Scope(A, T) as pe_:
            ugT = [sb("ugT0", [128, 8, 2048], BF16, stack=pe_), sb("ugT1", [128, 8, 512], BF16, stack=pe_)]
            actT = sb("actT", [128, 8, 2048], BF16, stack=pe_)
            NHB = 3
            wugh = [sb("wugh%d" % i, [128, 8, 4, 128], BF16, stack=pe_) for i in range(NHB)]
            wulh = [sb("wulh%d" % i, [128, 8, 4, 128], BF16, stack=pe_) for i in range(NHB)]
            wdb = sb("wdb", [128, 8, D], BF16, stack=pe_)
            NSTG = 4
            NXS = 6
            stg = [sb("stg%d" % i, [128, D], stack=pe_) for i in range(NSTG)]
            xst = [sb("xst%d" % i, [128, D], BF16, stack=pe_) for i in range(NXS)]
            osb = [sb("osb%d" % i, [128, D], stack=pe_) for i in range(2)]
            bu = [sb("bu%d" % i, [128, 16], stack=pe_) for i in range(2)]
            b7 = [sb("b7%d" % i, [128, 8], stack=pe_) for i in range(2)]
            b1 = [sb("b1%d" % i, [128, 8], stack=pe_) for i in range(2)]
            NZ = 2
            zg = [sb("zg%d" % i, [128, 512], stack=pe_) for i in range(NZ)]
            sg = [sb("sg%d" % i, [128, 512], stack=pe_) for i in range(NZ)]
            zl = [sb("zl%d" % i, [128, 512], stack=pe_) for i in range(NZ)]
            cnts = {'stg': 0, 'xst': 0, 'osb': 0, 'z': 0, 'cast': 0}

            def next_stg():
                s = cnts['stg'] % NSTG
                cnts['stg'] += 1
                return s

            def cast_eng():
                cnts['cast'] += 1
                return 'act' if cnts['cast'] % 2 == 0 else 'dve'

            def load_half(r, h, hb):
                for k in range(8):
                    s = next_stg()
                    T.dma('pool', stg[s][:, :], w_up2[:, :], reads=['WIU'], writes=[('stg', s)],
                          in_offset=bass.IndirectOffsetOnAxis(ap=WIU[:, r, 2 * k + h:2 * k + h + 1], axis=0))
                    v = stg[s][:, :].rearrange("p (a b c) -> p a b c", a=4, b=128, c=2)
                    ce = cast_eng()
                    if ce == 'act':
                        T.op('act', lambda e, v=v, k=k: e.copy(out=wugh[hb][:, k, :, :], in_=v[:, :, :, 0]),
                             reads=[('stg', s)], writes=[('wugh', hb, k)])
                        T.op('dve', lambda e, v=v, k=k: e.tensor_copy(out=wulh[hb][:, k, :, :], in_=v[:, :, :, 1]),
                             reads=[('stg', s)], writes=[('wulh', hb, k)])
                    else:
                        T.op('dve', lambda e, v=v, k=k: e.tensor_copy(out=wugh[hb][:, k, :, :], in_=v[:, :, :, 0]),
                             reads=[('stg', s)], writes=[('wugh', hb, k)])
                        T.op('act', lambda e, v=v, k=k: e.copy(out=wulh[hb][:, k, :, :], in_=v[:, :, :, 1]),
                             reads=[('stg', s)], writes=[('wulh', hb, k)])

            def load_bias(r):
                rb = posof[r] % 2
                T.dma('pool', bu[rb][:, :], b_up_d[:, :], reads=['BIU'], writes=[('bu', rb)],
                      in_offset=bass.IndirectOffsetOnAxis(ap=BIU[:, r:r + 1], axis=0))
                T.op('dve', lambda e: e.tensor_scalar(out=b7[rb][:], in0=bu[rb][:, 0:16:2], scalar1=-1.0, scalar2=7.0,
                                                      op0=ALU.mult, op1=ALU.add), reads=[('bu', rb)], writes=[('b7', rb)])
                T.op('dve', lambda e: e.tensor_scalar(out=b1[rb][:], in0=bu[rb][:, 1:16:2], scalar1=1.0, scalar2=None,
                                                      op0=ALU.add), reads=[('bu', rb)], writes=[('b1', rb)])

            def load_down_chunk(r, k):
                s = next_stg()
                T.dma('pool', stg[s][:, :], w_down2[:, :], reads=['WID'], writes=[('stg', s)],
                      in_offset=bass.IndirectOffsetOnAxis(ap=WID[:, r, k:k + 1], axis=0))
                ce = cast_eng()
                if ce == 'act':
                    T.op('act', lambda e: e.copy(out=wdb[:, k, :], in_=stg[s][:]), reads=[('stg', s)], writes=[('wdb', k)])
                else:
                    T.op('dve', lambda e: e.tensor_copy(out=wdb[:, k, :], in_=stg[s][:]),
                         reads=[('stg', s)], writes=[('wdb', k)])

            def build_ug(r):
                ub = posof[r] % 2
                for blk in range(CR[r] // 128):
                    xb_ = cnts['xst'] % NXS
                    cnts['xst'] += 1
                    row0 = CUM[r] + 128 * blk
                    T.dma('sp', xst[xb_][:], xs_d[row0:row0 + 128, :], writes=[('xst', xb_)])
                    pi = getps()
                    psv = psb[pi][:, :].bitcast(BF16)
                    for k in range(8):
                        T.op('pe', lambda e, k=k, psv=psv, xb_=xb_: e.transpose(
                            out=psv[:, 128 * k:128 * (k + 1)], in_=xst[xb_][:, 128 * k:128 * (k + 1)],
                            identity=identb[:]),
                            reads=[('xst', xb_)] if k == 0 else (), writes=[('ps', pi)] if k == 0 else (),
                            signal=(k == 7))
                    T._record((T.own['pe'], T.cnt['pe']), [('xst', xb_)], [('ps', pi)])
                    dstv = ugT[ub][:, :, 128 * blk:128 * (blk + 1)]
                    srcv = psv.rearrange("p (k s) -> p k s", k=8)
                    if blk % 2 == 0:
                        T.op('act', lambda e, dstv=dstv, srcv=srcv: e.copy(out=dstv, in_=srcv),
                             reads=[('ps', pi)], writes=[('ug', ub, blk)])
                    else:
                        T.op('dve', lambda e, dstv=dstv, srcv=srcv: e.tensor_copy(out=dstv, in_=srcv),
                             reads=[('ps', pi)], writes=[('ug', ub, blk)])

            order = []
            for i_ in range(NE // 2):
                order += [i_, NE - 1 - i_]
            posof = {r_: p_ for p_, r_ in enumerate(order)}
            halves = [(r, h) for r in order for h in range(2)]
            assert all(CR[r_] <= 512 for r_ in order[1::2])
            load_bias(order[0])
            load_half(order[0], 0, 0)
            load_half(order[0], 1, 1)
            build_ug(order[0])
            for ih, (r, h) in enumerate(halves):
                hb = ih % NHB
                ub = posof[r] % 2
                rb = posof[r] % 2
                C = CR[r]
                if ih + 2 < len(halves):
                    r2_, h2_ = halves[ih + 2]
                    if h2_ == 0:
                        load_bias(r2_)
                    load_half(r2_, h2_, (ih + 2) % NHB)
                tiles = [(o, min(512, C - o)) for o in range(0, C, 512)]
                for hp4 in range(4):
                    hp_ = 4 * h + hp4
                    for (o, n) in tiles:
                        i3 = cnts['z'] % NZ
                        cnts['z'] += 1
                        ugk = [('ug', ub, b_) for b_ in range(o // 128, (o + n) // 128)]
                        pg = getps()
                        mmgroup(psb[pg][:, 0:n], ('ps', pg),
                                [(wugh[hb][:, k, hp4, :], ugT[ub][:, k, o:o + n]) for k in range(8)],
                                reads=[('wugh', hb, k) for k in range(8)] + ugk)
                        pl = getps()
                        mmgroup(psb[pl][:, 0:n], ('ps', pl),
                                [(wulh[hb][:, k, hp4, :], ugT[ub][:, k, o:o + n]) for k in range(8)],
                                reads=[('wulh', hb, k) for k in range(8)] + ugk)
                        T.op('act', lambda e, i3=i3, pg=pg, rb=rb, hp_=hp_, n=n: e.activation(
                            out=zg[i3][:, 0:n], in_=psb[pg][:, 0:n], func=AF.Relu,
                            bias=b7[rb][:, hp_:hp_ + 1], scale=-1.0),
                            reads=[('ps', pg), ('b7', rb)], writes=[('zg', i3)])
                        T.op('act', lambda e, i3=i3, n=n: e.activation(
                            out=sg[i3][:, 0:n], in_=zg[i3][:, 0:n], func=AF.Silu, bias=float(7.0 * 1.702), scale=-1.702),
                            reads=[('zg', i3)], writes=[('sg', i3)])
                        T.op('dve', lambda e, i3=i3, pl=pl, rb=rb, hp_=hp_, n=n: e.tensor_scalar(
                            out=zl[i3][:, 0:n], in0=psb[pl][:, 0:n], scalar1=b1[rb][:, hp_:hp_ + 1], scalar2=8.0,
                            op0=ALU.add, op1=ALU.min), reads=[('ps', pl), ('b1', rb)], writes=[('zl', i3)])
                        T.op('dve', lambda e, i3=i3, hp_=hp_, o=o, n=n: e.scalar_tensor_tensor(
                            out=actT[:, hp_, o:o + n], in0=zl[i3][:, 0:n], scalar=-6.0, in1=sg[i3][:, 0:n],
                            op0=ALU.max, op1=ALU.mult),
                            reads=[('sg', i3), ('zl', i3)], writes=[('act', hp_, o // 512)])
                    load_down_chunk(r, hp_)
                if h == 0:
                    continue
                if posof[r] + 1 < NE:
                    build_ug(order[posof[r] + 1])
                for blk in range(C // 128):
                    ob = cnts['osb'] % 2
                    cnts['osb'] += 1
                    for n_ in range(2):
                        pi = getps()
                        mmgroup(psb[pi][:, :], ('ps', pi),
                                [(actT[:, k, 128 * blk:128 * (blk + 1)], wdb[:, k, 512 * n_:512 * (n_ + 1)])
                                 for k in range(8)],
                                reads=[('wdb', k) for k in range(8)] + [('act', k, blk // 4) for k in range(8)])
                        if n_ == 0:
                            T.op('act', lambda e, pi=pi, ob=ob: e.copy(out=osb[ob][:, 0:512], in_=psb[pi][:, :]),
                                 reads=[('ps', pi)], writes=[('osb', ob, 0)])
                        else:
                            T.op('dve', lambda e, pi=pi, ob=ob: e.tensor_copy(out=osb[ob][:, 512:1024], in_=psb[pi][:, :]),
                                 reads=[('ps', pi)], writes=[('osb', ob, 1)])
                    row0 = CUM[r] + 128 * blk
                    T.dma('sp', ys_d[row0:row0 + 128, :], osb[ob][:], reads=[('osb', ob, 0), ('osb', ob, 1)])

        yacc, yacc_blk = alloc_long([128, NT, D], F32)
        with Scope(A, T) as pi0:
            b_down = sb("b_down_s", [128, D], stack=pi0)
            gts = [sb("gts%d" % i, [128, 128], stack=pi0) for i in range(2)]
            ygt = [sb("ygt%d" % i, [128, D], stack=pi0) for i in range(3)]
            T.dma('sp', b_down[0:NE, :], b_down_d[:, :], writes=['b_down'])
            l2g = sb("l2g", [128, D], stack=pi0)
            l2b = sb("l2b", [128, D], stack=pi0)
            g2row = sb("g2row2", [128, D], stack=pi0)
            T.dma('sp', l2g[:], ln2_g_d[0:1, :].partition_broadcast(128), writes=['l2g'])
            T.dma('sp', l2b[:], ln2_b_d[0:1, :].partition_broadcast(128), writes=['l2b'])
            T.dma('sp', g2row[:], g2row_d[:, :], writes=['g2row'])
            h1r = [sb("h1r%d" % i, [128, D], stack=pi0) for i in range(2)]
            fo = [sb("fo%d" % i, [128, D], stack=pi0) for i in range(2)]
            mvt = [sb("mvt3%d" % i, [128, 8], stack=pi0) for i in range(2)]

            def p4_tile(tt):
                b = tt % 2
                ts_ = slice(128 * tt, 128 * (tt + 1))
                T.dma('sp', h1r[b][:], h1_d[ts_, :], writes=[('h1r', b)])
                T.op('dve', lambda e: e.tensor_tensor(out=yacc[:, tt, :], in0=yacc[:, tt, :], in1=g2row[:], op=ALU.mult),
                     reads=[('y', tt, 0), ('y', tt, 1), 'g2row'], writes=[('y', tt, 0), ('y', tt, 1)])
                T.op('dve', lambda e: e.scalar_tensor_tensor(
                    out=fo[b][:], in0=h1r[b][:], scalar=ALPHA, in1=yacc[:, tt, :], op0=ALU.mult, op1=ALU.add),
                    reads=[('h1r', b), ('y', tt, 0), ('y', tt, 1)], writes=[('fo', b)])
                rs, nm, keys = layernorm_rows(fo[b], ('fo', b), h1r[b], ('h1r', b), mvt[b], ('c', b))
                T.op('act', lambda e: e.activation(out=fo[b][:], in_=fo[b][:], func=AF.Identity, bias=nm, scale=rs),
                     reads=[('fo', b)] + keys, writes=[('fo', b)])
                T.op('dve', lambda e: e.tensor_tensor(out=fo[b][:], in0=fo[b][:], in1=l2g[:], op=ALU.mult),
                     reads=[('fo', b), 'l2g'], writes=[('fo', b)])
                T.op('dve', lambda e: e.tensor_tensor(out=fo[b][:], in0=fo[b][:], in1=l2b[:], op=ALU.add),
                     reads=[('fo', b), 'l2b'], writes=[('fo', b)])
                T.dma('sp', out_d[ts_, :], fo[b][:], reads=[('fo', b)])

            ig = 0
            for tt in range(NT):
                b = tt % 2
                pg_ = getps()
                T.op('pe', lambda e, pg_=pg_, tt=tt: e.transpose(
                    out=psb[pg_][0:NE, 0:128], in_=G[:, tt, :], identity=ident[:]),
                    reads=[], writes=[('ps', pg_)])
                T.op('act', lambda e, pg_=pg_, b=b: e.copy(out=gts[b][0:NE, :], in_=psb[pg_][0:NE, 0:128]),
                     reads=[('ps', pg_)], writes=[('gts', b)])
                for n in range(2):
                    pi = getps()
                    mmgroup(psb[pi][:, :], ('ps', pi), [(gts[b][0:NE, :], b_down[0:NE, 512 * n:512 * (n + 1)])],
                            reads=[('gts', b), 'b_down'])
                    T.op('act', lambda e, pi=pi, tt=tt, n=n: e.copy(
                        out=yacc[:, tt, 512 * n:512 * (n + 1)], in_=psb[pi][:, :]),
                        reads=[('ps', pi)], writes=[('y', tt, n)])
                for j in range(4):
                    g3 = ig % 3
                    ig += 1
                    T.dma('pool', ygt[g3][:, :], ys_d[:, :], writes=[('ygt', g3)],
                          in_offset=bass.IndirectOffsetOnAxis(ap=SLOT[:, 4 * tt + j:4 * tt + j + 1], axis=0))
                    T.op('dve', lambda e, g3=g3, tt=tt, j=j: e.scalar_tensor_tensor(
                        out=yacc[:, tt, :], in0=ygt[g3][:], scalar=GT[:, tt, j:j + 1], in1=yacc[:, tt, :],
                        op0=ALU.mult, op1=ALU.add),
                        reads=[('ygt', g3), ('y', tt, 0), ('y', tt, 1)], writes=[('y', tt, 0), ('y', tt, 1)])
                if tt >= 1:
                    p4_tile(tt - 1)
            p4_tile(NT - 1)

        T.finish()
    return nc


def _col(v):
    return np.ascontiguousarray(np.asarray(v, np.float32).reshape(8, 128).T)


def make_in_maps(inputs, cores):
    f = lambda a: np.ascontiguousarray(np.asarray(a, np.float32))
    x = f(inputs['x'])
    c = f(inputs['c'])
    shared = {
        "w_ada": f(inputs['w_ada'][0]),
        "b_ada_col": np.ascontiguousarray(f(inputs['b_ada'][0]).reshape(48, 128).T),
        "b_ada_row": f(inputs['b_ada'][0]).reshape(1, 6 * D),
        "w_in": f(inputs['w_in'][0]),
        "b_in_col": np.ascontiguousarray(f(inputs['b_in'][0]).reshape(48, 128).T),
        "w_cv_dw_col": np.ascontiguousarray(f(inputs['w_cv_dw'][0]).T.reshape(8, 128, 31).transpose(1, 0, 2)),
        "b_cv_dw_col": _col(inputs['b_cv_dw'][0]),
        "ln_cv_g_col": _col(inputs['ln_cv_g'][0]),
        "ln_cv_b_col": _col(inputs['ln_cv_b'][0]),
        "w_cv_out": f(inputs['w_cv_out'][0]),
        "w_lru_conv_col": np.ascontiguousarray(f(inputs['w_lru_conv'][0]).T.reshape(8, 128, 4).transpose(1, 0, 2)),
        "b_lru_conv_col": _col(inputs['b_lru_conv'][0]),
        "w_lru_a_t": np.ascontiguousarray(f(inputs['w_lru_a'][0]).transpose(1, 0, 2)),
        "b_lru_a_col": _col(inputs['b_lru_a'][0]),
        "w_lru_x_t": np.ascontiguousarray(f(inputs['w_lru_x'][0]).transpose(1, 0, 2)),
        "b_lru_x_col": _col(inputs['b_lru_x'][0]),
        "lru_lambda_col": _col(inputs['lru_lambda'][0]),
        "w_lru_out": f(inputs['w_lru_out'][0]),
        "w_o": f(inputs['w_o'][0]),
        "b_o_row": f(inputs['b_o'][0]).reshape(1, D),
        "ln1_g_row": f(inputs['ln1_g'][0]).reshape(1, D),
        "ln1_b_row": f(inputs['ln1_b'][0]).reshape(1, D),
        "w_router_t": np.ascontiguousarray(f(inputs['w_router'][0]).reshape(8, 128, NE).transpose(1, 0, 2)),
        "b_router_row": f(inputs['b_router'][0]).reshape(1, NE),
        "w_up": f(inputs['w_up'][0]),
        "b_up_rows": np.ascontiguousarray(f(inputs['b_up'][0]).reshape(NE, 8, 128, 2).transpose(0, 2, 1, 3)).reshape(NE * 128, 16),
        "w_down": f(inputs['w_down'][0]),
        "b_down": f(inputs['b_down'][0]),
        "ln2_g_row": f(inputs['ln2_g'][0]).reshape(1, D),
        "ln2_b_row": f(inputs['ln2_b'][0]).reshape(1, D),
    }
    maps = []
    for b in cores:
        m = dict(shared)
        m["x"] = np.ascontiguousarray(x[b])
        m["c_col"] = _col(c[b])
        maps.append(m)
    return maps


def kernel(**inputs):
    nc = build()
    maps = make_in_maps(inputs, list(range(8)))
    res = run_bass_kernel_spmd(nc, maps, core_ids=list(range(8)))
    out = np.stack([np.asarray(r["out"], np.float32) for r in res.results], axis=0)
    return out
```
